# Optimizing a Trainium2 kernel written in Bass

```python
import jax, jax.numpy as jnp
from jax import lax
import numpy as np

D_MODEL = 2048
BATCH = 16
SEQ = 256
DEPTH = 2
DEC_BATCH = 4
DEC_SEQ = 1024
PAST_LEN = 512

GRID_W = 64
N_MIXERS = 2
N_RET_LAYERS = (DEPTH + 1) // 2
N_NA_LAYERS = DEPTH // 2
RET_HEADS = 8
RET_DK = D_MODEL // RET_HEADS
RET_DV = 2 * RET_DK
RET_CHUNK = 64
NA_HEADS = 16
NA_DH = D_MODEL // NA_HEADS
WIN_H = 8
WIN_W = 16
Q_BLOCK = 128
N_GROUPS = 4
N_EXP_PER_GROUP = 8
N_EXPERTS = N_GROUPS * N_EXP_PER_GROUP
TOP_K_IN_GROUP = 2
D_EXPERT = D_MODEL // 4
ROPE_BASE = 10000.0
ALPHA = (2.0 * DEPTH) ** 0.25
BETA = (8.0 * DEPTH) ** -0.25
LN_EPS = 1e-5
F32 = jnp.float32
NEG_INF = -1e30

kernel_name = 'hybrid_retention_natten_hmoe_step'


def layer_norm(x, g, b):
    xf = x.astype(F32)
    mu = jnp.mean(xf, axis=-1, keepdims=True)
    var = jnp.mean(jnp.square(xf - mu), axis=-1, keepdims=True)
    y = (xf - mu) * lax.rsqrt(var + LN_EPS) * g.astype(F32) + b.astype(F32)
    return y.astype(x.dtype)


def modulation(c, w_mod, b_mod):
    m = jax.nn.silu(c) @ w_mod + b_mod
    return jnp.split(m[:, None, :], 6, axis=-1)


def heads_split(t, n_heads, d):
    b, n, _ = t.shape
    return t.reshape(b, n, n_heads, d).transpose(0, 2, 1, 3)


def axial_rope(x):
    b, h, n, dk = x.shape
    nf = dk // 4
    t = jnp.arange(n)
    rows = (t // GRID_W).astype(F32)
    cols = (t % GRID_W).astype(F32)
    inv_freq = ROPE_BASE ** (-jnp.arange(nf, dtype=F32) / nf)
    ang = jnp.stack([rows[:, None] * inv_freq, cols[:, None] * inv_freq], axis=1)
    cos, sin = jnp.cos(ang), jnp.sin(ang)
    xr = x.astype(F32).reshape(b, h, n, 2, 2, nf)
    x1, x2 = xr[..., 0, :], xr[..., 1, :]
    out = jnp.stack([x1 * cos - x2 * sin, x2 * cos + x1 * sin], axis=-2)
    return out.reshape(x.shape).astype(x.dtype)


def retention_chunkwise(q, k, v, log_g, s0):
    b, h, n, dk = q.shape
    dv = v.shape[-1]
    cs = RET_CHUNK
    nc = n // cs
    qc = q.reshape(b, h, nc, cs, dk)
    kc = k.reshape(b, h, nc, cs, dk)
    vc = v.reshape(b, h, nc, cs, dv)
    pos = jnp.arange(cs, dtype=F32)
    diff = pos[:, None] - pos[None, :]
    dmask = jnp.where(diff[None] >= 0, jnp.exp(jnp.maximum(diff, 0.0)[None] * log_g[:, None, None]), 0.0)
    inner = jnp.einsum('bhcid,bhcjd->bhcij', qc, kc) * dmask[None, :, None]
    o_inner = jnp.einsum('bhcij,bhcjv->bhciv', inner, vc)
    q_decay = jnp.exp((pos + 1.0)[None, :] * log_g[:, None])
    k_decay = jnp.exp((cs - 1.0 - pos)[None, :] * log_g[:, None])
    c_decay = jnp.exp(cs * log_g)[None, :, None, None]
    kv = jnp.einsum('bhcjd,bhcjv->cbhdv', kc * k_decay[None, :, None, :, None], vc)

    def step(s, kv_c):
        return c_decay * s + kv_c, s

    s_fin, s_prev = lax.scan(step, s0, kv)
    o_cross = jnp.einsum('bhcid,cbhdv->bhciv', qc * q_decay[None, :, None, :, None], s_prev)
    return (o_inner + o_cross).reshape(b, h, n, dv), s_fin


def head_group_norm(o, g, b):
    mu = jnp.mean(o, axis=-1, keepdims=True)
    var = jnp.mean(jnp.square(o - mu), axis=-1, keepdims=True)
    return (o - mu) * lax.rsqrt(var + LN_EPS) * g.astype(F32)[None, :, None, :] + b.astype(F32)[None, :, None, :]


def retention_mixer(h, s_init, w_in, decay_logit, gn_g, gn_b, w_out, use_rope):
    hk = RET_HEADS * RET_DK
    hv = RET_HEADS * RET_DV
    proj = h @ w_in
    q = heads_split(proj[..., :hk], RET_HEADS, RET_DK)
    k = heads_split(proj[..., hk:2 * hk], RET_HEADS, RET_DK)
    v = heads_split(proj[..., 2 * hk:2 * hk + hv], RET_HEADS, RET_DV).astype(F32)
    gf = heads_split(proj[..., 2 * hk + hv:2 * hk + 2 * hv], RET_HEADS, RET_DV).astype(F32)
    gb = heads_split(proj[..., 2 * hk + 2 * hv:], RET_HEADS, RET_DV).astype(F32)
    if use_rope:
        q = axial_rope(q)
        k = axial_rope(k)
    q = q.astype(F32)
    k = k.astype(F32) * (RET_DK ** -0.5)
    log_g = -jax.nn.softplus(-decay_logit.astype(F32))
    s0 = s_init.astype(F32)
    o_f, s_f = retention_chunkwise(q, k, v, log_g[0], s0[:, 0])
    o_b, s_b = retention_chunkwise(q[:, :, ::-1], k[:, :, ::-1], v[:, :, ::-1], log_g[1], s0[:, 1])
    o_b = o_b[:, :, ::-1]
    y = head_group_norm(o_f, gn_g, gn_b) * jax.nn.silu(gf) + head_group_norm(o_b, gn_g, gn_b) * jax.nn.silu(gb)
    b, n, _ = h.shape
    y = y.transpose(0, 2, 1, 3).reshape(b, n, hv).astype(h.dtype)
    return y @ w_out, jnp.stack([s_f, s_b], axis=1)


def dense_attention(q, k, v):
    b, h, s, dh = q.shape
    nb = s // Q_BLOCK
    qb = q.reshape(b, h, nb, Q_BLOCK, dh).transpose(2, 0, 1, 3, 4)

    def blk(qi):
        sc = jnp.einsum('bhqd,bhkd->bhqk', qi, k).astype(F32)
        p = jax.nn.softmax(sc, axis=-1).astype(v.dtype)
        return jnp.einsum('bhqk,bhkd->bhqd', p, v)

    o = lax.map(blk, qb)
    return o.transpose(1, 0, 3, 2, 4).reshape(b, s, h * dh)


def na_context(h, w_in, w_out):
    d = D_MODEL
    proj = h @ w_in
    q = heads_split(proj[..., :d], NA_HEADS, NA_DH) * (NA_DH ** -0.5)
    k = heads_split(proj[..., d:2 * d], NA_HEADS, NA_DH)
    v = heads_split(proj[..., 2 * d:], NA_HEADS, NA_DH)
    o = dense_attention(q, k, v)
    return o @ w_out, k, v


def na_latent(h, ck, cv, w_in, rpb, w_out):
    b, n, _ = h.shape
    d = D_MODEL
    rows = n // GRID_W
    kh = min(WIN_H, rows)
    kw = WIN_W
    proj = h @ w_in
    q = heads_split(proj[..., :d], NA_HEADS, NA_DH) * (NA_DH ** -0.5)
    k = heads_split(proj[..., d:2 * d], NA_HEADS, NA_DH)
    v = heads_split(proj[..., 2 * d:], NA_HEADS, NA_DH)
    qg = q.reshape(b, NA_HEADS, rows, GRID_W, NA_DH)
    kg = k.reshape(b, NA_HEADS, rows, GRID_W, NA_DH)
    vg = v.reshape(b, NA_HEADS, rows, GRID_W, NA_DH)
    col = jnp.arange(GRID_W)
    c0 = jnp.clip(col - kw // 2, 0, GRID_W - kw)
    col_ok = (col[None, :] >= c0[:, None]) & (col[None, :] < c0[:, None] + kw)
    dc_idx = jnp.clip(col[None, :] - col[:, None], -(WIN_W - 1), WIN_W - 1) + WIN_W - 1
    rpb_cols = rpb.astype(F32)[:, :, dc_idx]

    def row_block(r):
        r0 = jnp.clip(r - kh // 2, 0, rows - kh)
        kb = lax.dynamic_slice_in_dim(kg, r0, kh, axis=2)
        vb = lax.dynamic_slice_in_dim(vg, r0, kh, axis=2)
        qr = lax.dynamic_index_in_dim(qg, r, axis=2, keepdims=False)
        dr_idx = r0 + jnp.arange(kh) - r + WIN_H - 1
        bias = jnp.take(rpb_cols, dr_idx, axis=1).transpose(0, 2, 1, 3)
        s_loc = jnp.einsum('bhqd,bhrkd->bhqrk', qr, kb).astype(F32) + bias[None]
        s_loc = jnp.where(col_ok[:, None, :], s_loc, NEG_INF).reshape(b, NA_HEADS, GRID_W, kh * GRID_W)
        s_ctx = jnp.einsum('bhqd,bhpd->bhqp', qr, ck).astype(F32)
        p = jax.nn.softmax(jnp.concatenate([s_loc, s_ctx], axis=-1), axis=-1).astype(v.dtype)
        p_loc = p[..., :kh * GRID_W].reshape(b, NA_HEADS, GRID_W, kh, GRID_W)
        p_ctx = p[..., kh * GRID_W:]
        return jnp.einsum('bhqrk,bhrkd->bhqd', p_loc, vb) + jnp.einsum('bhqp,bhpd->bhqd', p_ctx, cv)

    o = lax.map(row_block, jnp.arange(rows))
    o = o.transpose(1, 0, 3, 2, 4).reshape(b, n, d)
    return o @ w_out


def hier_moe(h, w_rg, b_rg, w_re, b_re, w_gate, w_up, w_down):
    b, n, d = h.shape
    x = h.reshape(b * n, d)
    g_logits = (x @ w_rg).astype(F32) + b_rg.astype(F32)
    p_group = jax.nn.softmax(g_logits, axis=-1)
    g_sel = jnp.argmax(g_logits, axis=-1)
    p_g = jnp.take_along_axis(p_group, g_sel[:, None], axis=1)
    e_logits = jnp.einsum('td,gde->tge', x, w_re).astype(F32) + b_re.astype(F32)
    e_sel = jnp.take_along_axis(e_logits, g_sel[:, None, None], axis=1)[:, 0]
    top_p, top_i = lax.top_k(jax.nn.softmax(e_sel, axis=-1), TOP_K_IN_GROUP)
    top_p = top_p / jnp.sum(top_p, axis=-1, keepdims=True)
    expert_ids = g_sel[:, None] * N_EXP_PER_GROUP + top_i
    combine = jnp.sum(jax.nn.one_hot(expert_ids, N_EXPERTS, dtype=F32) * (p_g * top_p)[..., None], axis=1)
    hid = jax.nn.silu(jnp.einsum('td,edf->tef', x, w_gate)) * jnp.einsum('td,edf->tef', x, w_up)
    hid = hid * combine[:, :, None].astype(hid.dtype)
    y = jnp.einsum('tef,efd->td', hid, w_down)
    return y.reshape(b, n, d)


def setup_inputs(seed: int = 0) -> dict:
    key = jax.random.key(seed)
    ks = jax.random.split(key, 32)

    def nrm(k, shape, scale):
        return jax.random.normal(k, shape, F32) * scale

    d = D_MODEL
    hk = RET_HEADS * RET_DK
    hv = RET_HEADS * RET_DV
    gam = 1.0 - 2.0 ** (-5.0 - np.arange(RET_HEADS, dtype=np.float32))
    base_logit = jnp.asarray(np.log(gam / (1.0 - gam)).astype(np.float32))
    return {
        'x_prompt': nrm(ks[0], (BATCH, SEQ, d), 1.0),
        'x_sample': nrm(ks[1], (DEC_BATCH, DEC_SEQ, d), 1.0),
        'state_ret': nrm(ks[2], (DEC_BATCH, N_RET_LAYERS, 2, RET_HEADS, RET_DK, RET_DV), 0.5),
        'cache_na_k': nrm(ks[3], (DEC_BATCH, N_NA_LAYERS, NA_HEADS, PAST_LEN, NA_DH), 1.0),
        'cache_na_v': nrm(ks[4], (DEC_BATCH, N_NA_LAYERS, NA_HEADS, PAST_LEN, NA_DH), 1.0),
        'c': nrm(ks[5], (DEC_BATCH, d), 1.0),
        'c_ctx': nrm(ks[6], (d,), 1.0),
        'w_mod': nrm(ks[7], (DEPTH, d, 6 * d), 0.5 * d ** -0.5),
        'b_mod': nrm(ks[8], (DEPTH, 6 * d), 0.02),
        'ln1_g': 1.0 + nrm(ks[9], (DEPTH, d), 0.01),
        'ln1_b': nrm(ks[10], (DEPTH, d), 0.01),
        'ln2_g': 1.0 + nrm(ks[11], (DEPTH, d), 0.01),
        'ln2_b': nrm(ks[12], (DEPTH, d), 0.01),
        'w_ret_in': nrm(ks[13], (N_RET_LAYERS, d, 2 * hk + 3 * hv), d ** -0.5),
        'ret_decay_logit': base_logit[None, None, :] + nrm(ks[14], (N_RET_LAYERS, 2, RET_HEADS), 0.01),
        'ret_gn_g': 1.0 + nrm(ks[15], (N_RET_LAYERS, RET_HEADS, RET_DV), 0.01),
        'ret_gn_b': nrm(ks[16], (N_RET_LAYERS, RET_HEADS, RET_DV), 0.01),
        'w_ret_out': nrm(ks[17], (N_RET_LAYERS, hv, d), BETA * hv ** -0.5),
        'w_na_in': nrm(ks[18], (N_NA_LAYERS, d, 3 * d), d ** -0.5),
        'na_rpb': nrm(ks[19], (N_NA_LAYERS, NA_HEADS, 2 * WIN_H - 1, 2 * WIN_W - 1), 0.1),
        'w_na_out': nrm(ks[20], (N_NA_LAYERS, d, d), BETA * d ** -0.5),
        'w_rg': nrm(ks[21], (DEPTH, d, N_GROUPS), d ** -0.5),
        'b_rg': nrm(ks[22], (DEPTH, N_GROUPS), 0.01),
        'w_re': nrm(ks[23], (DEPTH, N_GROUPS, d, N_EXP_PER_GROUP), d ** -0.5),
        'b_re': nrm(ks[24], (DEPTH, N_GROUPS, N_EXP_PER_GROUP), 0.01),
        'w_gate': nrm(ks[25], (DEPTH, N_EXPERTS, d, D_EXPERT), d ** -0.5),
        'w_up': nrm(ks[26], (DEPTH, N_EXPERTS, d, D_EXPERT), d ** -0.5),
        'w_down': nrm(ks[27], (DEPTH, N_EXPERTS, D_EXPERT, d), BETA * D_EXPERT ** -0.5),
    }


def reference(x_prompt, x_sample, state_ret, cache_na_k, cache_na_v, c, c_ctx, w_mod, b_mod, ln1_g, ln1_b, ln2_g, ln2_b, w_ret_in, ret_decay_logit, ret_gn_g, ret_gn_b, w_ret_out, w_na_in, na_rpb, w_na_out, w_rg, b_rg, w_re, b_re, w_gate, w_up, w_down):
    xp, xs = x_prompt, x_sample
    bp = xp.shape[0]
    new_ret, new_k, new_v = [], [], []
    for l in range(DEPTH):
        j = l // N_MIXERS
        mp_ = modulation(c_ctx[None, :], w_mod[l], b_mod[l])
        ms_ = modulation(c, w_mod[l], b_mod[l])
        hp = xp * (1.0 + mp_[1]) + mp_[0]
        hs = xs * (1.0 + ms_[1]) + ms_[0]
        if l % N_MIXERS == 0:
            s_zero = jnp.zeros((bp, 2, RET_HEADS, RET_DK, RET_DV), F32)
            op, sp = retention_mixer(hp, s_zero, w_ret_in[j], ret_decay_logit[j], ret_gn_g[j], ret_gn_b[j], w_ret_out[j], False)
            os_, _ = retention_mixer(hs, state_ret[:, j], w_ret_in[j], ret_decay_logit[j], ret_gn_g[j], ret_gn_b[j], w_ret_out[j], True)
            new_ret.append(sp)
        else:
            op, kp, vp = na_context(hp, w_na_in[j], w_na_out[j])
            os_ = na_latent(hs, cache_na_k[:, j], cache_na_v[:, j], w_na_in[j], na_rpb[j], w_na_out[j])
            new_k.append(kp)
            new_v.append(vp)
        xp = layer_norm(ALPHA * xp + mp_[2] * op, ln1_g[l], ln1_b[l])
        xs = layer_norm(ALPHA * xs + ms_[2] * os_, ln1_g[l], ln1_b[l])
        hp = xp * (1.0 + mp_[4]) + mp_[3]
        hs = xs * (1.0 + ms_[4]) + ms_[3]
        yp = hier_moe(hp, w_rg[l], b_rg[l], w_re[l], b_re[l], w_gate[l], w_up[l], w_down[l])
        ys = hier_moe(hs, w_rg[l], b_rg[l], w_re[l], b_re[l], w_gate[l], w_up[l], w_down[l])
        xp = layer_norm(ALPHA * xp + mp_[5] * yp, ln2_g[l], ln2_b[l])
        xs = layer_norm(ALPHA * xs + ms_[5] * ys, ln2_g[l], ln2_b[l])
    return (xp, xs, jnp.stack(new_ret, axis=1), jnp.stack(new_k, axis=1), jnp.stack(new_v, axis=1))
```

```python
from contextlib import ExitStack
import numpy as np
import concourse.bass as bass
import concourse.mybir as mybir
from concourse.bass_utils import run_bass_kernel_spmd

F32 = mybir.dt.float32
BF16 = mybir.dt.bfloat16
AF = mybir.ActivationFunctionType
ALU = mybir.AluOpType

D = 2048
NTOK = 1024
NT = 8
NCH = 16
ALPHA = (2.0 * 2) ** 0.25
EPS = 1e-5
NEG = -1e30
MOE_STAGE = 3


class Res:
    __slots__ = ("name", "last_w", "readers")

    def __init__(self, name=""):
        self.name = name
        self.last_w = None
        self.readers = {}


class Op:
    __slots__ = ("eng", "fn", "deps", "signal", "count", "is_dma", "dma_key", "epoch")

    def __init__(self, eng, fn):
        self.epoch = 0
        self.eng = eng
        self.fn = fn
        self.deps = []
        self.signal = False
        self.count = 0
        self.is_dma = False
        self.dma_key = None


ENGINES = ("pe", "act", "dve", "pool", "sp")


class Sched:
    def __init__(self, nc):
        self.nc = nc
        self.ops = {e: [] for e in ENGINES}
        self.dma_count = {}
        self.dma_kind = {}
        self.last_dma = {}
        self.epoch = 0

    def _track(self, op, reads, writes):
        op.epoch = self.epoch
        deps = []
        for r in reads:
            if r.last_w is not None:
                deps.append(r.last_w)
        for w in writes:
            if w.last_w is not None:
                deps.append(w.last_w)
            deps.extend(w.readers.values())
        seen = set()
        for d in deps:
            if d is op or id(d) in seen:
                continue
            seen.add(id(d))
            if d.eng == "pe" and op.eng == "pe" and not d.is_dma and not op.is_dma:
                continue
            if d.epoch < self.epoch:
                continue
            op.deps.append((d, self.dma_count[d.dma_key] if d.is_dma else None))
            d.signal = True
        for r in reads:
            r.readers[op.dma_key if op.is_dma else op.eng] = op
        for w in writes:
            w.last_w = op
            w.readers = {}

    def add(self, eng, fn, reads=(), writes=()):
        op = Op(eng, fn)
        self._track(op, reads, writes)
        self.ops[eng].append(op)
        return op

    def dma(self, eng, fn, reads=(), writes=(), key="d"):
        kind = "sw" if eng == "pool" else "hw"
        key = key + "_" + kind
        self.dma_kind[key] = kind
        op = Op(eng, fn)
        op.is_dma = True
        op.dma_key = key
        self.dma_count.setdefault(key, 0)
        self._track(op, reads, writes)
        self.dma_count[key] += 1
        op.count = self.dma_count[key]
        self.ops[eng].append(op)
        self.last_dma[key] = op
        return op

    def barrier(self):
        lasts = []
        for e in ENGINES:
            for op in reversed(self.ops[e]):
                if not op.is_dma:
                    lasts.append(op)
                    break
        lasts.extend(self.last_dma.values())
        for e in ENGINES:
            op = Op(e, lambda eng: eng.nop(nofuse=True))
            for d in lasts:
                if d.eng == e and not d.is_dma:
                    continue
                op.deps.append((d, self.dma_count[d.dma_key] if d.is_dma else None))
                d.signal = True
            op.epoch = self.epoch
            self.ops[e].append(op)
        self.epoch += 1

    def emit(self, stack, block):
        nc = self.nc
        sems = {(e, ep): stack.enter_context(nc.semaphore("s_%s_%d" % (e, ep))) for e in ENGINES for ep in range(self.epoch + 1)}
        dsems = {k: stack.enter_context(nc.semaphore("d_" + k)) for k in self.dma_kind}
        for e in ENGINES:
            c = {}
            for op in self.ops[e]:
                if not op.is_dma and op.signal:
                    c[op.epoch] = c.get(op.epoch, 0) + 1
                    op.count = c[op.epoch]
            print("sched", e, "signals per epoch", c)

        def run(e, engine):
            known = {}
            for op in self.ops[e]:
                need = {}
                for d, dc_ in op.deps:
                    if d.is_dma:
                        s, v = dsems[d.dma_key], 16 * dc_
                    else:
                        s, v = sems[(d.eng, d.epoch)], d.count
                    k = id(s)
                    if k not in need or need[k][1] < v:
                        need[k] = (s, v)
                for k, (s, v) in need.items():
                    if known.get(k, 0) >= v:
                        continue
                    engine.wait_ge(s, v)
                    known[k] = v
                ins = op.fn(engine)
                if op.is_dma:
                    ins.then_inc(dsems[op.dma_key], 16)
                elif op.signal:
                    ins.then_inc(sems[(e, op.epoch)], 1)
            if e in ("pool", "sp"):
                kind = "sw" if e == "pool" else "hw"
                for k, kd in self.dma_kind.items():
                    if kd == kind:
                        engine.wait_ge(dsems[k], 16 * self.dma_count[k])

        block.tensor(lambda eng: run("pe", eng))
        block.scalar(lambda eng: run("act", eng))
        block.vector(lambda eng: run("dve", eng))
        block.gpsimd(lambda eng: run("pool", eng))
        block.sync(lambda eng: run("sp", eng))


def build(upto=6):
    nc = bass.Bass("TRN2", target_bir_lowering=False)

    def din(name, shape, dt=F32):
        return nc.dram_tensor(name, list(shape), dt, kind="ExternalInput").ap()

    def dout(name, shape, dt=F32):
        return nc.dram_tensor(name, list(shape), dt, kind="ExternalOutput").ap()

    def dint(name, shape, dt=F32):
        return nc.dram_tensor(name, list(shape), dt).ap()

    x_in = din("x_in", [NTOK, D])
    cvec = din("cvec", [128, 16])
    s0 = din("s0", [2, 8, 256, 512])
    flags = din("flags", [1, 8])
    ropec = din("ropec", [64, NCH, 256])
    ropes = din("ropes", [64, NCH, 256])
    consts = din("consts", [128, 1024])
    ck = din("ck", [16, 512, 128])
    cv = din("cv", [16, 512, 128])
    nabias = din("nabias", [64, 16, 15, 64])
    narow = din("narow", [1, 16, 16, 64])
    w_mod = [din("w_mod%d" % i, [D, 6 * D]) for i in range(2)]
    b_mod = din("b_mod", [2, 6 * D])
    ln1_g = din("ln1_g", [2, D]); ln1_b = din("ln1_b", [2, D])
    ln2_g = din("ln2_g", [2, D]); ln2_b = din("ln2_b", [2, D])
    w_ret_in = din("w_ret_in", [D, 16384])
    decay = din("decay", [1, 16])
    gn_g = din("gn_g", [8, 512]); gn_b = din("gn_b", [8, 512])
    w_ret_out = din("w_ret_out", [4096, D])
    w_na_in = din("w_na_in", [D, 3 * D])
    w_na_out = din("w_na_out", [D, D])
    w_rt = din("w_rt", [2, D, 36])
    b_rt = din("b_rt", [2, 36])
    w_gate = [din("w_gate%d" % i, [32, D, 512]) for i in range(2)]
    w_up = [din("w_up%d" % i, [32, D, 512]) for i in range(2)]
    w_down = [din("w_down%d" % i, [32, 512, D]) for i in range(2)]

    y_out = dout("y_out", [NTOK, D])
    st_out = dout("st_out", [4, 2, 8, 256, 512])
    k_out = dout("k_out", [16, NTOK, 128])
    v_out = dout("v_out", [16, NTOK, 128])

    m_dram = dint("m_dram", [2, 6 * D])
    xs = dint("xs", [NTOK, D])
    yT_dram = dint("yT_dram", [128, 32, NTOK], BF16)

    S = Sched(nc)
    stack = ExitStack()
    E = stack.enter_context

    AW = 48640
    arena = E(nc.sbuf_tensor("arena", [128, AW], F32))
    ident = E(nc.sbuf_tensor("ident", [128, 128], F32))
    identb = E(nc.sbuf_tensor("identb", [128, 128], BF16))
    onesf = E(nc.sbuf_tensor("onesf", [128, 128], F32))
    onesb = E(nc.sbuf_tensor("onesb", [1, 64], BF16))
    cst = E(nc.sbuf_tensor("cst", [128, 1024], F32))
    sil = E(nc.sbuf_tensor("sil", [128, 16], F32))
    flg = E(nc.sbuf_tensor("flg", [128, 8], F32))
    lg = E(nc.sbuf_tensor("lg", [128, 16], F32))
    small = E(nc.sbuf_tensor("small", [128, 64], F32))
    stt = E(nc.sbuf_tensor("stt", [128, 4, 6], F32))
    comb = E(nc.sbuf_tensor("comb", [128, NT, 32], F32))
    rl = E(nc.sbuf_tensor("rl", [128, 96], F32))
    rbias = E(nc.sbuf_tensor("rbias", [128, 2, 36], F32))

    ps = [E(nc.psum_tensor("ps%d" % i, [128, 512], F32)) for i in range(7)]
    psb = E(nc.psum_tensor("psb", [128, 1024], BF16))
    PS = [Res("ps%d" % i) for i in range(7)]
    PSB = Res("psb")

    R = {}

    def res(name):
        if name not in R:
            R[name] = Res(name)
        return R[name]

    class Arena:
        def __init__(self):
            self.off = 0

        def reset(self):
            self.off = 0

        def f32(self, shape):
            n = int(np.prod(shape[1:]))
            v = arena[0:shape[0], self.off:self.off + n]
            self.off += n
            assert self.off <= AW, ("arena overflow", self.off)
            return v if len(shape) == 2 else v.rearrange(
                "p (a b) -> p a b", b=shape[2]) if len(shape) == 3 else v.rearrange(
                "p (a b c) -> p a b c", b=shape[2], c=shape[3])

        def bf16(self, shape):
            n = int(np.prod(shape[1:]))
            assert n % 2 == 0
            v = arena[0:shape[0], self.off:self.off + n // 2].bitcast(BF16)
            self.off += n // 2
            assert self.off <= AW, ("arena overflow", self.off)
            return v if len(shape) == 2 else v.rearrange(
                "p (a b) -> p a b", b=shape[2]) if len(shape) == 3 else v.rearrange(
                "p (a b c) -> p a b c", b=shape[2], c=shape[3])

    A = Arena()

    S.dma("sp", lambda e: e.dma_start(out=cst[:], in_=consts[:, :]), writes=[res("cst")], key="c0")
    S.dma("sp", lambda e: e.dma_start(out=sil[:], in_=cvec[:, :]), writes=[res("sil")], key="c0")
    S.dma("sp", lambda e: e.dma_start(out=flg[:], in_=flags[0, :].partition_broadcast(128)), writes=[res("flg")], key="c0")
    S.dma("sp", lambda e: e.dma_start(out=lg[:], in_=decay[0, :].partition_broadcast(128)), writes=[res("lg")], key="c0")
    S.dma("sp", lambda e: e.dma_start(out=rbias[:, 0, :], in_=b_rt[0, :].partition_broadcast(128)), writes=[res("rbias")], key="c0")
    S.dma("sp", lambda e: e.dma_start(out=rbias[:, 1, :], in_=b_rt[1, :].partition_broadcast(128)), writes=[res("rbias")], key="c0")
    S.add("dve", lambda e: e.tensor_copy(ident[:], cst[:, 0:128]), reads=[res("cst")], writes=[res("ident")])
    S.add("dve", lambda e: e.tensor_copy(identb[:], cst[:, 0:128]), reads=[res("cst")], writes=[res("identb")])
    S.add("dve", lambda e: e.tensor_copy(onesf[:], cst[:, 128:256]), reads=[res("cst")], writes=[res("onesf")])
    S.add("dve", lambda e: e.tensor_copy(onesb[:], cst[0:1, 128:192]), reads=[res("cst")], writes=[res("onesb")])
    S.add("act", lambda e: e.activation(sil[:], sil[:], AF.Silu), reads=[res("sil")], writes=[res("sil")])
    S.add("act", lambda e: e.activation(lg[:], lg[:], AF.Sigmoid), reads=[res("lg")], writes=[res("lg")])
    S.add("act", lambda e: e.activation(lg[:], lg[:], AF.Ln), reads=[res("lg")], writes=[res("lg")])

    A.reset()
    macc = A.f32([128, D])
    mbias = A.f32([128, D])
    mring = [A.f32([128, D]) for _ in range(3)]
    MR = [Res("mring%d" % i) for i in range(3)]
    pi = 0
    for l in range(2):
        for cb in range(6):
            S.dma("sp", lambda e, l=l, cb=cb: e.dma_start(out=mbias[:], in_=b_mod[l, cb * D:(cb + 1) * D].partition_broadcast(128)),
                  writes=[res("mbias")], key="mb")
            for kc in range(16):
                slot = pi % 3
                pi += 1
                S.dma("sp", lambda e, l=l, cb=cb, kc=kc, slot=slot: e.dma_start(
                    out=mring[slot][:], in_=w_mod[l][kc * 128:(kc + 1) * 128, cb * D:(cb + 1) * D]),
                    writes=[MR[slot]], key="mr%d" % slot)
                if kc == 0:
                    S.add("dve", lambda e, kc=kc, slot=slot: e.tensor_scalar(macc[:], mring[slot][:], sil[:, kc:kc + 1], None, op0=ALU.mult),
                          reads=[MR[slot], res("sil")], writes=[res("macc")])
                else:
                    S.add("dve", lambda e, kc=kc, slot=slot: e.scalar_tensor_tensor(
                        macc[:], mring[slot][:], sil[:, kc:kc + 1], macc[:], op0=ALU.mult, op1=ALU.add),
                        reads=[MR[slot], res("sil"), res("macc")], writes=[res("macc")])
            for j in range(4):
                S.add("pe", lambda e, j=j: e.matmul(ps[j][:], onesf[:], macc[:, j * 512:(j + 1) * 512], start=True, stop=True),
                      reads=[res("onesf"), res("macc")], writes=[PS[j]])
                S.add("dve", lambda e, j=j: e.tensor_tensor(mbias[:, j * 512:(j + 1) * 512], ps[j][:], mbias[:, j * 512:(j + 1) * 512], op=ALU.add),
                      reads=[PS[j], res("mbias")], writes=[res("mbias")])
            S.dma("sp", lambda e, l=l, cb=cb: e.dma_start(out=m_dram[l:l + 1, cb * D:(cb + 1) * D], in_=mbias[0:1, :]),
                  reads=[res("mbias")], writes=[res("m_dram")], key="mo")
    S.barrier()

    def bc_load(dst, src_row, r, key):
        S.dma("sp", lambda e: e.dma_start(out=dst, in_=src_row.partition_broadcast(128)), reads=[res("m_dram")], writes=[r], key=key)

    ev_flip = [0]

    def evac(dst, src, rd, wr, scale=None):
        ev_flip[0] ^= 1
        if ev_flip[0]:
            if scale is None:
                S.add("act", lambda e: e.copy(dst, src), reads=rd, writes=wr)
            else:
                S.add("act", lambda e: e.mul(dst, src, scale), reads=rd, writes=wr)
        else:
            if scale is None:
                S.add("dve", lambda e: e.tensor_copy(dst, src), reads=rd, writes=wr)
            else:
                S.add("dve", lambda e: e.tensor_scalar(dst, src, scale, None, op0=ALU.mult), reads=rd, writes=wr)

    def to_feature_major(h_ap, h_res, hT, t, hT32=None):
        for g in range(4):
            b = g % 4
            for q in range(4):
                kc = g * 4 + q
                S.add("pe", lambda e, kc=kc, b=b, q=q: e.transpose(ps[b][:, q * 128:(q + 1) * 128], h_ap[:, kc * 128:(kc + 1) * 128], ident[:]),
                      reads=[h_res, res("ident")], writes=[PS[b]])
            evac(hT[:, g * 4:(g + 1) * 4, t * 128:(t + 1) * 128], ps[b][:].rearrange("p (a b) -> p a b", b=128),
                 [PS[b]], [res("hT%d" % t), res("pstok%d" % b)])
            if hT32 is not None:
                S.add("dve", lambda e, g=g, b=b: e.tensor_copy(hT32[:, g * 4:(g + 1) * 4, :], ps[b][:].rearrange("p (a b) -> p a b", b=128)),
                      reads=[PS[b]], writes=[res("hT32"), res("pstok%d" % b)])

    def layer_norm_tile(u, u_res, g_bc, b_bc, out_ap, out_res, rd):
        for j in range(4):
            S.add("dve", lambda e, j=j: e.bn_stats(stt[:, j, :], u[:, j * 512:(j + 1) * 512]), reads=[u_res], writes=[res("stt")])
        S.add("dve", lambda e: e.bn_aggr(small[:, 0:2], stt[:]), reads=[res("stt")], writes=[res("small")])
        S.add("dve", lambda e: e.tensor_scalar(small[:, 2:3], small[:, 1:2], EPS, None, op0=ALU.add), reads=[res("small")], writes=[res("small")])
        S.add("act", lambda e: e.activation(small[:, 2:3], small[:, 2:3], AF.Ln), reads=[res("small")], writes=[res("small")])
        S.add("act", lambda e: e.activation(small[:, 2:3], small[:, 2:3], AF.Exp, scale=-0.5), reads=[res("small")], writes=[res("small")])
        S.add("dve", lambda e: e.tensor_scalar(u, u, small[:, 0:1], small[:, 2:3], op0=ALU.subtract, op1=ALU.mult),
              reads=[u_res, res("small")], writes=[u_res])
        S.add("dve", lambda e: e.tensor_tensor(u, u, g_bc, op=ALU.mult), reads=[u_res] + rd, writes=[u_res])
        S.add("dve", lambda e: e.tensor_tensor(out_ap, u, b_bc, op=ALU.add), reads=[u_res] + rd, writes=[out_res])

    def stream_w(slot_ap, src_ap, slot_res, key):
        S.dma("pool", lambda e: e.dma_start(out=slot_ap, in_=src_ap), writes=[slot_res], key=key)

    def prologue(l, x_src, hT, j_shift, j_scale):
        sc = A.f32([128, D]); sh = A.f32([128, D])
        xw = [A.f32([128, D]) for _ in range(2)]
        bc_load(sc, m_dram[l, j_scale * D:(j_scale + 1) * D], res("sc"), "bc")
        bc_load(sh, m_dram[l, j_shift * D:(j_shift + 1) * D], res("sh"), "bc")
        S.add("dve", lambda e: e.tensor_scalar(sc, sc, 1.0, None, op0=ALU.add), reads=[res("sc")], writes=[res("sc")])
        for t in range(NT):
            xt = xw[t % 2]
            xr = res("xw%d" % (t % 2))
            S.dma("sp", lambda e, t=t, xt=xt: e.dma_start(out=xt, in_=x_src[t * 128:(t + 1) * 128, :]), reads=[res("xs")], writes=[xr], key="xl%d" % (t % 2))
            S.add("dve", lambda e, xt=xt: e.tensor_tensor(xt, xt, sc, op=ALU.mult), reads=[xr, res("sc")], writes=[xr])
            S.add("dve", lambda e, xt=xt: e.tensor_tensor(xt, xt, sh, op=ALU.add), reads=[xr, res("sh")], writes=[xr])
            to_feature_major(xt, xr, hT, t)

    def epilogue(l, x_src, o_half, th, gate_j, lng, lnb, next_mod, hT_next, want_router, x_dst, l_router):
        pass

    def moe_block(l, x_src, x_dst, next_l):
        A.reset()
        hT = A.bf16([128, 16, NTOK])
        yacc = A.f32([128, NT, D])
        hid = A.bf16([128, 4, NTOK])
        ring = [A.bf16([128, 8192]) for _ in range(3)]
        RG = [Res("ring%d" % i) for i in range(3)]
        sgt = [A.bf16([128, 512]) for _ in range(2)]
        mark = A.off
        sc = A.f32([128, D]); sh = A.f32([128, D])
        xw = [A.f32([128, D])] * 2
        hT32 = A.f32([128, 16, 128])
        wr32 = A.f32([128, 16, 36])
        bc_load(sc, m_dram[l, 4 * D:5 * D], res("sc"), "bc")
        bc_load(sh, m_dram[l, 3 * D:4 * D], res("sh"), "bc")
        S.add("dve", lambda e: e.tensor_scalar(sc, sc, 1.0, None, op0=ALU.add), reads=[res("sc")], writes=[res("sc")])
        S.dma("sp", lambda e: e.dma_start(out=wr32, in_=w_rt[l].rearrange("(kc p) c -> p kc c", p=128)), writes=[res("wr32")], key="wr")
        for t in range(NT):
            xt = xw[0]
            xr = res("xw0")
            S.dma("sp", lambda e, t=t, xt=xt: e.dma_start(out=xt, in_=x_src[t * 128:(t + 1) * 128, :]), reads=[res("xs")], writes=[xr], key="xl0")
            S.add("dve", lambda e, xt=xt: e.tensor_tensor(xt, xt, sc, op=ALU.mult), reads=[xr, res("sc")], writes=[xr])
            S.add("dve", lambda e, xt=xt: e.tensor_tensor(xt, xt, sh, op=ALU.add), reads=[xr, res("sh")], writes=[xr])
            to_feature_major(xt, xr, hT, t, hT32=(hT32 if MOE_STAGE >= 0.5 else None))
            if MOE_STAGE < 1:
                continue
            for kc in range(16):
                S.add("pe", lambda e, kc=kc: e.matmul(ps[4][:, 0:36], hT32[:, kc, :], wr32[:, kc, :], start=(kc == 0), stop=(kc == 15)),
                      reads=[res("hT32"), res("wr32")], writes=[PS[4]])
            rr = res("rl")
            S.add("dve", lambda e: e.tensor_tensor(rl[:, 0:36], ps[4][:, 0:36], rbias[:, l, :], op=ALU.add), reads=[PS[4], res("rbias")], writes=[rr])
            S.add("dve", lambda e: e.tensor_reduce(small[:, 8:9], rl[:, 0:4], mybir.AxisListType.X, ALU.max), reads=[rr], writes=[res("sm_g")])
            S.add("dve", lambda e: e.tensor_scalar(rl[:, 40:44], rl[:, 0:4], small[:, 8:9], None, op0=ALU.is_equal), reads=[rr, res("sm_g")], writes=[rr])
            S.add("dve", lambda e: e.tensor_scalar(rl[:, 44:48], rl[:, 0:4], small[:, 8:9], None, op0=ALU.subtract), reads=[rr, res("sm_g")], writes=[rr])
            S.add("act", lambda e: e.activation(rl[:, 44:48], rl[:, 44:48], AF.Exp, accum_out=small[:, 9:10]), reads=[rr], writes=[rr, res("sm_g2")])
            S.add("dve", lambda e: e.reciprocal(small[:, 10:11], small[:, 9:10]), reads=[res("sm_g2")], writes=[res("sm_pg")])
            S.add("dve", lambda e: e.tensor_scalar(rl[:, 40:44], rl[:, 40:44], 1.0, 1e30, op0=ALU.subtract, op1=ALU.mult), reads=[rr], writes=[rr])
            for g_ in range(4):
                S.add("dve", lambda e, g_=g_: e.tensor_scalar(rl[:, 48 + 8 * g_:56 + 8 * g_], rl[:, 4 + 8 * g_:12 + 8 * g_], rl[:, 40 + g_:41 + g_], None, op0=ALU.add),
                      reads=[rr], writes=[rr])
            S.add("dve", lambda e: e.tensor_reduce(small[:, 11:12], rl[:, 48:80], mybir.AxisListType.X, ALU.max), reads=[rr], writes=[res("sm_m1")])
            S.add("dve", lambda e, t=t: e.tensor_scalar(comb[:, t, :], rl[:, 48:80], small[:, 11:12], None, op0=ALU.is_equal), reads=[rr, res("sm_m1")], writes=[res("comb")])
            S.add("dve", lambda e, t=t: e.scalar_tensor_tensor(rl[:, 48:80], comb[:, t, :], -1e30, rl[:, 48:80], op0=ALU.mult, op1=ALU.add),
                  reads=[rr, res("comb")], writes=[rr])
            S.add("dve", lambda e: e.tensor_reduce(small[:, 12:13], rl[:, 48:80], mybir.AxisListType.X, ALU.max), reads=[rr], writes=[res("sm_m2")])
            S.add("dve", lambda e: e.tensor_scalar(rl[:, 4:36], rl[:, 48:80], small[:, 12:13], None, op0=ALU.is_equal), reads=[rr, res("sm_m2")], writes=[rr])
            S.add("dve", lambda e: e.tensor_tensor(small[:, 13:14], small[:, 12:13], small[:, 11:12], op=ALU.subtract), reads=[res("sm_m1"), res("sm_m2")], writes=[res("sm_w")])
            S.add("act", lambda e: e.activation(small[:, 13:14], small[:, 13:14], AF.Exp), reads=[res("sm_w")], writes=[res("sm_w")])
            S.add("dve", lambda e: e.tensor_scalar(small[:, 14:15], small[:, 13:14], 1.0, None, op0=ALU.add), reads=[res("sm_w")], writes=[res("sm_d")])
            S.add("dve", lambda e: e.reciprocal(small[:, 14:15], small[:, 14:15]), reads=[res("sm_d")], writes=[res("sm_d")])
            S.add("dve", lambda e: e.tensor_tensor(small[:, 15:16], small[:, 14:15], small[:, 10:11], op=ALU.mult), reads=[res("sm_d"), res("sm_pg")], writes=[res("sm_t1")])
            S.add("dve", lambda e: e.tensor_tensor(small[:, 16:17], small[:, 15:16], small[:, 13:14], op=ALU.mult), reads=[res("sm_t1"), res("sm_w")], writes=[res("sm_t2")])
            S.add("dve", lambda e, t=t: e.tensor_scalar(comb[:, t, :], comb[:, t, :], small[:, 15:16], None, op0=ALU.mult), reads=[res("comb"), res("sm_t1")], writes=[res("comb")])
            S.add("dve", lambda e, t=t: e.scalar_tensor_tensor(comb[:, t, :], rl[:, 4:36], small[:, 16:17], comb[:, t, :], op0=ALU.mult, op1=ALU.add),
                  reads=[rr, res("sm_t2"), res("comb")], writes=[res("comb")])
        if MOE_STAGE < 2:
            S.barrier()
            return
        HT = [res("hT%d" % t) for t in range(NT)]
        si = [0]

        def nxt():
            s = si[0] % 3
            si[0] += 1
            return s
        for ex in range(32):
            sg_, su_, sd_ = nxt(), nxt(), nxt()
            G = ring[sg_].rearrange("p (k c) -> p k c", c=512)
            U = ring[su_].rearrange("p (k c) -> p k c", c=512)
            Dw = ring[sd_].rearrange("p (k c) -> p k c", c=D)
            stream_w(G, w_gate[l][ex].rearrange("(kc p) c -> p kc c", p=128), RG[sg_], "rg%d" % sg_)
            stream_w(U, w_up[l][ex].rearrange("(kc p) c -> p kc c", p=128), RG[su_], "rg%d" % su_)
            stream_w(Dw, w_down[l][ex].rearrange("(kc p) c -> p kc c", p=128), RG[sd_], "rg%d" % sd_)
            it = 0
            for th in range(2):
                for fc in range(4):
                    bg, bu = (0, 1) if it % 2 == 0 else (2, 3)
                    sgi = it % 2
                    it += 1
                    for kc in range(16):
                        S.add("pe", lambda e, kc=kc, fc=fc, th=th, bg=bg, G=G: e.matmul(ps[bg][:], G[:, kc, fc * 128:(fc + 1) * 128], hT[:, kc, th * 512:(th + 1) * 512],
                                                                                 start=(kc == 0), stop=(kc == 15)),
                              reads=[RG[sg_]] + HT[th * 4:(th + 1) * 4], writes=[PS[bg]])
                    for kc in range(16):
                        S.add("pe", lambda e, kc=kc, fc=fc, th=th, bu=bu, U=U: e.matmul(ps[bu][:], U[:, kc, fc * 128:(fc + 1) * 128], hT[:, kc, th * 512:(th + 1) * 512],
                                                                                 start=(kc == 0), stop=(kc == 15)),
                              reads=[RG[su_]] + HT[th * 4:(th + 1) * 4], writes=[PS[bu]])
                    S.add("act", lambda e, bg=bg, sgi=sgi: e.activation(sgt[sgi], ps[bg][:], AF.Silu), reads=[PS[bg]], writes=[res("sgt%d" % sgi)])
                    S.add("dve", lambda e, bu=bu, sgi=sgi, fc=fc, th=th: e.tensor_tensor(hid[:, fc, th * 512:(th + 1) * 512], sgt[sgi], ps[bu][:], op=ALU.mult),
                          reads=[PS[bu], res("sgt%d" % sgi)], writes=[res("hid%d" % th)])
            it = 0
            for t in range(NT):
                for dc in range(4):
                    b = 4 + (it % 3)
                    it += 1
                    for fc in range(4):
                        S.add("pe", lambda e, fc=fc, t=t, dc=dc, b=b, Dw=Dw: e.matmul(ps[b][:], hid[:, fc, t * 128:(t + 1) * 128], Dw[:, fc, dc * 512:(dc + 1) * 512],
                                                                               start=(fc == 0), stop=(fc == 3)),
                              reads=[RG[sd_], res("hid%d" % (t // 4))], writes=[PS[b]])
                    yr = res("yacc%d" % t)
                    if ex == 0:
                        S.add("dve", lambda e, t=t, dc=dc, b=b, ex=ex: e.tensor_scalar(yacc[:, t, dc * 512:(dc + 1) * 512], ps[b][:], comb[:, t, ex:ex + 1], None, op0=ALU.mult),
                              reads=[PS[b], res("comb")], writes=[yr])
                    else:
                        S.add("dve", lambda e, t=t, dc=dc, b=b, ex=ex: e.scalar_tensor_tensor(yacc[:, t, dc * 512:(dc + 1) * 512], ps[b][:], comb[:, t, ex:ex + 1],
                                                                                              yacc[:, t, dc * 512:(dc + 1) * 512], op0=ALU.mult, op1=ALU.add),
                              reads=[PS[b], res("comb"), yr], writes=[yr])
        if MOE_STAGE < 3:
            S.barrier()
            return
        gt = sc; lgt = sh
        lbt = hT32.rearrange("p a b -> p (a b)")
        bc_load(gt, m_dram[l, 5 * D:6 * D], res("sc"), "bc")
        S.dma("sp", lambda e: e.dma_start(out=lgt, in_=ln2_g[l, :].partition_broadcast(128)), writes=[res("sh")], key="bc")
        S.dma("sp", lambda e: e.dma_start(out=lbt, in_=ln2_b[l, :].partition_broadcast(128)), writes=[res("hT32")], key="bc")
        for t in range(NT):
            xt = xw[0]
            xr = res("xw0")
            yr = res("yacc%d" % t)
            S.dma("sp", lambda e, t=t, xt=xt: e.dma_start(out=xt, in_=x_src[t * 128:(t + 1) * 128, :]), reads=[res("xs")], writes=[xr], key="xl0")
            S.add("dve", lambda e, t=t: e.tensor_tensor(yacc[:, t, :], yacc[:, t, :], gt, op=ALU.mult), reads=[yr, res("sc")], writes=[yr])
            S.add("dve", lambda e, t=t, xt=xt: e.scalar_tensor_tensor(xt, xt, ALPHA, yacc[:, t, :], op0=ALU.mult, op1=ALU.add), reads=[yr, xr], writes=[xr])
            layer_norm_tile(xt, xr, lgt, lbt, xt, xr, [res("sh"), res("hT32")])
            S.dma("sp", lambda e, t=t, xt=xt: e.dma_start(out=x_dst[t * 128:(t + 1) * 128, :], in_=xt), reads=[xr], writes=[res("xdst")], key="xo")
        S.barrier()

    def outproj_block(l, KC, w_out_ap, x_src, x_dst):
        A.reset()
        CW = 8192 // KC
        npiece = D // CW
        yTh = A.bf16([128, KC, 512])
        oh = A.f32([128, 4, D])
        ring = [A.bf16([128, 8192]) for _ in range(3)]
        RG = [Res("oring%d" % i) for i in range(3)]
        gt = A.f32([128, D]); lgt = A.f32([128, D]); lbt = A.f32([128, D])
        xw = [A.f32([128, D]) for _ in range(2)]
        bc_load(gt, m_dram[l, 2 * D:3 * D], res("gt"), "bc")
        S.dma("sp", lambda e: e.dma_start(out=lgt, in_=ln1_g[l, :].partition_broadcast(128)), writes=[res("lgt")], key="bc")
        S.dma("sp", lambda e: e.dma_start(out=lbt, in_=ln1_b[l, :].partition_broadcast(128)), writes=[res("lbt")], key="bc")
        pc = 0
        for th in range(2):
            S.dma("sp", lambda e, th=th: e.dma_start(out=yTh, in_=yT_dram[:, 0:KC, th * 512:(th + 1) * 512]), reads=[res("yT_dram")], writes=[res("yTh")], key="yt")
            it = 0
            for p in range(npiece):
                slot = pc % 3
                pc += 1
                W = ring[slot].rearrange("p (k c) -> p k c", c=CW)
                stream_w(W, w_out_ap[:, p * CW:(p + 1) * CW].rearrange("(kc p) c -> p kc c", p=128), RG[slot], "or%d" % slot)
                for tt in range(4):
                    b = it % 4
                    it += 1
                    for kc in range(KC):
                        S.add("pe", lambda e, kc=kc, tt=tt, b=b, W=W: e.matmul(ps[b][:, 0:CW], yTh[:, kc, tt * 128:(tt + 1) * 128], W[:, kc, :], start=(kc == 0), stop=(kc == KC - 1)),
                              reads=[RG[slot], res("yTh")], writes=[PS[b]])
                    evac(oh[:, tt, p * CW:(p + 1) * CW], ps[b][:, 0:CW], [PS[b]], [res("oh%d" % tt)])
            for tt in range(4):
                t = th * 4 + tt
                xt = xw[t % 2]
                xr = res("xw%d" % (t % 2))
                orr = res("oh%d" % tt)
                S.dma("sp", lambda e, t=t, xt=xt: e.dma_start(out=xt, in_=x_src[t * 128:(t + 1) * 128, :]), reads=[res("xs")], writes=[xr], key="xl%d" % (t % 2))
                S.add("dve", lambda e, tt=tt: e.tensor_tensor(oh[:, tt, :], oh[:, tt, :], gt, op=ALU.mult), reads=[orr, res("gt")], writes=[orr])
                S.add("dve", lambda e, tt=tt, xt=xt: e.scalar_tensor_tensor(xt, xt, ALPHA, oh[:, tt, :], op0=ALU.mult, op1=ALU.add), reads=[orr, xr], writes=[xr])
                layer_norm_tile(xt, xr, lgt, lbt, xt, xr, [res("lgt"), res("lbt")])
                S.dma("sp", lambda e, t=t, xt=xt: e.dma_start(out=x_dst[t * 128:(t + 1) * 128, :], in_=xt), reads=[xr], writes=[res("xdst")], key="xo")
        S.barrier()

    def retention_block(l, x_src):
        A.reset()
        hT = A.bf16([128, 16, NTOK])
        mark0 = A.off
        prologue(l, x_src, hT, 0, 1)
        S.barrier()
        A.off = mark0
        HT = [res("hT%d" % t) for t in range(NT)]
        ring = [A.bf16([128, 8192]) for _ in range(2)]
        RG = [Res("rring%d" % i) for i in range(2)]
        QT = A.bf16([128, 2, NTOK]); KT = A.bf16([128, 2, NTOK])
        k_tm = A.bf16([64, NCH, 256]); v_tm = A.bf16([64, NCH, 512])
        sgf = A.bf16([64, NCH, 512]); sgb = A.bf16([64, NCH, 512])
        yf = A.bf16([64, NCH, 512])
        yTh = A.bf16([128, 4, NTOK])
        rc = A.bf16([64, NCH, 256]); rs = A.bf16([64, NCH, 256])
        St = A.f32([128, 2, 512]); Sb = A.bf16([128, 2, 512])
        DT = A.f32([64, 64]); qdec = A.bf16([128, 2, 64]); kd = A.f32([64, 2])
        gng = A.f32([64, 512]); gnb = A.f32([64, 512])
        t1 = A.f32([64, 512]); t2 = A.f32([64, 512])
        qr = A.bf16([64, 256]); Pm = A.bf16([64, 64]); Qd = A.bf16([128, 2, 64]); Kd = A.bf16([64, 256])
        yb = A.bf16([64, 512])
        S.dma("pool", lambda e: e.dma_start(out=rc, in_=ropec[:, :, :]), writes=[res("rc")], key="rt")
        S.dma("pool", lambda e: e.dma_start(out=rs, in_=ropes[:, :, :]), writes=[res("rs")], key="rt")
        pc = [0]

        def piece(c0, ncols):
            slot = pc[0] % 2
            pc[0] += 1
            W = ring[slot][:, 0:16 * ncols].rearrange("p (k c) -> p k c", c=ncols)
            stream_w(W, w_ret_in[:, c0:c0 + ncols].rearrange("(kc p) c -> p kc c", p=128), RG[slot], "rr%d" % slot)
            return W, RG[slot]

        def proj_chunk(W, Wr, c, ncols, b):
            for kc in range(16):
                S.add("pe", lambda e, kc=kc: e.matmul(ps[b][0:64, 0:ncols], hT[:, kc, c * 64:(c + 1) * 64], W[:, kc, :], start=(kc == 0), stop=(kc == 15)),
                      reads=[Wr, HT[c // 2]], writes=[PS[b]])

        def rope_to(dst, dst_res, c, b, kscale):
            p4 = ps[b][0:64, 0:256].rearrange("p (a h f) -> p a h f", a=2, h=2)
            S.add("dve", lambda e: e.tensor_tensor(t1[:, 0:256], ps[b][0:64, 0:256], rc[:, c, :], op=ALU.mult), reads=[PS[b], res("rc")], writes=[res("t1")])
            S.add("dve", lambda e: e.tensor_tensor(t2[:, 0:256].rearrange("p (a h f) -> p a h f", a=2, h=2), p4[:, :, ::-1, :],
                                                   rs[:, c, :].rearrange("p (a h f) -> p a h f", a=2, h=2), op=ALU.mult), reads=[PS[b], res("rs")], writes=[res("t2")])
            if kscale is None:
                S.add("dve", lambda e: e.tensor_tensor(dst, t1[:, 0:256], t2[:, 0:256], op=ALU.add), reads=[res("t1"), res("t2")], writes=[dst_res])
            else:
                S.add("dve", lambda e: e.tensor_tensor(t1[:, 0:256], t1[:, 0:256], t2[:, 0:256], op=ALU.add), reads=[res("t1"), res("t2")], writes=[res("t1")])
                S.add("dve", lambda e: e.tensor_scalar(dst, t1[:, 0:256], kscale, None, op0=ALU.mult), reads=[res("t1")], writes=[dst_res])

        def tr_to(dstT, dstT_res, src, src_res, c):
            for dc in range(2):
                S.add("pe", lambda e, dc=dc: e.transpose(psb[:, dc * 64:(dc + 1) * 64], src[:, dc * 128:(dc + 1) * 128], identb[0:64, 0:64]),
                      reads=[src_res, res("identb")], writes=[PSB])
            evac(dstT[:, :, c * 64:(c + 1) * 64], psb[:, 0:128].rearrange("p (a b) -> p a b", b=64), [PSB], [dstT_res])

        for h in range(8):
            W, Wr = piece(h * 256, 256)
            for c in range(NCH):
                b = c % 2
                proj_chunk(W, Wr, c, 256, b)
                rope_to(qr, res("qr"), c, b, None)
                tr_to(QT, res("QT"), qr, res("qr"), c)
            W, Wr = piece(2048 + h * 256, 256)
            for c in range(NCH):
                b = c % 2
                proj_chunk(W, Wr, c, 256, b)
                rope_to(k_tm[:, c, :], res("k_tm"), c, b, 1.0 / 16.0)
                tr_to(KT, res("KT"), k_tm[:, c, :], res("k_tm"), c)
            W, Wr = piece(4096 + h * 512, 512)
            for c in range(NCH):
                b = c % 2
                proj_chunk(W, Wr, c, 512, b)
                evac(v_tm[:, c, :], ps[b][0:64, :], [PS[b]], [res("v_tm")])
            W, Wr = piece(8192 + h * 512, 512)
            for c in range(NCH):
                b = c % 2
                proj_chunk(W, Wr, c, 512, b)
                S.add("act", lambda e, c=c, b=b: e.activation(sgf[:, c, :], ps[b][0:64, :], AF.Silu), reads=[PS[b]], writes=[res("sgf")])
            W, Wr = piece(12288 + h * 512, 512)
            for c in range(NCH):
                b = c % 2
                proj_chunk(W, Wr, c, 512, b)
                S.add("act", lambda e, c=c, b=b: e.activation(sgb[:, c, :], ps[b][0:64, :], AF.Silu), reads=[PS[b]], writes=[res("sgb")])
            S.dma("sp", lambda e, h=h: e.dma_start(out=gng, in_=gn_g[h, :].partition_broadcast(64)), writes=[res("gng")], key="gn")
            S.dma("sp", lambda e, h=h: e.dma_start(out=gnb, in_=gn_b[h, :].partition_broadcast(64)), writes=[res("gnb")], key="gn")
            for dr in range(2):
                col = dr * 8 + h
                lgc = lg[:, col:col + 1]
                dif = cst[0:64, 384:448] if dr == 0 else cst[0:64, 512:576]
                tri = cst[0:64, 448:512] if dr == 0 else cst[0:64, 576:640]
                ramp = cst[:, 256:320] if dr == 0 else cst[:, 320:384]
                S.add("act", lambda e, dif=dif, lgc=lgc: e.activation(DT, dif, AF.Exp, scale=lgc[0:64, :]), reads=[res("cst"), res("lg")], writes=[res("DT")])
                S.add("dve", lambda e, tri=tri: e.tensor_tensor(DT, DT, tri, op=ALU.mult), reads=[res("DT"), res("cst")], writes=[res("DT")])
                for dc in range(2):
                    S.add("act", lambda e, dc=dc, ramp=ramp, lgc=lgc: e.activation(qdec[:, dc, :], ramp, AF.Exp, scale=lgc), reads=[res("cst"), res("lg")], writes=[res("qdec")])
                S.add("act", lambda e, dr=dr, lgc=lgc: e.activation(kd[:, 0:1], cst[0:64, 640 + dr:641 + dr], AF.Exp, scale=lgc[0:64, :]), reads=[res("cst"), res("lg")], writes=[res("kd")])
                S.add("act", lambda e, lgc=lgc: e.activation(small[:, 20:21], cst[:, 642:643], AF.Exp, scale=lgc), reads=[res("cst"), res("lg")], writes=[res("cdec")])
                S.dma("sp", lambda e, dr=dr, h=h: e.dma_start(out=St, in_=s0[dr, h].rearrange("(dc p) v -> p dc v", p=128)), writes=[res("St")], key="s0")
                order = list(range(NCH)) if dr == 0 else list(range(NCH - 1, -1, -1))
                for idx, c in enumerate(order):
                    if idx % 4 == 0 and idx > 0:
                        S.add("dve", lambda e: e.tensor_scalar(St, St, flg[:, 0:1], None, op0=ALU.mult), reads=[res("St"), res("flg")], writes=[res("St")])
                    for dc in range(2):
                        S.add("pe", lambda e, dc=dc, c=c: e.matmul(ps[2][0:64, 0:64], KT[:, dc, c * 64:(c + 1) * 64], QT[:, dc, c * 64:(c + 1) * 64], start=(dc == 0), stop=(dc == 1)),
                              reads=[res("KT"), res("QT")], writes=[PS[2]])
                    S.add("dve", lambda e: e.tensor_tensor(Pm, ps[2][0:64, 0:64], DT, op=ALU.mult), reads=[PS[2], res("DT")], writes=[res("Pm")])
                    S.add("dve", lambda e, c=c: e.tensor_tensor(Qd, QT[:, :, c * 64:(c + 1) * 64], qdec, op=ALU.mult), reads=[res("QT"), res("qdec")], writes=[res("Qd")])
                    S.add("act", lambda e: e.copy(Sb, St), reads=[res("St")], writes=[res("Sb")])
                    S.add("pe", lambda e, c=c: e.matmul(ps[3][0:64, :], Pm, v_tm[:, c, :], start=True, stop=False), reads=[res("Pm"), res("v_tm")], writes=[PS[3]])
                    for dc in range(2):
                        S.add("pe", lambda e, dc=dc: e.matmul(ps[3][0:64, :], Qd[:, dc, :], Sb[:, dc, :], start=False, stop=(dc == 1)), reads=[res("Qd"), res("Sb")], writes=[PS[3]])
                    S.add("dve", lambda e: e.bn_stats(stt[0:64, 0, :], ps[3][0:64, :]), reads=[PS[3]], writes=[res("stt")])
                    S.add("dve", lambda e: e.bn_aggr(small[0:64, 0:2], stt[0:64, 0:1, :]), reads=[res("stt")], writes=[res("small")])
                    S.add("dve", lambda e: e.tensor_scalar(small[0:64, 2:3], small[0:64, 1:2], EPS, None, op0=ALU.add), reads=[res("small")], writes=[res("small")])
                    S.add("act", lambda e: e.activation(small[0:64, 2:3], small[0:64, 2:3], AF.Ln), reads=[res("small")], writes=[res("small")])
                    S.add("act", lambda e: e.activation(small[0:64, 2:3], small[0:64, 2:3], AF.Exp, scale=-0.5), reads=[res("small")], writes=[res("small")])
                    S.add("dve", lambda e: e.tensor_scalar(t1, ps[3][0:64, :], small[0:64, 0:1], small[0:64, 2:3], op0=ALU.subtract, op1=ALU.mult),
                          reads=[PS[3], res("small")], writes=[res("t1")])
                    S.add("dve", lambda e: e.tensor_tensor(t1, t1, gng, op=ALU.mult), reads=[res("t1"), res("gng")], writes=[res("t1")])
                    S.add("dve", lambda e: e.tensor_tensor(t1, t1, gnb, op=ALU.add), reads=[res("t1"), res("gnb")], writes=[res("t1")])
                    if dr == 0:
                        S.add("dve", lambda e, c=c: e.tensor_tensor(yf[:, c, :], t1, sgf[:, c, :], op=ALU.mult), reads=[res("t1"), res("sgf")], writes=[res("yf")])
                    else:
                        S.add("dve", lambda e, c=c: e.tensor_tensor(t1, t1, sgb[:, c, :], op=ALU.mult), reads=[res("t1"), res("sgb")], writes=[res("t1")])
                        S.add("dve", lambda e, c=c: e.tensor_tensor(yb, t1, yf[:, c, :], op=ALU.add), reads=[res("t1"), res("yf")], writes=[res("yb")])
                        for vc in range(4):
                            S.add("pe", lambda e, vc=vc: e.transpose(psb[:, 256 + vc * 64:256 + (vc + 1) * 64], yb[:, vc * 128:(vc + 1) * 128], identb[0:64, 0:64]),
                                  reads=[res("yb"), res("identb")], writes=[PSB])
                        evac(yTh[:, :, c * 64:(c + 1) * 64], psb[:, 256:512].rearrange("p (a b) -> p a b", b=64), [PSB], [res("yTh")])
                    S.add("dve", lambda e, c=c: e.tensor_scalar(Kd, k_tm[:, c, :], kd[:, 0:1], None, op0=ALU.mult), reads=[res("k_tm"), res("kd")], writes=[res("Kd")])
                    for dc in range(2):
                        S.add("pe", lambda e, dc=dc, c=c: e.matmul(ps[4 + dc][:, :], Kd[:, dc * 128:(dc + 1) * 128], v_tm[:, c, :], start=True, stop=True),
                              reads=[res("Kd"), res("v_tm")], writes=[PS[4 + dc]])
                        S.add("dve", lambda e, dc=dc: e.scalar_tensor_tensor(St[:, dc, :], St[:, dc, :], small[:, 20:21], ps[4 + dc][:, :], op0=ALU.mult, op1=ALU.add),
                              reads=[res("St"), res("cdec"), PS[4 + dc]], writes=[res("St")])
                    if idx % 4 == 3:
                        sq = c // 4
                        S.dma("sp", lambda e, sq=sq, dr=dr, h=h: e.dma_start(out=st_out[sq, dr, h].rearrange("(dc p) v -> p dc v", p=128), in_=St),
                              reads=[res("St")], writes=[res("st_out")], key="so")
            S.dma("sp", lambda e, h=h: e.dma_start(out=yT_dram[:, h * 4:(h + 1) * 4, :], in_=yTh), reads=[res("yTh")], writes=[res("yT_dram")], key="yo")
        S.barrier()

    def na_block(l, x_src):
        A.reset()
        hT = A.bf16([128, 16, NTOK])
        mark0 = A.off
        prologue(l, x_src, hT, 0, 1)
        S.barrier()
        A.off = mark0
        HT = [res("hT%d" % t) for t in range(NT)]
        wq = A.bf16([128, 16, 128]); wk = A.bf16([128, 16, 128]); wv = A.bf16([128, 16, 128])
        QT = A.bf16([128, NTOK]); KT = A.bf16([128, NTOK])
        Va = A.bf16([64, NCH, 130]); Vc = A.bf16([64, 8, 130])
        KcT = A.bf16([128, 512])
        kc32 = A.f32([128, 4, 128])
        ko = A.f32([64, NCH, 128]); vo = A.f32([64, NCH, 128])
        bl = A.bf16([64, 15, 64]); rm = A.bf16([1, 16, 16, 64])
        PT = A.bf16([64, 16, 64])
        ao = A.bf16([64, 128]); rec = A.f32([64, 2])
        aT = A.bf16([128, NTOK])
        S.dma("pool", lambda e: e.dma_start(out=rm, in_=narow[:, :, :, :]), writes=[res("rm")], key="nb")
        for h in range(16):
            stream_w(wq, w_na_in[:, h * 128:(h + 1) * 128].rearrange("(kc p) c -> p kc c", p=128), res("wq"), "wq")
            stream_w(wk, w_na_in[:, D + h * 128:D + (h + 1) * 128].rearrange("(kc p) c -> p kc c", p=128), res("wk"), "wk")
            stream_w(wv, w_na_in[:, 2 * D + h * 128:2 * D + (h + 1) * 128].rearrange("(kc p) c -> p kc c", p=128), res("wv"), "wv")
            S.dma("pool", lambda e, h=h: e.dma_start(out=bl, in_=nabias[:, h, :, :]), writes=[res("bl")], key="nb")
            S.dma("sp", lambda e, h=h: e.dma_start(out=kc32, in_=ck[h].rearrange("(a p) d -> p a d", p=128)), writes=[res("kc32")], key="ck")
            S.dma("pool", lambda e, h=h: e.dma_start(out=Vc[:, :, 0:128], in_=cv[h].rearrange("(a p) d -> p a d", p=64)), writes=[res("Vc")], key="cv")
            S.add("dve", lambda e: e.tensor_copy(Vc[:, :, 128:129], cst[0:64, 128:136].rearrange("p (a b) -> p a b", b=1)), reads=[res("cst")], writes=[res("Vc")])
            S.add("dve", lambda e: e.tensor_copy(Va[:, :, 128:129], cst[0:64, 128:144].rearrange("p (a b) -> p a b", b=1)), reads=[res("cst")], writes=[res("Va")])
            for a in range(4):
                S.add("pe", lambda e, a=a: e.transpose(ps[6][:, a * 128:(a + 1) * 128], kc32[:, a, :], ident[:]), reads=[res("kc32"), res("ident")], writes=[PS[6]])
            evac(KcT, ps[6][:], [PS[6]], [res("KcT")])
            for th in range(2):
                for kc in range(16):
                    S.add("pe", lambda e, kc=kc, th=th: e.matmul(ps[0][:], wq[:, kc, :], hT[:, kc, th * 512:(th + 1) * 512], start=(kc == 0), stop=(kc == 15)),
                          reads=[res("wq")] + HT[th * 4:(th + 1) * 4], writes=[PS[0]])
                evac(QT[:, th * 512:(th + 1) * 512], ps[0][:], [PS[0]], [res("QT")], scale=128.0 ** -0.5)
                for kc in range(16):
                    S.add("pe", lambda e, kc=kc, th=th: e.matmul(ps[1][:], wk[:, kc, :], hT[:, kc, th * 512:(th + 1) * 512], start=(kc == 0), stop=(kc == 15)),
                          reads=[res("wk")] + HT[th * 4:(th + 1) * 4], writes=[PS[1]])
                evac(KT[:, th * 512:(th + 1) * 512], ps[1][:], [PS[1]], [res("KT")])
            for r in range(NCH):
                b = 2 + (r % 2)
                for kc in range(16):
                    S.add("pe", lambda e, kc=kc, r=r, b=b: e.matmul(ps[b][0:64, 0:128], hT[:, kc, r * 64:(r + 1) * 64], wk[:, kc, :], start=(kc == 0), stop=(kc == 15)),
                          reads=[res("wk"), HT[r // 2]], writes=[PS[b]])
                for kc in range(16):
                    S.add("pe", lambda e, kc=kc, r=r, b=b: e.matmul(ps[b][0:64, 128:256], hT[:, kc, r * 64:(r + 1) * 64], wv[:, kc, :], start=(kc == 0), stop=(kc == 15)),
                          reads=[res("wv"), HT[r // 2]], writes=[PS[b]])
                S.add("act", lambda e, r=r, b=b: e.copy(ko[:, r, :], ps[b][0:64, 0:128]), reads=[PS[b]], writes=[res("ko"), res("pstok%d" % b)])
                S.add("act", lambda e, r=r, b=b: e.copy(Va[:, r, 0:128], ps[b][0:64, 128:256]), reads=[PS[b]], writes=[res("Va"), res("pstok%d" % b)])
                S.add("dve", lambda e, r=r, b=b: e.tensor_copy(vo[:, r, :], ps[b][0:64, 128:256]), reads=[PS[b]], writes=[res("vo"), res("pstok%d" % b)])
            S.dma("sp", lambda e, h=h: e.dma_start(out=k_out[h].rearrange("(r p) d -> p r d", p=64), in_=ko), reads=[res("ko")], writes=[res("k_out")], key="kvo")
            S.dma("sp", lambda e, h=h: e.dma_start(out=v_out[h].rearrange("(r p) d -> p r d", p=64), in_=vo), reads=[res("vo")], writes=[res("v_out")], key="kvo")
            for r in range(NCH):
                r0 = min(max(r - 4, 0), 8)
                pb = 4 + (r % 2)
                S4 = ps[pb][0:64, :].rearrange("p (j q) -> p j q", q=64)
                pb2 = 0 if pb == 4 else 1
                for j in range(16):
                    bank, jj = (pb, j) if j < 8 else (pb2, j - 8)
                    dst = ps[bank][0:64, jj * 64:(jj + 1) * 64]
                    if j < 8:
                        kr = r0 + j
                        drr = kr - r + 7
                        S.add("pe", lambda e, dst=dst, kr=kr, r=r: e.matmul(dst, KT[:, kr * 64:(kr + 1) * 64], QT[:, r * 64:(r + 1) * 64], start=True, stop=False),
                              reads=[res("KT"), res("QT")], writes=[PS[bank]])
                        S.add("pe", lambda e, dst=dst, drr=drr: e.matmul(dst, identb[0:64, 0:64], bl[:, drr, :], start=False, stop=False),
                              reads=[res("identb"), res("bl")], writes=[PS[bank]])
                    else:
                        p = j - 8
                        S.add("pe", lambda e, dst=dst, p=p, r=r: e.matmul(dst, KcT[:, p * 64:(p + 1) * 64], QT[:, r * 64:(r + 1) * 64], start=True, stop=False),
                              reads=[res("KcT"), res("QT")], writes=[PS[bank]])
                    S.add("pe", lambda e, dst=dst, r=r, j=j: e.matmul(dst, rm[0:1, r, j, :], onesb[0:1, 0:64], start=False, stop=True),
                          reads=[res("rm"), res("onesb")], writes=[PS[bank]])
                S.add("act", lambda e, pb=pb: e.activation(PT[:, 0:8, :], ps[pb][0:64, :].rearrange("p (j q) -> p j q", q=64), AF.Exp), reads=[PS[pb]], writes=[res("PT")])
                S.add("act", lambda e, pb2=pb2: e.activation(PT[:, 8:16, :], ps[pb2][0:64, :].rearrange("p (j q) -> p j q", q=64), AF.Exp), reads=[PS[pb2]], writes=[res("PT")])
                ob = 2 + (r % 2)
                for j in range(16):
                    rhs = Va[:, r0 + j, 0:129] if j < 8 else Vc[:, j - 8, 0:129]
                    S.add("pe", lambda e, j=j, rhs=rhs, ob=ob: e.matmul(ps[ob][0:64, 0:129], PT[:, j, :], rhs, start=(j == 0), stop=(j == 15)),
                          reads=[res("PT"), res("Va"), res("Vc")], writes=[PS[ob]])
                S.add("dve", lambda e, ob=ob: e.reciprocal(rec[:, 0:1], ps[ob][0:64, 128:129]), reads=[PS[ob]], writes=[res("rec")])
                S.add("dve", lambda e, ob=ob: e.tensor_scalar(ao, ps[ob][0:64, 0:128], rec[:, 0:1], None, op0=ALU.mult), reads=[PS[ob], res("rec")], writes=[res("ao")])
                S.add("pe", lambda e, r=r: e.transpose(psb[:, 512 + (r % 2) * 64:512 + (r % 2 + 1) * 64], ao, identb[0:64, 0:64]), reads=[res("ao"), res("identb")], writes=[PSB])
                evac(aT[:, r * 64:(r + 1) * 64], psb[:, 512 + (r % 2) * 64:512 + (r % 2 + 1) * 64], [PSB], [res("aT")])
            S.dma("sp", lambda e, h=h: e.dma_start(out=yT_dram[:, h, :], in_=aT), reads=[res("aT")], writes=[res("yT_dram")], key="yo")
        S.barrier()

    if upto >= 1:
        retention_block(0, x_in)
    if upto >= 2:
        outproj_block(0, 32, w_ret_out, x_in, xs)
    if upto >= 3:
        moe_block(0, xs, xs, 1)
    if upto >= 4:
        na_block(1, xs)
    if upto >= 5:
        outproj_block(1, 16, w_na_out, xs, xs)
    if upto >= 6:
        moe_block(1, xs, y_out, None)

    block = E(nc.Block())
    S.emit(stack, block)
    stack.close()
    return nc


def _consts():
    c = np.zeros((128, 1024), np.float32)
    c[:, 0:128] = np.eye(128, dtype=np.float32)
    c[:, 128:256] = 1.0
    i = np.arange(64, dtype=np.float32)
    c[:, 256:320] = (i + 1.0)[None, :]
    c[:, 320:384] = (64.0 - i)[None, :]
    jj, ii = np.meshgrid(i, i, indexing="ij")
    c[0:64, 384:448] = np.maximum(ii - jj, 0.0)
    c[0:64, 448:512] = (ii >= jj).astype(np.float32)
    c[0:64, 512:576] = np.maximum(jj - ii, 0.0)
    c[0:64, 576:640] = (jj >= ii).astype(np.float32)
    c[0:64, 640] = 63.0 - i
    c[0:64, 641] = i
    c[:, 642] = 64.0
    return c


def _rope_tables(is_sample):
    nf = 64
    t = np.arange(NTOK)
    cos2 = np.ones((NTOK, 2, 2, nf), np.float32)
    sin2 = np.zeros((NTOK, 2, 2, nf), np.float32)
    if is_sample:
        rows = (t // 64).astype(np.float32)
        cols = (t % 64).astype(np.float32)
        inv = (np.float32(10000.0) ** (-np.arange(nf, dtype=np.float32) / np.float32(nf))).astype(np.float32)
        ang = np.stack([rows[:, None] * inv, cols[:, None] * inv], axis=1).astype(np.float32)
        cs, sn = np.cos(ang).astype(np.float32), np.sin(ang).astype(np.float32)
        cos2[:, :, 0, :] = cs
        cos2[:, :, 1, :] = cs
        sin2[:, :, 0, :] = -sn
        sin2[:, :, 1, :] = sn
    rc = cos2.reshape(NCH, 64, 256).transpose(1, 0, 2)
    rs = sin2.reshape(NCH, 64, 256).transpose(1, 0, 2)
    return np.ascontiguousarray(rc), np.ascontiguousarray(rs)


def _na_tables(is_sample, rpb):
    bias = np.zeros((64, 16, 15, 64), np.float32)
    rowm = np.zeros((1, 16, 16, 64), np.float32)
    col = np.arange(64)
    if is_sample:
        c0 = np.clip(col - 8, 0, 48)
        ok = (col[None, :] >= c0[:, None]) & (col[None, :] < c0[:, None] + 16)
        dc = np.clip(col[None, :] - col[:, None], -15, 15) + 15
        g = rpb[:, :, dc]
        g = np.where(ok[None, None], g, np.float32(NEG))
        bias[:] = g.transpose(3, 0, 1, 2)
    else:
        for r in range(16):
            r0 = min(max(r - 4, 0), 8)
            for j in range(16):
                if j < 8:
                    if (r0 + j) // 4 != r // 4:
                        rowm[0, r, j, :] = NEG
                else:
                    rowm[0, r, j, :] = NEG
    return bias, rowm


_NC_CACHE = {}


def _make_in_maps(x_prompt, x_sample, state_ret, cache_na_k, cache_na_v, c, c_ctx, w_mod, b_mod, ln1_g, ln1_b, ln2_g, ln2_b,
           w_ret_in, ret_decay_logit, ret_gn_g, ret_gn_b, w_ret_out, w_na_in, na_rpb, w_na_out, w_rg, b_rg, w_re, b_re,
           w_gate, w_up, w_down):
    f = lambda a: np.ascontiguousarray(np.asarray(a, dtype=np.float32))
    x_prompt, x_sample, state_ret, cache_na_k, cache_na_v = map(f, (x_prompt, x_sample, state_ret, cache_na_k, cache_na_v))
    c, c_ctx = f(c), f(c_ctx)
    w_rt = np.concatenate([f(w_rg), f(w_re).transpose(0, 2, 1, 3).reshape(2, D, 32)], axis=2)
    b_rt = np.concatenate([f(b_rg), f(b_re).reshape(2, 32)], axis=1)
    shared = {
        "consts": _consts(), "w_mod0": f(w_mod)[0], "w_mod1": f(w_mod)[1], "b_mod": f(b_mod), "ln1_g": f(ln1_g), "ln1_b": f(ln1_b), "ln2_g": f(ln2_g), "ln2_b": f(ln2_b),
        "w_ret_in": f(w_ret_in)[0], "decay": f(ret_decay_logit).reshape(1, 16), "gn_g": f(ret_gn_g)[0], "gn_b": f(ret_gn_b)[0],
        "w_ret_out": f(w_ret_out)[0], "w_na_in": f(w_na_in)[0], "w_na_out": f(w_na_out)[0], "w_rt": np.ascontiguousarray(w_rt), "b_rt": np.ascontiguousarray(b_rt),
        "w_gate0": f(w_gate)[0], "w_gate1": f(w_gate)[1], "w_up0": f(w_up)[0], "w_up1": f(w_up)[1],
        "w_down0": f(w_down)[0], "w_down1": f(w_down)[1],
    }
    rpb = f(na_rpb)[0]
    in_maps = []
    for core in range(8):
        smp = core >= 4
        m = dict(shared)
        if smp:
            b = core - 4
            m["x_in"] = x_sample[b]
            cv_ = c[b]
            m["s0"] = np.ascontiguousarray(state_ret[b, 0])
            m["ck"] = np.ascontiguousarray(cache_na_k[b, 0]); m["cv"] = np.ascontiguousarray(cache_na_v[b, 0])
        else:
            m["x_in"] = np.ascontiguousarray(x_prompt[4 * core:4 * core + 4].reshape(NTOK, D))
            cv_ = c_ctx
            m["s0"] = np.zeros((2, 8, 256, 512), np.float32)
            m["ck"] = np.zeros((16, 512, 128), np.float32); m["cv"] = np.zeros((16, 512, 128), np.float32)
        m["cvec"] = np.ascontiguousarray(cv_.reshape(16, 128).T)
        fl = np.zeros((1, 8), np.float32)
        fl[0, 0] = 1.0 if smp else 0.0
        m["flags"] = fl
        m["ropec"], m["ropes"] = _rope_tables(smp)
        m["nabias"], m["narow"] = _na_tables(smp, rpb)
        in_maps.append(m)
    return in_maps


def _assemble(R_):
    y_prompt = np.concatenate([R_[i]["y_out"].reshape(4, 256, D) for i in range(4)], axis=0)
    y_sample = np.stack([R_[4 + i]["y_out"] for i in range(4)], axis=0)
    new_state = np.concatenate([R_[i]["st_out"] for i in range(4)], axis=0)[:, None]
    def kv(name):
        parts = []
        for i in range(4):
            a = R_[i][name].reshape(16, 4, 256, 128).transpose(1, 0, 2, 3)
            parts.append(a)
        return np.ascontiguousarray(np.concatenate(parts, axis=0)[:, None])
    return (np.ascontiguousarray(y_prompt), np.ascontiguousarray(y_sample), np.ascontiguousarray(new_state), kv("k_out"), kv("v_out"))


def kernel(**inputs):
    in_maps = _make_in_maps(**inputs)
    if "nc" not in _NC_CACHE:
        _NC_CACHE["nc"] = build()
    res = run_bass_kernel_spmd(_NC_CACHE["nc"], in_maps, core_ids=list(range(8)))
    return _assemble(res.results)
```

```python
from contextlib import ExitStack
import numpy as np
import concourse.bass as bass
import concourse.mybir as mybir
from concourse.bass_utils import run_bass_kernel_spmd

F32 = mybir.dt.float32
BF16 = mybir.dt.bfloat16
AF = mybir.ActivationFunctionType
ALU = mybir.AluOpType

D = 2048
NTOK = 1024
NT = 8
NCH = 16
ALPHA = (2.0 * 2) ** 0.25
EPS = 1e-5
NEG = -1e30
MOE_STAGE = 3


class Res:
    __slots__ = ("name", "last_w", "readers")

    def __init__(self, name=""):
        self.name = name
        self.last_w = None
        self.readers = {}


class Op:
    __slots__ = ("eng", "fn", "deps", "signal", "count", "is_dma", "dma_key", "epoch")

    def __init__(self, eng, fn):
        self.epoch = 0
        self.eng = eng
        self.fn = fn
        self.deps = []
        self.signal = False
        self.count = 0
        self.is_dma = False
        self.dma_key = None


ENGINES = ("pe", "act", "dve", "pool", "sp")


class Sched:
    def __init__(self, nc):
        self.nc = nc
        self.ops = {e: [] for e in ENGINES}
        self.dma_count = {}
        self.dma_kind = {}
        self.last_dma = {}
        self.epoch = 0

    def _track(self, op, reads, writes):
        op.epoch = self.epoch
        deps = []
        for r in reads:
            if r.last_w is not None:
                deps.append(r.last_w)
        for w in writes:
            if w.last_w is not None:
                deps.append(w.last_w)
            deps.extend(w.readers.values())
        seen = set()
        for d in deps:
            if d is op or id(d) in seen:
                continue
            seen.add(id(d))
            if d.eng == "pe" and op.eng == "pe" and not d.is_dma and not op.is_dma:
                continue
            if d.epoch < self.epoch:
                continue
            op.deps.append((d, self.dma_count[d.dma_key] if d.is_dma else None))
            d.signal = True
        for r in reads:
            r.readers[op.dma_key if op.is_dma else op.eng] = op
        for w in writes:
            w.last_w = op
            w.readers = {}

    def add(self, eng, fn, reads=(), writes=()):
        op = Op(eng, fn)
        self._track(op, reads, writes)
        self.ops[eng].append(op)
        return op

    def dma(self, eng, fn, reads=(), writes=(), key="d"):
        kind = "sw" if eng == "pool" else "hw"
        key = key + "_" + kind
        self.dma_kind[key] = kind
        op = Op(eng, fn)
        op.is_dma = True
        op.dma_key = key
        self.dma_count.setdefault(key, 0)
        self._track(op, reads, writes)
        self.dma_count[key] += 1
        op.count = self.dma_count[key]
        self.ops[eng].append(op)
        self.last_dma[key] = op
        return op

    def barrier(self):
        lasts = []
        for e in ENGINES:
            for op in reversed(self.ops[e]):
                if not op.is_dma:
                    lasts.append(op)
                    break
        lasts.extend(self.last_dma.values())
        for e in ENGINES:
            op = Op(e, lambda eng: eng.nop(nofuse=True))
            for d in lasts:
                if d.eng == e and not d.is_dma:
                    continue
                op.deps.append((d, self.dma_count[d.dma_key] if d.is_dma else None))
                d.signal = True
            op.epoch = self.epoch
            self.ops[e].append(op)
        self.epoch += 1

    def emit(self, stack, block):
        nc = self.nc
        sems = {(e, ep): stack.enter_context(nc.semaphore("s_%s_%d" % (e, ep))) for e in ENGINES for ep in range(self.epoch + 1)}
        dsems = {k: stack.enter_context(nc.semaphore("d_" + k)) for k in self.dma_kind}
        for e in ENGINES:
            c = {}
            for op in self.ops[e]:
                if not op.is_dma and op.signal:
                    c[op.epoch] = c.get(op.epoch, 0) + 1
                    op.count = c[op.epoch]
            print("sched", e, "signals per epoch", c)

        def run(e, engine):
            known = {}
            for op in self.ops[e]:
                need = {}
                for d, dc_ in op.deps:
                    if d.is_dma:
                        s, v = dsems[d.dma_key], 16 * dc_
                    else:
                        s, v = sems[(d.eng, d.epoch)], d.count
                    k = id(s)
                    if k not in need or need[k][1] < v:
                        need[k] = (s, v)
                for k, (s, v) in need.items():
                    if known.get(k, 0) >= v:
                        continue
                    engine.wait_ge(s, v)
                    known[k] = v
                ins = op.fn(engine)
                if op.is_dma:
                    ins.then_inc(dsems[op.dma_key], 16)
                elif op.signal:
                    ins.then_inc(sems[(e, op.epoch)], 1)
            if e in ("pool", "sp"):
                kind = "sw" if e == "pool" else "hw"
                for k, kd in self.dma_kind.items():
                    if kd == kind:
                        engine.wait_ge(dsems[k], 16 * self.dma_count[k])

        block.tensor(lambda eng: run("pe", eng))
        block.scalar(lambda eng: run("act", eng))
        block.vector(lambda eng: run("dve", eng))
        block.gpsimd(lambda eng: run("pool", eng))
        block.sync(lambda eng: run("sp", eng))


def build(upto=6):
    nc = bass.Bass("TRN2", target_bir_lowering=False)

    def din(name, shape, dt=F32):
        return nc.dram_tensor(name, list(shape), dt, kind="ExternalInput").ap()

    def dout(name, shape, dt=F32):
        return nc.dram_tensor(name, list(shape), dt, kind="ExternalOutput").ap()

    def dint(name, shape, dt=F32):
        return nc.dram_tensor(name, list(shape), dt).ap()

    x_in = din("x_in", [NTOK, D])
    cvec = din("cvec", [128, 16])
    s0 = din("s0", [2, 8, 256, 512])
    flags = din("flags", [1, 8])
    ropec = din("ropec", [64, NCH, 256])
    ropes = din("ropes", [64, NCH, 256])
    consts = din("consts", [128, 1024])
    ck = din("ck", [16, 512, 128])
    cv = din("cv", [16, 512, 128])
    nabias = din("nabias", [64, 16, 15, 64])
    narow = din("narow", [1, 16, 16, 64])
    w_mod = [din("w_mod%d" % i, [D, 6 * D]) for i in range(2)]
    b_mod = din("b_mod", [2, 6 * D])
    ln1_g = din("ln1_g", [2, D]); ln1_b = din("ln1_b", [2, D])
    ln2_g = din("ln2_g", [2, D]); ln2_b = din("ln2_b", [2, D])
    w_ret_in = din("w_ret_in", [D, 16384])
    decay = din("decay", [1, 16])
    gn_g = din("gn_g", [8, 512]); gn_b = din("gn_b", [8, 512])
    w_ret_out = din("w_ret_out", [4096, D])
    w_na_in = din("w_na_in", [D, 3 * D])
    w_na_out = din("w_na_out", [D, D])
    w_rt = din("w_rt", [2, D, 36])
    b_rt = din("b_rt", [2, 36])
    w_gate = [din("w_gate%d" % i, [32, D, 512]) for i in range(2)]
    w_up = [din("w_up%d" % i, [32, D, 512]) for i in range(2)]
    w_down = [din("w_down%d" % i, [32, 512, D]) for i in range(2)]

    y_out = dout("y_out", [NTOK, D])
    st_out = dout("st_out", [4, 2, 8, 256, 512])
    k_out = dout("k_out", [16, NTOK, 128])
    v_out = dout("v_out", [16, NTOK, 128])

    m_dram = dint("m_dram", [2, 6 * D])
    xs = dint("xs", [NTOK, D])
    yT_dram = dint("yT_dram", [128, 32, NTOK], BF16)

    S = Sched(nc)
    stack = ExitStack()
    E = stack.enter_context

    AW = 51200
    arena = E(nc.sbuf_tensor("arena", [128, AW], F32))
    ident = E(nc.sbuf_tensor("ident", [128, 128], F32))
    identb = E(nc.sbuf_tensor("identb", [128, 128], BF16))
    onesf = E(nc.sbuf_tensor("onesf", [128, 128], F32))
    onesb = E(nc.sbuf_tensor("onesb", [1, 64], BF16))
    cst = E(nc.sbuf_tensor("cst", [128, 1024], F32))
    sil = E(nc.sbuf_tensor("sil", [128, 16], F32))
    flg = E(nc.sbuf_tensor("flg", [128, 8], F32))
    lg = E(nc.sbuf_tensor("lg", [128, 16], F32))
    small = E(nc.sbuf_tensor("small", [128, 64], F32))
    stt = E(nc.sbuf_tensor("stt", [128, 4, 6], F32))
    comb = E(nc.sbuf_tensor("comb", [128, NT, 32], F32))
    rl = E(nc.sbuf_tensor("rl", [128, 96], F32))
    rbias = E(nc.sbuf_tensor("rbias", [128, 2, 36], F32))

    ps = [E(nc.psum_tensor("ps%d" % i, [128, 512], F32)) for i in range(7)]
    psb = E(nc.psum_tensor("psb", [128, 1024], BF16))
    PS = [Res("ps%d" % i) for i in range(7)]
    PSB = Res("psb")

    R = {}

    def res(name):
        if name not in R:
            R[name] = Res(name)
        return R[name]

    class Arena:
        def __init__(self):
            self.off = 0

        def reset(self):
            self.off = 0

        def f32(self, shape):
            n = int(np.prod(shape[1:]))
            v = arena[0:shape[0], self.off:self.off + n]
            self.off += n
            assert self.off <= AW, ("arena overflow", self.off)
            return v if len(shape) == 2 else v.rearrange(
                "p (a b) -> p a b", b=shape[2]) if len(shape) == 3 else v.rearrange(
                "p (a b c) -> p a b c", b=shape[2], c=shape[3])

        def bf16(self, shape):
            n = int(np.prod(shape[1:]))
            assert n % 2 == 0
            v = arena[0:shape[0], self.off:self.off + n // 2].bitcast(BF16)
            self.off += n // 2
            assert self.off <= AW, ("arena overflow", self.off)
            return v if len(shape) == 2 else v.rearrange(
                "p (a b) -> p a b", b=shape[2]) if len(shape) == 3 else v.rearrange(
                "p (a b c) -> p a b c", b=shape[2], c=shape[3])

    A = Arena()

    S.dma("sp", lambda e: e.dma_start(out=cst[:], in_=consts[:, :]), writes=[res("cst")], key="c0")
    S.dma("sp", lambda e: e.dma_start(out=sil[:], in_=cvec[:, :]), writes=[res("sil")], key="c0")
    S.dma("sp", lambda e: e.dma_start(out=flg[:], in_=flags[0, :].partition_broadcast(128)), writes=[res("flg")], key="c0")
    S.dma("sp", lambda e: e.dma_start(out=lg[:], in_=decay[0, :].partition_broadcast(128)), writes=[res("lg")], key="c0")
    S.dma("sp", lambda e: e.dma_start(out=rbias[:, 0, :], in_=b_rt[0, :].partition_broadcast(128)), writes=[res("rbias")], key="c0")
    S.dma("sp", lambda e: e.dma_start(out=rbias[:, 1, :], in_=b_rt[1, :].partition_broadcast(128)), writes=[res("rbias")], key="c0")
    S.add("dve", lambda e: e.tensor_copy(ident[:], cst[:, 0:128]), reads=[res("cst")], writes=[res("ident")])
    S.add("dve", lambda e: e.tensor_copy(identb[:], cst[:, 0:128]), reads=[res("cst")], writes=[res("identb")])
    S.add("dve", lambda e: e.tensor_copy(onesf[:], cst[:, 128:256]), reads=[res("cst")], writes=[res("onesf")])
    S.add("dve", lambda e: e.tensor_copy(onesb[:], cst[0:1, 128:192]), reads=[res("cst")], writes=[res("onesb")])
    S.add("act", lambda e: e.activation(sil[:], sil[:], AF.Silu), reads=[res("sil")], writes=[res("sil")])
    S.add("act", lambda e: e.activation(lg[:], lg[:], AF.Sigmoid), reads=[res("lg")], writes=[res("lg")])
    S.add("act", lambda e: e.activation(lg[:], lg[:], AF.Ln), reads=[res("lg")], writes=[res("lg")])

    A.reset()
    macc = A.f32([128, D])
    mbias = A.f32([128, D])
    mring = [A.f32([128, D]) for _ in range(3)]
    MR = [Res("mring%d" % i) for i in range(3)]
    pi = 0
    for l in range(2):
        for cb in range(6):
            S.dma("sp", lambda e, l=l, cb=cb: e.dma_start(out=mbias[:], in_=b_mod[l, cb * D:(cb + 1) * D].partition_broadcast(128)),
                  writes=[res("mbias")], key="mb")
            for kc in range(16):
                slot = pi % 3
                pi += 1
                S.dma("sp", lambda e, l=l, cb=cb, kc=kc, slot=slot: e.dma_start(
                    out=mring[slot][:], in_=w_mod[l][kc * 128:(kc + 1) * 128, cb * D:(cb + 1) * D]),
                    writes=[MR[slot]], key="mr%d" % slot)
                if kc == 0:
                    S.add("dve", lambda e, kc=kc, slot=slot: e.tensor_scalar(macc[:], mring[slot][:], sil[:, kc:kc + 1], None, op0=ALU.mult),
                          reads=[MR[slot], res("sil")], writes=[res("macc")])
                else:
                    S.add("dve", lambda e, kc=kc, slot=slot: e.scalar_tensor_tensor(
                        macc[:], mring[slot][:], sil[:, kc:kc + 1], macc[:], op0=ALU.mult, op1=ALU.add),
                        reads=[MR[slot], res("sil"), res("macc")], writes=[res("macc")])
            for j in range(4):
                S.add("pe", lambda e, j=j: e.matmul(ps[j][:], onesf[:], macc[:, j * 512:(j + 1) * 512], start=True, stop=True),
                      reads=[res("onesf"), res("macc")], writes=[PS[j]])
                S.add("dve", lambda e, j=j: e.tensor_tensor(mbias[:, j * 512:(j + 1) * 512], ps[j][:], mbias[:, j * 512:(j + 1) * 512], op=ALU.add),
                      reads=[PS[j], res("mbias")], writes=[res("mbias")])
            S.dma("sp", lambda e, l=l, cb=cb: e.dma_start(out=m_dram[l:l + 1, cb * D:(cb + 1) * D], in_=mbias[0:1, :]),
                  reads=[res("mbias")], writes=[res("m_dram")], key="mo")
    S.barrier()

    def bc_load(dst, src_row, r, key):
        S.dma("sp", lambda e: e.dma_start(out=dst, in_=src_row.partition_broadcast(128)), reads=[res("m_dram")], writes=[r], key=key)

    ev_flip = [0]

    def evac(dst, src, rd, wr, scale=None):
        ev_flip[0] ^= 1
        if ev_flip[0]:
            if scale is None:
                S.add("act", lambda e: e.copy(dst, src), reads=rd, writes=wr)
            else:
                S.add("act", lambda e: e.mul(dst, src, scale), reads=rd, writes=wr)
        else:
            if scale is None:
                S.add("dve", lambda e: e.tensor_copy(dst, src), reads=rd, writes=wr)
            else:
                S.add("dve", lambda e: e.tensor_scalar(dst, src, scale, None, op0=ALU.mult), reads=rd, writes=wr)

    def to_feature_major(h_ap, h_res, hT, t, hT32=None):
        for g in range(4):
            b = g % 4
            for q in range(4):
                kc = g * 4 + q
                S.add("pe", lambda e, kc=kc, b=b, q=q: e.transpose(ps[b][:, q * 128:(q + 1) * 128], h_ap[:, kc * 128:(kc + 1) * 128], ident[:]),
                      reads=[h_res, res("ident")], writes=[PS[b]])
            evac(hT[:, g * 4:(g + 1) * 4, t * 128:(t + 1) * 128], ps[b][:].rearrange("p (a b) -> p a b", b=128),
                 [PS[b]], [res("hT%d" % t), res("pstok%d" % b)])
            if hT32 is not None:
                S.add("dve", lambda e, g=g, b=b: e.tensor_copy(hT32[:, g * 4:(g + 1) * 4, :], ps[b][:].rearrange("p (a b) -> p a b", b=128)),
                      reads=[PS[b]], writes=[res("hT32"), res("pstok%d" % b)])

    def layer_norm_tile(u, u_res, g_bc, b_bc, out_ap, out_res, rd):
        for j in range(4):
            S.add("dve", lambda e, j=j: e.bn_stats(stt[:, j, :], u[:, j * 512:(j + 1) * 512]), reads=[u_res], writes=[res("stt")])
        S.add("dve", lambda e: e.bn_aggr(small[:, 0:2], stt[:]), reads=[res("stt")], writes=[res("small")])
        S.add("dve", lambda e: e.tensor_scalar(small[:, 2:3], small[:, 1:2], EPS, None, op0=ALU.add), reads=[res("small")], writes=[res("small")])
        S.add("act", lambda e: e.activation(small[:, 2:3], small[:, 2:3], AF.Ln), reads=[res("small")], writes=[res("small")])
        S.add("act", lambda e: e.activation(small[:, 2:3], small[:, 2:3], AF.Exp, scale=-0.5), reads=[res("small")], writes=[res("small")])
        S.add("dve", lambda e: e.tensor_scalar(u, u, small[:, 0:1], small[:, 2:3], op0=ALU.subtract, op1=ALU.mult),
              reads=[u_res, res("small")], writes=[u_res])
        S.add("dve", lambda e: e.tensor_tensor(u, u, g_bc, op=ALU.mult), reads=[u_res] + rd, writes=[u_res])
        S.add("dve", lambda e: e.tensor_tensor(out_ap, u, b_bc, op=ALU.add), reads=[u_res] + rd, writes=[out_res])

    def stream_w(slot_ap, src_ap, slot_res, key):
        S.dma("pool", lambda e: e.dma_start(out=slot_ap, in_=src_ap), writes=[slot_res], key=key)

    def prologue(l, x_src, hT, j_shift, j_scale):
        sc = A.f32([128, D]); sh = A.f32([128, D])
        xw = [A.f32([128, D]) for _ in range(2)]
        bc_load(sc, m_dram[l, j_scale * D:(j_scale + 1) * D], res("sc"), "bc")
        bc_load(sh, m_dram[l, j_shift * D:(j_shift + 1) * D], res("sh"), "bc")
        S.add("dve", lambda e: e.tensor_scalar(sc, sc, 1.0, None, op0=ALU.add), reads=[res("sc")], writes=[res("sc")])
        for t in range(NT):
            xt = xw[t % 2]
            xr = res("xw%d" % (t % 2))
            S.dma("sp", lambda e, t=t, xt=xt: e.dma_start(out=xt, in_=x_src[t * 128:(t + 1) * 128, :]), reads=[res("xs")], writes=[xr], key="xl%d" % (t % 2))
            S.add("dve", lambda e, xt=xt: e.tensor_tensor(xt, xt, sc, op=ALU.mult), reads=[xr, res("sc")], writes=[xr])
            S.add("dve", lambda e, xt=xt: e.tensor_tensor(xt, xt, sh, op=ALU.add), reads=[xr, res("sh")], writes=[xr])
            to_feature_major(xt, xr, hT, t)

    def epilogue(l, x_src, o_half, th, gate_j, lng, lnb, next_mod, hT_next, want_router, x_dst, l_router):
        pass

    def moe_block(l, x_src, x_dst, next_l):
        A.reset()
        hT = A.bf16([128, 16, NTOK])
        yacc = A.f32([128, NT, D])
        hid = A.bf16([128, 4, NTOK])
        ring = [A.bf16([128, 8192]) for _ in range(3)]
        RG = [Res("ring%d" % i) for i in range(3)]
        sgt = [A.bf16([128, 512]) for _ in range(2)]
        mark = A.off
        sc = A.f32([128, D]); sh = A.f32([128, D])
        xw = [A.f32([128, D])] * 2
        hT32 = A.f32([128, 16, 128])
        wr32 = A.f32([128, 16, 36])
        bc_load(sc, m_dram[l, 4 * D:5 * D], res("sc"), "bc")
        bc_load(sh, m_dram[l, 3 * D:4 * D], res("sh"), "bc")
        S.add("dve", lambda e: e.tensor_scalar(sc, sc, 1.0, None, op0=ALU.add), reads=[res("sc")], writes=[res("sc")])
        S.dma("sp", lambda e: e.dma_start(out=wr32, in_=w_rt[l].rearrange("(kc p) c -> p kc c", p=128)), writes=[res("wr32")], key="wr")
        for t in range(NT):
            xt = xw[0]
            xr = res("xw0")
            S.dma("sp", lambda e, t=t, xt=xt: e.dma_start(out=xt, in_=x_src[t * 128:(t + 1) * 128, :]), reads=[res("xs")], writes=[xr], key="xl0")
            S.add("dve", lambda e, xt=xt: e.tensor_tensor(xt, xt, sc, op=ALU.mult), reads=[xr, res("sc")], writes=[xr])
            S.add("dve", lambda e, xt=xt: e.tensor_tensor(xt, xt, sh, op=ALU.add), reads=[xr, res("sh")], writes=[xr])
            to_feature_major(xt, xr, hT, t, hT32=(hT32 if MOE_STAGE >= 0.5 else None))
            if MOE_STAGE < 1:
                continue
            for kc in range(16):
                S.add("pe", lambda e, kc=kc: e.matmul(ps[4][:, 0:36], hT32[:, kc, :], wr32[:, kc, :], start=(kc == 0), stop=(kc == 15)),
                      reads=[res("hT32"), res("wr32")], writes=[PS[4]])
            rr = res("rl")
            S.add("dve", lambda e: e.tensor_tensor(rl[:, 0:36], ps[4][:, 0:36], rbias[:, l, :], op=ALU.add), reads=[PS[4], res("rbias")], writes=[rr])
            S.add("dve", lambda e: e.tensor_reduce(small[:, 8:9], rl[:, 0:4], mybir.AxisListType.X, ALU.max), reads=[rr], writes=[res("sm_g")])
            S.add("dve", lambda e: e.tensor_scalar(rl[:, 40:44], rl[:, 0:4], small[:, 8:9], None, op0=ALU.is_equal), reads=[rr, res("sm_g")], writes=[rr])
            S.add("dve", lambda e: e.tensor_scalar(rl[:, 44:48], rl[:, 0:4], small[:, 8:9], None, op0=ALU.subtract), reads=[rr, res("sm_g")], writes=[rr])
            S.add("act", lambda e: e.activation(rl[:, 44:48], rl[:, 44:48], AF.Exp, accum_out=small[:, 9:10]), reads=[rr], writes=[rr, res("sm_g2")])
            S.add("dve", lambda e: e.reciprocal(small[:, 10:11], small[:, 9:10]), reads=[res("sm_g2")], writes=[res("sm_pg")])
            S.add("dve", lambda e: e.tensor_scalar(rl[:, 40:44], rl[:, 40:44], 1.0, 1e30, op0=ALU.subtract, op1=ALU.mult), reads=[rr], writes=[rr])
            for g_ in range(4):
                S.add("dve", lambda e, g_=g_: e.tensor_scalar(rl[:, 48 + 8 * g_:56 + 8 * g_], rl[:, 4 + 8 * g_:12 + 8 * g_], rl[:, 40 + g_:41 + g_], None, op0=ALU.add),
                      reads=[rr], writes=[rr])
            S.add("dve", lambda e: e.tensor_reduce(small[:, 11:12], rl[:, 48:80], mybir.AxisListType.X, ALU.max), reads=[rr], writes=[res("sm_m1")])
            S.add("dve", lambda e, t=t: e.tensor_scalar(comb[:, t, :], rl[:, 48:80], small[:, 11:12], None, op0=ALU.is_equal), reads=[rr, res("sm_m1")], writes=[res("comb")])
            S.add("dve", lambda e, t=t: e.scalar_tensor_tensor(rl[:, 48:80], comb[:, t, :], -1e30, rl[:, 48:80], op0=ALU.mult, op1=ALU.add),
                  reads=[rr, res("comb")], writes=[rr])
            S.add("dve", lambda e: e.tensor_reduce(small[:, 12:13], rl[:, 48:80], mybir.AxisListType.X, ALU.max), reads=[rr], writes=[res("sm_m2")])
            S.add("dve", lambda e: e.tensor_scalar(rl[:, 4:36], rl[:, 48:80], small[:, 12:13], None, op0=ALU.is_equal), reads=[rr, res("sm_m2")], writes=[rr])
            S.add("dve", lambda e: e.tensor_tensor(small[:, 13:14], small[:, 12:13], small[:, 11:12], op=ALU.subtract), reads=[res("sm_m1"), res("sm_m2")], writes=[res("sm_w")])
            S.add("act", lambda e: e.activation(small[:, 13:14], small[:, 13:14], AF.Exp), reads=[res("sm_w")], writes=[res("sm_w")])
            S.add("dve", lambda e: e.tensor_scalar(small[:, 14:15], small[:, 13:14], 1.0, None, op0=ALU.add), reads=[res("sm_w")], writes=[res("sm_d")])
            S.add("dve", lambda e: e.reciprocal(small[:, 14:15], small[:, 14:15]), reads=[res("sm_d")], writes=[res("sm_d")])
            S.add("dve", lambda e: e.tensor_tensor(small[:, 15:16], small[:, 14:15], small[:, 10:11], op=ALU.mult), reads=[res("sm_d"), res("sm_pg")], writes=[res("sm_t1")])
            S.add("dve", lambda e: e.tensor_tensor(small[:, 16:17], small[:, 15:16], small[:, 13:14], op=ALU.mult), reads=[res("sm_t1"), res("sm_w")], writes=[res("sm_t2")])
            S.add("dve", lambda e, t=t: e.tensor_scalar(comb[:, t, :], comb[:, t, :], small[:, 15:16], None, op0=ALU.mult), reads=[res("comb"), res("sm_t1")], writes=[res("comb")])
            S.add("dve", lambda e, t=t: e.scalar_tensor_tensor(comb[:, t, :], rl[:, 4:36], small[:, 16:17], comb[:, t, :], op0=ALU.mult, op1=ALU.add),
                  reads=[rr, res("sm_t2"), res("comb")], writes=[res("comb")])
        if MOE_STAGE < 2:
            S.barrier()
            return
        HT = [res("hT%d" % t) for t in range(NT)]
        si = [0]

        def nxt():
            s = si[0] % 3
            si[0] += 1
            return s
        for ex in range(32):
            sg_, su_, sd_ = nxt(), nxt(), nxt()
            G = ring[sg_].rearrange("p (k c) -> p k c", c=512)
            U = ring[su_].rearrange("p (k c) -> p k c", c=512)
            Dw = ring[sd_].rearrange("p (k c) -> p k c", c=D)
            stream_w(G, w_gate[l][ex].rearrange("(kc p) c -> p kc c", p=128), RG[sg_], "rg%d" % sg_)
            stream_w(U, w_up[l][ex].rearrange("(kc p) c -> p kc c", p=128), RG[su_], "rg%d" % su_)
            stream_w(Dw, w_down[l][ex].rearrange("(kc p) c -> p kc c", p=128), RG[sd_], "rg%d" % sd_)
            it = 0
            for th in range(2):
                for fc in range(4):
                    bg, bu = (0, 1) if it % 2 == 0 else (2, 3)
                    sgi = it % 2
                    it += 1
                    for kc in range(16):
                        S.add("pe", lambda e, kc=kc, fc=fc, th=th, bg=bg, G=G: e.matmul(ps[bg][:], G[:, kc, fc * 128:(fc + 1) * 128], hT[:, kc, th * 512:(th + 1) * 512],
                                                                                 start=(kc == 0), stop=(kc == 15)),
                              reads=[RG[sg_]] + HT[th * 4:(th + 1) * 4], writes=[PS[bg]])
                    for kc in range(16):
                        S.add("pe", lambda e, kc=kc, fc=fc, th=th, bu=bu, U=U: e.matmul(ps[bu][:], U[:, kc, fc * 128:(fc + 1) * 128], hT[:, kc, th * 512:(th + 1) * 512],
                                                                                 start=(kc == 0), stop=(kc == 15)),
                              reads=[RG[su_]] + HT[th * 4:(th + 1) * 4], writes=[PS[bu]])
                    S.add("act", lambda e, bg=bg, sgi=sgi: e.activation(sgt[sgi], ps[bg][:], AF.Silu), reads=[PS[bg]], writes=[res("sgt%d" % sgi)])
                    S.add("dve", lambda e, bu=bu, sgi=sgi, fc=fc, th=th: e.tensor_tensor(hid[:, fc, th * 512:(th + 1) * 512], sgt[sgi], ps[bu][:], op=ALU.mult),
                          reads=[PS[bu], res("sgt%d" % sgi)], writes=[res("hid%d" % th)])
            it = 0
            for t in range(NT):
                for dc in range(4):
                    b = 4 + (it % 3)
                    it += 1
                    for fc in range(4):
                        S.add("pe", lambda e, fc=fc, t=t, dc=dc, b=b, Dw=Dw: e.matmul(ps[b][:], hid[:, fc, t * 128:(t + 1) * 128], Dw[:, fc, dc * 512:(dc + 1) * 512],
                                                                               start=(fc == 0), stop=(fc == 3)),
                              reads=[RG[sd_], res("hid%d" % (t // 4))], writes=[PS[b]])
                    yr = res("yacc%d" % t)
                    if ex == 0:
                        S.add("dve", lambda e, t=t, dc=dc, b=b, ex=ex: e.tensor_scalar(yacc[:, t, dc * 512:(dc + 1) * 512], ps[b][:], comb[:, t, ex:ex + 1], None, op0=ALU.mult),
                              reads=[PS[b], res("comb")], writes=[yr])
                    else:
                        S.add("dve", lambda e, t=t, dc=dc, b=b, ex=ex: e.scalar_tensor_tensor(yacc[:, t, dc * 512:(dc + 1) * 512], ps[b][:], comb[:, t, ex:ex + 1],
                                                                                              yacc[:, t, dc * 512:(dc + 1) * 512], op0=ALU.mult, op1=ALU.add),
                              reads=[PS[b], res("comb"), yr], writes=[yr])
        if MOE_STAGE < 3:
            S.barrier()
            return
        gt = sc; lgt = sh
        lbt = hT32.rearrange("p a b -> p (a b)")
        bc_load(gt, m_dram[l, 5 * D:6 * D], res("sc"), "bc")
        S.dma("sp", lambda e: e.dma_start(out=lgt, in_=ln2_g[l, :].partition_broadcast(128)), writes=[res("sh")], key="bc")
        S.dma("sp", lambda e: e.dma_start(out=lbt, in_=ln2_b[l, :].partition_broadcast(128)), writes=[res("hT32")], key="bc")
        for t in range(NT):
            xt = xw[0]
            xr = res("xw0")
            yr = res("yacc%d" % t)
            S.dma("sp", lambda e, t=t, xt=xt: e.dma_start(out=xt, in_=x_src[t * 128:(t + 1) * 128, :]), reads=[res("xs")], writes=[xr], key="xl0")
            S.add("dve", lambda e, t=t: e.tensor_tensor(yacc[:, t, :], yacc[:, t, :], gt, op=ALU.mult), reads=[yr, res("sc")], writes=[yr])
            S.add("dve", lambda e, t=t, xt=xt: e.scalar_tensor_tensor(xt, xt, ALPHA, yacc[:, t, :], op0=ALU.mult, op1=ALU.add), reads=[yr, xr], writes=[xr])
            layer_norm_tile(xt, xr, lgt, lbt, xt, xr, [res("sh"), res("hT32")])
            S.dma("sp", lambda e, t=t, xt=xt: e.dma_start(out=x_dst[t * 128:(t + 1) * 128, :], in_=xt), reads=[xr], writes=[res("xdst")], key="xo")
        S.barrier()

    def outproj_block(l, KC, w_out_ap, x_src, x_dst):
        A.reset()
        CW = 8192 // KC
        npiece = D // CW
        yTh = A.bf16([128, KC, 512])
        oh = A.f32([128, 4, D])
        ring = [A.bf16([128, 8192]) for _ in range(3)]
        RG = [Res("oring%d" % i) for i in range(3)]
        gt = A.f32([128, D]); lgt = A.f32([128, D]); lbt = A.f32([128, D])
        xw = [A.f32([128, D]) for _ in range(2)]
        bc_load(gt, m_dram[l, 2 * D:3 * D], res("gt"), "bc")
        S.dma("sp", lambda e: e.dma_start(out=lgt, in_=ln1_g[l, :].partition_broadcast(128)), writes=[res("lgt")], key="bc")
        S.dma("sp", lambda e: e.dma_start(out=lbt, in_=ln1_b[l, :].partition_broadcast(128)), writes=[res("lbt")], key="bc")
        pc = 0
        for th in range(2):
            S.dma("sp", lambda e, th=th: e.dma_start(out=yTh, in_=yT_dram[:, 0:KC, th * 512:(th + 1) * 512]), reads=[res("yT_dram")], writes=[res("yTh")], key="yt")
            it = 0
            for p in range(npiece):
                slot = pc % 3
                pc += 1
                W = ring[slot].rearrange("p (k c) -> p k c", c=CW)
                stream_w(W, w_out_ap[:, p * CW:(p + 1) * CW].rearrange("(kc p) c -> p kc c", p=128), RG[slot], "or%d" % slot)
                for tt in range(4):
                    b = it % 4
                    it += 1
                    for kc in range(KC):
                        S.add("pe", lambda e, kc=kc, tt=tt, b=b, W=W: e.matmul(ps[b][:, 0:CW], yTh[:, kc, tt * 128:(tt + 1) * 128], W[:, kc, :], start=(kc == 0), stop=(kc == KC - 1)),
                              reads=[RG[slot], res("yTh")], writes=[PS[b]])
                    evac(oh[:, tt, p * CW:(p + 1) * CW], ps[b][:, 0:CW], [PS[b]], [res("oh%d" % tt)])
            for tt in range(4):
                t = th * 4 + tt
                xt = xw[t % 2]
                xr = res("xw%d" % (t % 2))
                orr = res("oh%d" % tt)
                S.dma("sp", lambda e, t=t, xt=xt: e.dma_start(out=xt, in_=x_src[t * 128:(t + 1) * 128, :]), reads=[res("xs")], writes=[xr], key="xl%d" % (t % 2))
                S.add("dve", lambda e, tt=tt: e.tensor_tensor(oh[:, tt, :], oh[:, tt, :], gt, op=ALU.mult), reads=[orr, res("gt")], writes=[orr])
                S.add("dve", lambda e, tt=tt, xt=xt: e.scalar_tensor_tensor(xt, xt, ALPHA, oh[:, tt, :], op0=ALU.mult, op1=ALU.add), reads=[orr, xr], writes=[xr])
                layer_norm_tile(xt, xr, lgt, lbt, xt, xr, [res("lgt"), res("lbt")])
                S.dma("sp", lambda e, t=t, xt=xt: e.dma_start(out=x_dst[t * 128:(t + 1) * 128, :], in_=xt), reads=[xr], writes=[res("xdst")], key="xo")
        S.barrier()

    def retention_block(l, x_src):
        A.reset()
        hT = A.bf16([128, 16, NTOK])
        mark0 = A.off
        prologue(l, x_src, hT, 0, 1)
        S.barrier()
        A.off = mark0
        HT = [res("hT%d" % t) for t in range(NT)]
        ring = [A.bf16([128, 8192]) for _ in range(2)]
        RG = [Res("rring%d" % i) for i in range(2)]
        QT = A.bf16([128, 2, NTOK]); KT = A.bf16([128, 2, NTOK])
        k_tm = A.bf16([64, NCH, 256]); v_tm = A.bf16([64, NCH, 512])
        sgf = A.bf16([64, NCH, 512]); sgb = A.bf16([64, NCH, 512])
        yf = A.bf16([64, NCH, 512])
        off_rc = A.off
        rc = A.bf16([64, NCH, 256]); rs = A.bf16([64, NCH, 256])
        yTh = arena[0:128, off_rc:off_rc + 2048].bitcast(BF16).rearrange("p (a b) -> p a b", b=NTOK)
        St2 = [A.f32([128, 2, 512]) for _ in range(2)]; Sb2 = [A.bf16([128, 2, 512]) for _ in range(2)]
        DT2 = [A.f32([64, 64]) for _ in range(2)]; qdec2 = [A.bf16([128, 2, 64]) for _ in range(2)]; kd2 = [A.f32([64, 2]) for _ in range(2)]
        gng = A.f32([64, 512]); gnb = A.f32([64, 512])
        t12 = [A.f32([64, 512]) for _ in range(2)]
        t1 = t12[0]; t2 = t12[1]
        qr = A.bf16([64, 256])
        Pm2 = [A.bf16([64, 64]) for _ in range(2)]; Qd2 = [A.bf16([128, 2, 64]) for _ in range(2)]; Kd2 = [A.bf16([64, 256]) for _ in range(2)]
        ybk = A.bf16([64, NCH, 512])
        S.dma("pool", lambda e: e.dma_start(out=rs, in_=ropes[:, :, :]), writes=[res("rs")], key="rt")
        pc = [0]

        def piece(c0, ncols):
            slot = pc[0] % 2
            pc[0] += 1
            W = ring[slot][:, 0:16 * ncols].rearrange("p (k c) -> p k c", c=ncols)
            stream_w(W, w_ret_in[:, c0:c0 + ncols].rearrange("(kc p) c -> p kc c", p=128), RG[slot], "rr%d" % slot)
            return W, RG[slot]

        def proj_chunk(W, Wr, c, ncols, b):
            for kc in range(16):
                S.add("pe", lambda e, kc=kc: e.matmul(ps[b][0:64, 0:ncols], hT[:, kc, c * 64:(c + 1) * 64], W[:, kc, :], start=(kc == 0), stop=(kc == 15)),
                      reads=[Wr, HT[c // 2]], writes=[PS[b]])

        def rope_to(dst, dst_res, c, b, kscale):
            p4 = ps[b][0:64, 0:256].rearrange("p (a h f) -> p a h f", a=2, h=2)
            S.add("dve", lambda e: e.tensor_tensor(t1[:, 0:256], ps[b][0:64, 0:256], rc[:, c, :], op=ALU.mult), reads=[PS[b], res("rc")], writes=[res("t1")])
            S.add("dve", lambda e: e.tensor_tensor(t2[:, 0:256].rearrange("p (a h f) -> p a h f", a=2, h=2), p4[:, :, ::-1, :],
                                                   rs[:, c, :].rearrange("p (a h f) -> p a h f", a=2, h=2), op=ALU.mult), reads=[PS[b], res("rs")], writes=[res("t2")])
            if kscale is None:
                S.add("dve", lambda e: e.tensor_tensor(dst, t1[:, 0:256], t2[:, 0:256], op=ALU.add), reads=[res("t1"), res("t2")], writes=[dst_res])
            else:
                S.add("dve", lambda e: e.tensor_tensor(t1[:, 0:256], t1[:, 0:256], t2[:, 0:256], op=ALU.add), reads=[res("t1"), res("t2")], writes=[res("t1")])
                S.add("dve", lambda e: e.tensor_scalar(dst, t1[:, 0:256], kscale, None, op0=ALU.mult), reads=[res("t1")], writes=[dst_res])

        def tr_to(dstT, dstT_res, src, src_res, c):
            for dc in range(2):
                S.add("pe", lambda e, dc=dc: e.transpose(psb[:, dc * 64:(dc + 1) * 64], src[:, dc * 128:(dc + 1) * 128], identb[0:64, 0:64]),
                      reads=[src_res, res("identb")], writes=[PSB])
            evac(dstT[:, :, c * 64:(c + 1) * 64], psb[:, 0:128].rearrange("p (a b) -> p a b", b=64), [PSB], [dstT_res])

        for h in range(8):
            S.dma("pool", lambda e: e.dma_start(out=rc, in_=ropec[:, :, :]), writes=[res("rc")], key="rtc")
            W, Wr = piece(h * 256, 256)
            for c in range(NCH):
                b = c % 2
                proj_chunk(W, Wr, c, 256, b)
                rope_to(qr, res("qr"), c, b, None)
                tr_to(QT, res("QT"), qr, res("qr"), c)
            W, Wr = piece(2048 + h * 256, 256)
            for c in range(NCH):
                b = c % 2
                proj_chunk(W, Wr, c, 256, b)
                rope_to(k_tm[:, c, :], res("k_tm"), c, b, 1.0 / 16.0)
                tr_to(KT, res("KT"), k_tm[:, c, :], res("k_tm"), c)
            W, Wr = piece(4096 + h * 512, 512)
            for c in range(NCH):
                b = c % 2
                proj_chunk(W, Wr, c, 512, b)
                evac(v_tm[:, c, :], ps[b][0:64, :], [PS[b]], [res("v_tm")])
            W, Wr = piece(8192 + h * 512, 512)
            for c in range(NCH):
                b = c % 2
                proj_chunk(W, Wr, c, 512, b)
                S.add("act", lambda e, c=c, b=b: e.activation(sgf[:, c, :], ps[b][0:64, :], AF.Silu), reads=[PS[b]], writes=[res("sgf")])
            W, Wr = piece(12288 + h * 512, 512)
            for c in range(NCH):
                b = c % 2
                proj_chunk(W, Wr, c, 512, b)
                S.add("act", lambda e, c=c, b=b: e.activation(sgb[:, c, :], ps[b][0:64, :], AF.Silu), reads=[PS[b]], writes=[res("sgb")])
            S.dma("sp", lambda e, h=h: e.dma_start(out=gng, in_=gn_g[h, :].partition_broadcast(64)), writes=[res("gng")], key="gn")
            S.dma("sp", lambda e, h=h: e.dma_start(out=gnb, in_=gn_b[h, :].partition_broadcast(64)), writes=[res("gnb")], key="gn")
            for dr in range(2):
                col = dr * 8 + h
                lgc = lg[:, col:col + 1]
                dif = cst[0:64, 384:448] if dr == 0 else cst[0:64, 512:576]
                tri = cst[0:64, 448:512] if dr == 0 else cst[0:64, 576:640]
                ramp = cst[:, 256:320] if dr == 0 else cst[:, 320:384]
                DTd, qdd, kdd = DT2[dr], qdec2[dr], kd2[dr]
                S.add("act", lambda e, dif=dif, lgc=lgc, DTd=DTd: e.activation(DTd, dif, AF.Exp, scale=lgc[0:64, :]), reads=[res("cst"), res("lg")], writes=[res("DT%d" % dr)])
                S.add("dve", lambda e, tri=tri, DTd=DTd: e.tensor_tensor(DTd, DTd, tri, op=ALU.mult), reads=[res("DT%d" % dr), res("cst")], writes=[res("DT%d" % dr)])
                for dc in range(2):
                    S.add("act", lambda e, dc=dc, ramp=ramp, lgc=lgc, qdd=qdd: e.activation(qdd[:, dc, :], ramp, AF.Exp, scale=lgc), reads=[res("cst"), res("lg")], writes=[res("qdec%d" % dr)])
                S.add("act", lambda e, dr=dr, lgc=lgc, kdd=kdd: e.activation(kdd[:, 0:1], cst[0:64, 640 + dr:641 + dr], AF.Exp, scale=lgc[0:64, :]), reads=[res("cst"), res("lg")], writes=[res("kd%d" % dr)])
                S.add("act", lambda e, dr=dr, lgc=lgc: e.activation(small[:, 20 + dr:21 + dr], cst[:, 642:643], AF.Exp, scale=lgc), reads=[res("cst"), res("lg")], writes=[res("cdec%d" % dr)])
                S.dma("sp", lambda e, dr=dr, h=h: e.dma_start(out=St2[dr], in_=s0[dr, h].rearrange("(dc p) v -> p dc v", p=128)), writes=[res("St%d" % dr)], key="s0%d" % dr)
            for idx in range(NCH):
                for dr in range(2):
                    c = idx if dr == 0 else NCH - 1 - idx
                    Std, Sbd, DTd, qdd, kdd = St2[dr], Sb2[dr], DT2[dr], qdec2[dr], kd2[dr]
                    Pmd, Qdd, Kdd, t1d = Pm2[dr], Qd2[dr], Kd2[dr], t12[dr]
                    bI, bO, bK = (2, 3, 4) if dr == 0 else (5, 6, 0)
                    sm0 = 0 if dr == 0 else 4
                    rSt, rSb, rSm, rT1 = res("St%d" % dr), res("Sb%d" % dr), res("small%d" % dr), res("t1%d" % dr)
                    if idx % 4 == 0 and idx > 0:
                        S.add("dve", lambda e, Std=Std: e.tensor_scalar(Std, Std, flg[:, 0:1], None, op0=ALU.mult), reads=[rSt, res("flg")], writes=[rSt])
                    for dc in range(2):
                        S.add("pe", lambda e, dc=dc, c=c, bI=bI: e.matmul(ps[bI][0:64, 0:64], KT[:, dc, c * 64:(c + 1) * 64], QT[:, dc, c * 64:(c + 1) * 64], start=(dc == 0), stop=(dc == 1)),
                              reads=[res("KT"), res("QT")], writes=[PS[bI]])
                    S.add("dve", lambda e, bI=bI, Pmd=Pmd, DTd=DTd: e.tensor_tensor(Pmd, ps[bI][0:64, 0:64], DTd, op=ALU.mult), reads=[PS[bI], res("DT%d" % dr)], writes=[res("Pm%d" % dr)])
                    S.add("pool", lambda e, c=c, Qdd=Qdd, qdd=qdd: e.tensor_tensor(Qdd, QT[:, :, c * 64:(c + 1) * 64], qdd, op=ALU.mult), reads=[res("QT"), res("qdec%d" % dr)], writes=[res("Qd%d" % dr)])
                    S.add("act", lambda e, Sbd=Sbd, Std=Std: e.copy(Sbd, Std), reads=[rSt], writes=[rSb])
                    S.add("pe", lambda e, c=c, bO=bO, Pmd=Pmd: e.matmul(ps[bO][0:64, :], Pmd, v_tm[:, c, :], start=True, stop=False), reads=[res("Pm%d" % dr), res("v_tm")], writes=[PS[bO]])
                    for dc in range(2):
                        S.add("pe", lambda e, dc=dc, bO=bO, Qdd=Qdd, Sbd=Sbd: e.matmul(ps[bO][0:64, :], Qdd[:, dc, :], Sbd[:, dc, :], start=False, stop=(dc == 1)), reads=[res("Qd%d" % dr), rSb], writes=[PS[bO]])
                    S.add("dve", lambda e, bO=bO, dr=dr: e.bn_stats(stt[0:64, dr, :], ps[bO][0:64, :]), reads=[PS[bO]], writes=[res("stt%d" % dr)])
                    S.add("dve", lambda e, dr=dr, sm0=sm0: e.bn_aggr(small[0:64, sm0:sm0 + 2], stt[0:64, dr:dr + 1, :]), reads=[res("stt%d" % dr)], writes=[rSm])
                    S.add("dve", lambda e, sm0=sm0: e.tensor_scalar(small[0:64, sm0 + 2:sm0 + 3], small[0:64, sm0 + 1:sm0 + 2], EPS, None, op0=ALU.add), reads=[rSm], writes=[rSm])
                    S.add("act", lambda e, sm0=sm0: e.activation(small[0:64, sm0 + 2:sm0 + 3], small[0:64, sm0 + 2:sm0 + 3], AF.Ln), reads=[rSm], writes=[rSm])
                    S.add("act", lambda e, sm0=sm0: e.activation(small[0:64, sm0 + 2:sm0 + 3], small[0:64, sm0 + 2:sm0 + 3], AF.Exp, scale=-0.5), reads=[rSm], writes=[rSm])
                    S.add("dve", lambda e, bO=bO, sm0=sm0, t1d=t1d: e.tensor_scalar(t1d, ps[bO][0:64, :], small[0:64, sm0:sm0 + 1], small[0:64, sm0 + 2:sm0 + 3], op0=ALU.subtract, op1=ALU.mult),
                          reads=[PS[bO], rSm], writes=[rT1])
                    S.add("dve", lambda e, t1d=t1d: e.tensor_tensor(t1d, t1d, gng, op=ALU.mult), reads=[rT1, res("gng")], writes=[rT1])
                    S.add("dve", lambda e, t1d=t1d: e.tensor_tensor(t1d, t1d, gnb, op=ALU.add), reads=[rT1, res("gnb")], writes=[rT1])
                    if dr == 0:
                        S.add("dve", lambda e, c=c, t1d=t1d: e.tensor_tensor(yf[:, c, :], t1d, sgf[:, c, :], op=ALU.mult), reads=[rT1, res("sgf")], writes=[res("yf%d" % c)])
                    else:
                        S.add("dve", lambda e, c=c, t1d=t1d: e.tensor_tensor(ybk[:, c, :], t1d, sgb[:, c, :], op=ALU.mult), reads=[rT1, res("sgb")], writes=[res("ybk%d" % c)])
                    S.add("dve", lambda e, c=c, Kdd=Kdd, kdd=kdd: e.tensor_scalar(Kdd, k_tm[:, c, :], kdd[:, 0:1], None, op0=ALU.mult), reads=[res("k_tm"), res("kd%d" % dr)], writes=[res("Kd%d" % dr)])
                    for dc in range(2):
                        S.add("pe", lambda e, dc=dc, c=c, bK=bK, Kdd=Kdd: e.matmul(ps[bK][:, :], Kdd[:, dc * 128:(dc + 1) * 128], v_tm[:, c, :], start=True, stop=True),
                              reads=[res("Kd%d" % dr), res("v_tm")], writes=[PS[bK]])
                        S.add("dve", lambda e, dc=dc, bK=bK, Std=Std, dr=dr: e.scalar_tensor_tensor(Std[:, dc, :], Std[:, dc, :], small[:, 20 + dr:21 + dr], ps[bK][:, :], op0=ALU.mult, op1=ALU.add),
                              reads=[rSt, res("cdec%d" % dr), PS[bK]], writes=[rSt])
                    if idx % 4 == 3:
                        sq = c // 4
                        S.dma("sp", lambda e, sq=sq, dr=dr, h=h, Std=Std: e.dma_start(out=st_out[sq, dr, h].rearrange("(dc p) v -> p dc v", p=128), in_=Std),
                              reads=[rSt], writes=[res("st_out")], key="so%d" % dr)
            for c in range(NCH):
                S.add("pool", lambda e, c=c: e.tensor_tensor(yf[:, c, :], yf[:, c, :], ybk[:, c, :], op=ALU.add), reads=[res("yf%d" % c), res("ybk%d" % c)], writes=[res("yf%d" % c)])
                po = 256 + (c % 2) * 256
                for vc in range(4):
                    S.add("pe", lambda e, vc=vc, c=c, po=po: e.transpose(psb[:, po + vc * 64:po + (vc + 1) * 64], yf[:, c, vc * 128:(vc + 1) * 128], identb[0:64, 0:64]),
                          reads=[res("yf%d" % c), res("identb")], writes=[PSB])
                evac(yTh[:, :, c * 64:(c + 1) * 64], psb[:, po:po + 256].rearrange("p (a b) -> p a b", b=64), [PSB], [res("rc")])
            S.dma("sp", lambda e, h=h: e.dma_start(out=yT_dram[:, h * 4:(h + 1) * 4, :], in_=yTh), reads=[res("rc")], writes=[res("yT_dram")], key="yo")
        S.barrier()

    def na_block(l, x_src):
        A.reset()
        hT = A.bf16([128, 16, NTOK])
        mark0 = A.off
        prologue(l, x_src, hT, 0, 1)
        S.barrier()
        A.off = mark0
        HT = [res("hT%d" % t) for t in range(NT)]
        wq = A.bf16([128, 16, 128]); wk = A.bf16([128, 16, 128]); wv = A.bf16([128, 16, 128])
        QT = A.bf16([128, NTOK]); KT = A.bf16([128, NTOK])
        Va = A.bf16([64, NCH, 130]); Vc = A.bf16([64, 8, 130])
        KcT = A.bf16([128, 512])
        kc32 = A.f32([128, 4, 128])
        ko = A.f32([64, NCH, 128]); vo = A.f32([64, NCH, 128])
        KT32 = A.f32([128, NTOK]); VT32 = A.f32([128, NTOK])
        bl = A.bf16([64, 15, 64]); rm = A.bf16([1, 16, 16, 64])
        PT = A.bf16([64, 16, 64])
        ao = A.bf16([64, 128]); rec = A.f32([64, 2])
        aT = A.bf16([128, NTOK])
        S.dma("pool", lambda e: e.dma_start(out=rm, in_=narow[:, :, :, :]), writes=[res("rm")], key="nb")
        for h in range(16):
            stream_w(wq, w_na_in[:, h * 128:(h + 1) * 128].rearrange("(kc p) c -> p kc c", p=128), res("wq"), "wq")
            stream_w(wk, w_na_in[:, D + h * 128:D + (h + 1) * 128].rearrange("(kc p) c -> p kc c", p=128), res("wk"), "wk")
            stream_w(wv, w_na_in[:, 2 * D + h * 128:2 * D + (h + 1) * 128].rearrange("(kc p) c -> p kc c", p=128), res("wv"), "wv")
            S.dma("pool", lambda e, h=h: e.dma_start(out=bl, in_=nabias[:, h, :, :]), writes=[res("bl")], key="nb")
            S.dma("sp", lambda e, h=h: e.dma_start(out=kc32, in_=ck[h].rearrange("(a p) d -> p a d", p=128)), writes=[res("kc32")], key="ck")
            S.dma("pool", lambda e, h=h: e.dma_start(out=Vc[:, :, 0:128], in_=cv[h].rearrange("(a p) d -> p a d", p=64)), writes=[res("Vc")], key="cv")
            S.add("dve", lambda e: e.tensor_copy(Vc[:, :, 128:129], cst[0:64, 128:136].rearrange("p (a b) -> p a b", b=1)), reads=[res("cst")], writes=[res("Vc")])
            S.add("dve", lambda e: e.tensor_copy(Va[:, :, 128:129], cst[0:64, 128:144].rearrange("p (a b) -> p a b", b=1)), reads=[res("cst")], writes=[res("Va")])
            for a in range(4):
                S.add("pe", lambda e, a=a: e.transpose(ps[6][:, a * 128:(a + 1) * 128], kc32[:, a, :], ident[:]), reads=[res("kc32"), res("ident")], writes=[PS[6]])
            evac(KcT, ps[6][:], [PS[6]], [res("KcT")])
            for th in range(2):
                for kc in range(16):
                    S.add("pe", lambda e, kc=kc, th=th: e.matmul(ps[0][:], wq[:, kc, :], hT[:, kc, th * 512:(th + 1) * 512], start=(kc == 0), stop=(kc == 15)),
                          reads=[res("wq")] + HT[th * 4:(th + 1) * 4], writes=[PS[0]])
                evac(QT[:, th * 512:(th + 1) * 512], ps[0][:], [PS[0]], [res("QT")], scale=128.0 ** -0.5)
                for kc in range(16):
                    S.add("pe", lambda e, kc=kc, th=th: e.matmul(ps[1][:], wk[:, kc, :], hT[:, kc, th * 512:(th + 1) * 512], start=(kc == 0), stop=(kc == 15)),
                          reads=[res("wk")] + HT[th * 4:(th + 1) * 4], writes=[PS[1]])
                evac(KT32[:, th * 512:(th + 1) * 512], ps[1][:], [PS[1]], [res("KT32")])
                S.add("pool", lambda e, th=th: e.tensor_copy(KT[:, th * 512:(th + 1) * 512], KT32[:, th * 512:(th + 1) * 512]), reads=[res("KT32")], writes=[res("KT")])
                for kc in range(16):
                    S.add("pe", lambda e, kc=kc, th=th: e.matmul(ps[6][:], wv[:, kc, :], hT[:, kc, th * 512:(th + 1) * 512], start=(kc == 0), stop=(kc == 15)),
                          reads=[res("wv")] + HT[th * 4:(th + 1) * 4], writes=[PS[6]])
                evac(VT32[:, th * 512:(th + 1) * 512], ps[6][:], [PS[6]], [res("VT32")])
            for r in range(NCH):
                b = 2 + (r % 2)
                S.add("pe", lambda e, r=r, b=b: e.transpose(ps[b][0:64, 0:128], KT32[:, r * 64:(r + 1) * 64], ident[:]), reads=[res("KT32"), res("ident")], writes=[PS[b]])
                S.add("pe", lambda e, r=r, b=b: e.transpose(ps[b][0:64, 128:256], VT32[:, r * 64:(r + 1) * 64], ident[:]), reads=[res("VT32"), res("ident")], writes=[PS[b]])
                S.add("act", lambda e, r=r, b=b: e.copy(ko[:, r, :], ps[b][0:64, 0:128]), reads=[PS[b]], writes=[res("ko"), res("pstok%d" % b)])
                S.add("act", lambda e, r=r, b=b: e.copy(Va[:, r, 0:128], ps[b][0:64, 128:256]), reads=[PS[b]], writes=[res("Va"), res("pstok%d" % b)])
                S.add("dve", lambda e, r=r, b=b: e.tensor_copy(vo[:, r, :], ps[b][0:64, 128:256]), reads=[PS[b]], writes=[res("vo"), res("pstok%d" % b)])
            S.dma("sp", lambda e, h=h: e.dma_start(out=k_out[h].rearrange("(r p) d -> p r d", p=64), in_=ko), reads=[res("ko")], writes=[res("k_out")], key="kvo")
            S.dma("sp", lambda e, h=h: e.dma_start(out=v_out[h].rearrange("(r p) d -> p r d", p=64), in_=vo), reads=[res("vo")], writes=[res("v_out")], key="kvo")
            for r in range(NCH):
                r0 = min(max(r - 4, 0), 8)
                pb = 4 + (r % 2)
                S4 = ps[pb][0:64, :].rearrange("p (j q) -> p j q", q=64)
                pb2 = 0 if pb == 4 else 1
                for j in range(16):
                    bank, jj = (pb, j) if j < 8 else (pb2, j - 8)
                    dst = ps[bank][0:64, jj * 64:(jj + 1) * 64]
                    if j < 8:
                        kr = r0 + j
                        drr = kr - r + 7
                        S.add("pe", lambda e, dst=dst, kr=kr, r=r: e.matmul(dst, KT[:, kr * 64:(kr + 1) * 64], QT[:, r * 64:(r + 1) * 64], start=True, stop=False),
                              reads=[res("KT"), res("QT")], writes=[PS[bank]])
                        S.add("pe", lambda e, dst=dst, drr=drr: e.matmul(dst, identb[0:64, 0:64], bl[:, drr, :], start=False, stop=False),
                              reads=[res("identb"), res("bl")], writes=[PS[bank]])
                        S.add("pe", lambda e, dst=dst, r=r, j=j: e.matmul(dst, rm[0:1, r, j, :], onesb[0:1, 0:64], start=False, stop=True),
                              reads=[res("rm"), res("onesb")], writes=[PS[bank]])
                    else:
                        p = j - 8
                        S.add("pe", lambda e, dst=dst, p=p, r=r: e.matmul(dst, KcT[:, p * 64:(p + 1) * 64], QT[:, r * 64:(r + 1) * 64], start=True, stop=True),
                              reads=[res("KcT"), res("QT")], writes=[PS[bank]])
                S.add("act", lambda e, pb=pb: e.activation(PT[:, 0:8, :], ps[pb][0:64, :].rearrange("p (j q) -> p j q", q=64), AF.Exp), reads=[PS[pb]], writes=[res("PT")])
                S.add("act", lambda e, pb2=pb2: e.activation(PT[:, 8:16, :], ps[pb2][0:64, :].rearrange("p (j q) -> p j q", q=64), AF.Exp, bias=flg[0:64, 1:2]),
                      reads=[PS[pb2], res("flg")], writes=[res("PT")])
                ob = 2 + (r % 2)
                for j in range(16):
                    rhs = Va[:, r0 + j, 0:129] if j < 8 else Vc[:, j - 8, 0:129]
                    S.add("pe", lambda e, j=j, rhs=rhs, ob=ob: e.matmul(ps[ob][0:64, 0:129], PT[:, j, :], rhs, start=(j == 0), stop=(j == 15)),
                          reads=[res("PT"), res("Va"), res("Vc")], writes=[PS[ob]])
                S.add("dve", lambda e, ob=ob: e.reciprocal(rec[:, 0:1], ps[ob][0:64, 128:129]), reads=[PS[ob]], writes=[res("rec")])
                S.add("dve", lambda e, ob=ob: e.tensor_scalar(ao, ps[ob][0:64, 0:128], rec[:, 0:1], None, op0=ALU.mult), reads=[PS[ob], res("rec")], writes=[res("ao")])
                S.add("pe", lambda e, r=r: e.transpose(psb[:, 512 + (r % 2) * 64:512 + (r % 2 + 1) * 64], ao, identb[0:64, 0:64]), reads=[res("ao"), res("identb")], writes=[PSB])
                evac(aT[:, r * 64:(r + 1) * 64], psb[:, 512 + (r % 2) * 64:512 + (r % 2 + 1) * 64], [PSB], [res("aT")])
            S.dma("sp", lambda e, h=h: e.dma_start(out=yT_dram[:, h, :], in_=aT), reads=[res("aT")], writes=[res("yT_dram")], key="yo")
        S.barrier()

    if upto >= 1:
        retention_block(0, x_in)
    if upto >= 2:
        outproj_block(0, 32, w_ret_out, x_in, xs)
    if upto >= 3:
        moe_block(0, xs, xs, 1)
    if upto >= 4:
        na_block(1, xs)
    if upto >= 5:
        outproj_block(1, 16, w_na_out, xs, xs)
    if upto >= 6:
        moe_block(1, xs, y_out, None)

    block = E(nc.Block())
    S.emit(stack, block)
    stack.close()
    return nc


def _consts():
    c = np.zeros((128, 1024), np.float32)
    c[:, 0:128] = np.eye(128, dtype=np.float32)
    c[:, 128:256] = 1.0
    i = np.arange(64, dtype=np.float32)
    c[:, 256:320] = (i + 1.0)[None, :]
    c[:, 320:384] = (64.0 - i)[None, :]
    jj, ii = np.meshgrid(i, i, indexing="ij")
    c[0:64, 384:448] = np.maximum(ii - jj, 0.0)
    c[0:64, 448:512] = (ii >= jj).astype(np.float32)
    c[0:64, 512:576] = np.maximum(jj - ii, 0.0)
    c[0:64, 576:640] = (jj >= ii).astype(np.float32)
    c[0:64, 640] = 63.0 - i
    c[0:64, 641] = i
    c[:, 642] = 64.0
    return c


def _rope_tables(is_sample):
    nf = 64
    t = np.arange(NTOK)
    cos2 = np.ones((NTOK, 2, 2, nf), np.float32)
    sin2 = np.zeros((NTOK, 2, 2, nf), np.float32)
    if is_sample:
        rows = (t // 64).astype(np.float32)
        cols = (t % 64).astype(np.float32)
        inv = (np.float32(10000.0) ** (-np.arange(nf, dtype=np.float32) / np.float32(nf))).astype(np.float32)
        ang = np.stack([rows[:, None] * inv, cols[:, None] * inv], axis=1).astype(np.float32)
        cs, sn = np.cos(ang).astype(np.float32), np.sin(ang).astype(np.float32)
        cos2[:, :, 0, :] = cs
        cos2[:, :, 1, :] = cs
        sin2[:, :, 0, :] = -sn
        sin2[:, :, 1, :] = sn
    rc = cos2.reshape(NCH, 64, 256).transpose(1, 0, 2)
    rs = sin2.reshape(NCH, 64, 256).transpose(1, 0, 2)
    return np.ascontiguousarray(rc), np.ascontiguousarray(rs)


def _na_tables(is_sample, rpb):
    bias = np.zeros((64, 16, 15, 64), np.float32)
    rowm = np.zeros((1, 16, 16, 64), np.float32)
    col = np.arange(64)
    if is_sample:
        c0 = np.clip(col - 8, 0, 48)
        ok = (col[None, :] >= c0[:, None]) & (col[None, :] < c0[:, None] + 16)
        dc = np.clip(col[None, :] - col[:, None], -15, 15) + 15
        g = rpb[:, :, dc]
        g = np.where(ok[None, None], g, np.float32(NEG))
        bias[:] = g.transpose(3, 0, 1, 2)
    else:
        for r in range(16):
            r0 = min(max(r - 4, 0), 8)
            for j in range(16):
                if j < 8:
                    if (r0 + j) // 4 != r // 4:
                        rowm[0, r, j, :] = NEG
                else:
                    rowm[0, r, j, :] = NEG
    return bias, rowm


_NC_CACHE = {}


def _make_in_maps(x_prompt, x_sample, state_ret, cache_na_k, cache_na_v, c, c_ctx, w_mod, b_mod, ln1_g, ln1_b, ln2_g, ln2_b,
           w_ret_in, ret_decay_logit, ret_gn_g, ret_gn_b, w_ret_out, w_na_in, na_rpb, w_na_out, w_rg, b_rg, w_re, b_re,
           w_gate, w_up, w_down):
    f = lambda a: np.ascontiguousarray(np.asarray(a, dtype=np.float32))
    x_prompt, x_sample, state_ret, cache_na_k, cache_na_v = map(f, (x_prompt, x_sample, state_ret, cache_na_k, cache_na_v))
    c, c_ctx = f(c), f(c_ctx)
    w_rt = np.concatenate([f(w_rg), f(w_re).transpose(0, 2, 1, 3).reshape(2, D, 32)], axis=2)
    b_rt = np.concatenate([f(b_rg), f(b_re).reshape(2, 32)], axis=1)
    shared = {
        "consts": _consts(), "w_mod0": f(w_mod)[0], "w_mod1": f(w_mod)[1], "b_mod": f(b_mod), "ln1_g": f(ln1_g), "ln1_b": f(ln1_b), "ln2_g": f(ln2_g), "ln2_b": f(ln2_b),
        "w_ret_in": f(w_ret_in)[0], "decay": f(ret_decay_logit).reshape(1, 16), "gn_g": f(ret_gn_g)[0], "gn_b": f(ret_gn_b)[0],
        "w_ret_out": f(w_ret_out)[0], "w_na_in": f(w_na_in)[0], "w_na_out": f(w_na_out)[0], "w_rt": np.ascontiguousarray(w_rt), "b_rt": np.ascontiguousarray(b_rt),
        "w_gate0": f(w_gate)[0], "w_gate1": f(w_gate)[1], "w_up0": f(w_up)[0], "w_up1": f(w_up)[1],
        "w_down0": f(w_down)[0], "w_down1": f(w_down)[1],
    }
    rpb = f(na_rpb)[0]
    in_maps = []
    for core in range(8):
        smp = core >= 4
        m = dict(shared)
        if smp:
            b = core - 4
            m["x_in"] = x_sample[b]
            cv_ = c[b]
            m["s0"] = np.ascontiguousarray(state_ret[b, 0])
            m["ck"] = np.ascontiguousarray(cache_na_k[b, 0]); m["cv"] = np.ascontiguousarray(cache_na_v[b, 0])
        else:
            m["x_in"] = np.ascontiguousarray(x_prompt[4 * core:4 * core + 4].reshape(NTOK, D))
            cv_ = c_ctx
            m["s0"] = np.zeros((2, 8, 256, 512), np.float32)
            m["ck"] = np.zeros((16, 512, 128), np.float32); m["cv"] = np.zeros((16, 512, 128), np.float32)
        m["cvec"] = np.ascontiguousarray(cv_.reshape(16, 128).T)
        fl = np.zeros((1, 8), np.float32)
        fl[0, 0] = 1.0 if smp else 0.0
        fl[0, 1] = 0.0 if smp else NEG
        m["flags"] = fl
        m["ropec"], m["ropes"] = _rope_tables(smp)
        m["nabias"], m["narow"] = _na_tables(smp, rpb)
        in_maps.append(m)
    return in_maps


def _assemble(R_):
    y_prompt = np.concatenate([R_[i]["y_out"].reshape(4, 256, D) for i in range(4)], axis=0)
    y_sample = np.stack([R_[4 + i]["y_out"] for i in range(4)], axis=0)
    new_state = np.concatenate([R_[i]["st_out"] for i in range(4)], axis=0)[:, None]
    def kv(name):
        parts = []
        for i in range(4):
            a = R_[i][name].reshape(16, 4, 256, 128).transpose(1, 0, 2, 3)
            parts.append(a)
        return np.ascontiguousarray(np.concatenate(parts, axis=0)[:, None])
    return (np.ascontiguousarray(y_prompt), np.ascontiguousarray(y_sample), np.ascontiguousarray(new_state), kv("k_out"), kv("v_out"))


def kernel(**inputs):
    in_maps = _make_in_maps(**inputs)
    if "nc" not in _NC_CACHE:
        _NC_CACHE["nc"] = build()
    res = run_bass_kernel_spmd(_NC_CACHE["nc"], in_maps, core_ids=list(range(8)))
    return _assemble(res.results)
```

```python
from contextlib import ExitStack
import numpy as np
import concourse.bass as bass
import concourse.mybir as mybir
from concourse.bass_utils import run_bass_kernel_spmd

F32 = mybir.dt.float32
BF16 = mybir.dt.bfloat16
AF = mybir.ActivationFunctionType
ALU = mybir.AluOpType

D = 2048
NTOK = 1024
NT = 8
NCH = 16
ALPHA = (2.0 * 2) ** 0.25
EPS = 1e-5
NEG = -1e30
MOE_STAGE = 3


class Res:
    __slots__ = ("name", "last_w", "readers")

    def __init__(self, name=""):
        self.name = name
        self.last_w = None
        self.readers = {}


class Op:
    __slots__ = ("eng", "fn", "deps", "signal", "count", "is_dma", "dma_key", "epoch")

    def __init__(self, eng, fn):
        self.epoch = 0
        self.eng = eng
        self.fn = fn
        self.deps = []
        self.signal = False
        self.count = 0
        self.is_dma = False
        self.dma_key = None


ENGINES = ("pe", "act", "dve", "pool", "sp")


class Sched:
    def __init__(self, nc):
        self.nc = nc
        self.ops = {e: [] for e in ENGINES}
        self.dma_count = {}
        self.dma_kind = {}
        self.last_dma = {}
        self.epoch = 0

    def _track(self, op, reads, writes):
        op.epoch = self.epoch
        deps = []
        for r in reads:
            if r.last_w is not None:
                deps.append(r.last_w)
        for w in writes:
            if w.last_w is not None:
                deps.append(w.last_w)
            deps.extend(w.readers.values())
        seen = set()
        for d in deps:
            if d is op or id(d) in seen:
                continue
            seen.add(id(d))
            if d.eng == "pe" and op.eng == "pe" and not d.is_dma and not op.is_dma:
                continue
            if d.epoch < self.epoch:
                continue
            op.deps.append((d, self.dma_count[d.dma_key] if d.is_dma else None))
            d.signal = True
        for r in reads:
            r.readers[op.dma_key if op.is_dma else op.eng] = op
        for w in writes:
            w.last_w = op
            w.readers = {}

    def add(self, eng, fn, reads=(), writes=()):
        op = Op(eng, fn)
        self._track(op, reads, writes)
        self.ops[eng].append(op)
        return op

    def dma(self, eng, fn, reads=(), writes=(), key="d"):
        kind = "sw" if eng == "pool" else "hw"
        key = key + "_" + kind
        self.dma_kind[key] = kind
        op = Op(eng, fn)
        op.is_dma = True
        op.dma_key = key
        self.dma_count.setdefault(key, 0)
        self._track(op, reads, writes)
        self.dma_count[key] += 1
        op.count = self.dma_count[key]
        self.ops[eng].append(op)
        self.last_dma[key] = op
        return op

    def barrier(self):
        lasts = []
        for e in ENGINES:
            for op in reversed(self.ops[e]):
                if not op.is_dma:
                    lasts.append(op)
                    break
        lasts.extend(self.last_dma.values())
        for e in ENGINES:
            op = Op(e, lambda eng: eng.nop(nofuse=True))
            for d in lasts:
                if d.eng == e and not d.is_dma:
                    continue
                op.deps.append((d, self.dma_count[d.dma_key] if d.is_dma else None))
                d.signal = True
            op.epoch = self.epoch
            self.ops[e].append(op)
        self.epoch += 1

    def emit(self, stack, block):
        nc = self.nc
        sems = {(e, ep): stack.enter_context(nc.semaphore("s_%s_%d" % (e, ep))) for e in ENGINES for ep in range(self.epoch + 1)}
        dsems = {k: stack.enter_context(nc.semaphore("d_" + k)) for k in self.dma_kind}
        for e in ENGINES:
            c = {}
            for op in self.ops[e]:
                if not op.is_dma and op.signal:
                    c[op.epoch] = c.get(op.epoch, 0) + 1
                    op.count = c[op.epoch]
            print("sched", e, "signals per epoch", c)

        def run(e, engine):
            known = {}
            for op in self.ops[e]:
                need = {}
                for d, dc_ in op.deps:
                    if d.is_dma:
                        s, v = dsems[d.dma_key], 16 * dc_
                    else:
                        s, v = sems[(d.eng, d.epoch)], d.count
                    k = id(s)
                    if k not in need or need[k][1] < v:
                        need[k] = (s, v)
                for k, (s, v) in need.items():
                    if known.get(k, 0) >= v:
                        continue
                    engine.wait_ge(s, v)
                    known[k] = v
                ins = op.fn(engine)
                if op.is_dma:
                    ins.then_inc(dsems[op.dma_key], 16)
                elif op.signal:
                    ins.then_inc(sems[(e, op.epoch)], 1)
            if e in ("pool", "sp"):
                kind = "sw" if e == "pool" else "hw"
                for k, kd in self.dma_kind.items():
                    if kd == kind:
                        engine.wait_ge(dsems[k], 16 * self.dma_count[k])

        block.tensor(lambda eng: run("pe", eng))
        block.scalar(lambda eng: run("act", eng))
        block.vector(lambda eng: run("dve", eng))
        block.gpsimd(lambda eng: run("pool", eng))
        block.sync(lambda eng: run("sp", eng))


def build(upto=6):
    nc = bass.Bass("TRN2", target_bir_lowering=False)

    def din(name, shape, dt=F32):
        return nc.dram_tensor(name, list(shape), dt, kind="ExternalInput").ap()

    def dout(name, shape, dt=F32):
        return nc.dram_tensor(name, list(shape), dt, kind="ExternalOutput").ap()

    def dint(name, shape, dt=F32):
        return nc.dram_tensor(name, list(shape), dt).ap()

    x_in = din("x_in", [NTOK, D])
    cvec = din("cvec", [128, 16])
    s0 = din("s0", [2, 8, 256, 512])
    flags = din("flags", [1, 8])
    ropec = din("ropec", [64, NCH, 256])
    ropes = din("ropes", [64, NCH, 256])
    consts = din("consts", [128, 1024])
    ck = din("ck", [16, 512, 128])
    cv = din("cv", [16, 512, 128])
    nabias = din("nabias", [64, 16, 15, 64])
    narow = din("narow", [1, 16, 16, 64])
    w_mod = [din("w_mod%d" % i, [D, 6 * D]) for i in range(2)]
    b_mod = din("b_mod", [2, 6 * D])
    ln1_g = din("ln1_g", [2, D]); ln1_b = din("ln1_b", [2, D])
    ln2_g = din("ln2_g", [2, D]); ln2_b = din("ln2_b", [2, D])
    w_ret_in = din("w_ret_in", [D, 16384])
    decay = din("decay", [1, 16])
    gn_g = din("gn_g", [8, 512]); gn_b = din("gn_b", [8, 512])
    w_ret_out = din("w_ret_out", [4096, D])
    w_na_in = din("w_na_in", [D, 3 * D])
    w_na_out = din("w_na_out", [D, D])
    w_rt = din("w_rt", [2, D, 36])
    b_rt = din("b_rt", [2, 36])
    w_gate = [din("w_gate%d" % i, [32, D, 512]) for i in range(2)]
    w_up = [din("w_up%d" % i, [32, D, 512]) for i in range(2)]
    w_down = [din("w_down%d" % i, [32, 512, D]) for i in range(2)]

    y_out = dout("y_out", [NTOK, D])
    st_out = dout("st_out", [4, 2, 8, 256, 512])
    k_out = dout("k_out", [16, NTOK, 128])
    v_out = dout("v_out", [16, NTOK, 128])

    m_dram = dint("m_dram", [2, 6 * D])
    xs = dint("xs", [NTOK, D])
    yT_dram = dint("yT_dram", [128, 32, NTOK], BF16)

    S = Sched(nc)
    stack = ExitStack()
    E = stack.enter_context

    AW = 51200
    arena = E(nc.sbuf_tensor("arena", [128, AW], F32))
    ident = E(nc.sbuf_tensor("ident", [128, 128], F32))
    identb = E(nc.sbuf_tensor("identb", [128, 128], BF16))
    onesf = E(nc.sbuf_tensor("onesf", [128, 128], F32))
    onesb = E(nc.sbuf_tensor("onesb", [1, 64], BF16))
    cst = E(nc.sbuf_tensor("cst", [128, 1024], F32))
    sil = E(nc.sbuf_tensor("sil", [128, 16], F32))
    flg = E(nc.sbuf_tensor("flg", [128, 8], F32))
    lg = E(nc.sbuf_tensor("lg", [128, 16], F32))
    small = E(nc.sbuf_tensor("small", [128, 64], F32))
    stt = E(nc.sbuf_tensor("stt", [128, 4, 6], F32))
    comb = E(nc.sbuf_tensor("comb", [128, NT, 32], F32))
    rl = E(nc.sbuf_tensor("rl", [128, 96], F32))
    rbias = E(nc.sbuf_tensor("rbias", [128, 2, 36], F32))

    ps = [E(nc.psum_tensor("ps%d" % i, [128, 512], F32)) for i in range(7)]
    psb = E(nc.psum_tensor("psb", [128, 1024], BF16))
    PS = [Res("ps%d" % i) for i in range(7)]
    PSB = Res("psb")

    R = {}

    def res(name):
        if name not in R:
            R[name] = Res(name)
        return R[name]

    class Arena:
        def __init__(self):
            self.off = 0

        def reset(self):
            self.off = 0

        def f32(self, shape):
            n = int(np.prod(shape[1:]))
            v = arena[0:shape[0], self.off:self.off + n]
            self.off += n
            assert self.off <= AW, ("arena overflow", self.off)
            return v if len(shape) == 2 else v.rearrange(
                "p (a b) -> p a b", b=shape[2]) if len(shape) == 3 else v.rearrange(
                "p (a b c) -> p a b c", b=shape[2], c=shape[3])

        def bf16(self, shape):
            n = int(np.prod(shape[1:]))
            assert n % 2 == 0
            v = arena[0:shape[0], self.off:self.off + n // 2].bitcast(BF16)
            self.off += n // 2
            assert self.off <= AW, ("arena overflow", self.off)
            return v if len(shape) == 2 else v.rearrange(
                "p (a b) -> p a b", b=shape[2]) if len(shape) == 3 else v.rearrange(
                "p (a b c) -> p a b c", b=shape[2], c=shape[3])

    A = Arena()

    S.dma("sp", lambda e: e.dma_start(out=cst[:], in_=consts[:, :]), writes=[res("cst")], key="c0")
    S.dma("sp", lambda e: e.dma_start(out=sil[:], in_=cvec[:, :]), writes=[res("sil")], key="c0")
    S.dma("sp", lambda e: e.dma_start(out=flg[:], in_=flags[0, :].partition_broadcast(128)), writes=[res("flg")], key="c0")
    S.dma("sp", lambda e: e.dma_start(out=lg[:], in_=decay[0, :].partition_broadcast(128)), writes=[res("lg")], key="c0")
    S.dma("sp", lambda e: e.dma_start(out=rbias[:, 0, :], in_=b_rt[0, :].partition_broadcast(128)), writes=[res("rbias")], key="c0")
    S.dma("sp", lambda e: e.dma_start(out=rbias[:, 1, :], in_=b_rt[1, :].partition_broadcast(128)), writes=[res("rbias")], key="c0")
    S.add("dve", lambda e: e.tensor_copy(ident[:], cst[:, 0:128]), reads=[res("cst")], writes=[res("ident")])
    S.add("dve", lambda e: e.tensor_copy(identb[:], cst[:, 0:128]), reads=[res("cst")], writes=[res("identb")])
    S.add("dve", lambda e: e.tensor_copy(onesf[:], cst[:, 128:256]), reads=[res("cst")], writes=[res("onesf")])
    S.add("dve", lambda e: e.tensor_copy(onesb[:], cst[0:1, 128:192]), reads=[res("cst")], writes=[res("onesb")])
    S.add("act", lambda e: e.activation(sil[:], sil[:], AF.Silu), reads=[res("sil")], writes=[res("sil")])
    S.add("act", lambda e: e.activation(lg[:], lg[:], AF.Sigmoid), reads=[res("lg")], writes=[res("lg")])
    S.add("act", lambda e: e.activation(lg[:], lg[:], AF.Ln), reads=[res("lg")], writes=[res("lg")])

    A.reset()
    macc = A.f32([128, D])
    mbias = A.f32([128, D])
    mring = [A.f32([128, D]) for _ in range(3)]
    MR = [Res("mring%d" % i) for i in range(3)]
    pi = 0
    for l in range(2):
        for cb in range(6):
            S.dma("sp", lambda e, l=l, cb=cb: e.dma_start(out=mbias[:], in_=b_mod[l, cb * D:(cb + 1) * D].partition_broadcast(128)),
                  writes=[res("mbias")], key="mb")
            for kc in range(16):
                slot = pi % 3
                pi += 1
                S.dma("sp", lambda e, l=l, cb=cb, kc=kc, slot=slot: e.dma_start(
                    out=mring[slot][:], in_=w_mod[l][kc * 128:(kc + 1) * 128, cb * D:(cb + 1) * D]),
                    writes=[MR[slot]], key="mr%d" % slot)
                if kc == 0:
                    S.add("dve", lambda e, kc=kc, slot=slot: e.tensor_scalar(macc[:], mring[slot][:], sil[:, kc:kc + 1], None, op0=ALU.mult),
                          reads=[MR[slot], res("sil")], writes=[res("macc")])
                else:
                    S.add("dve", lambda e, kc=kc, slot=slot: e.scalar_tensor_tensor(
                        macc[:], mring[slot][:], sil[:, kc:kc + 1], macc[:], op0=ALU.mult, op1=ALU.add),
                        reads=[MR[slot], res("sil"), res("macc")], writes=[res("macc")])
            for j in range(4):
                S.add("pe", lambda e, j=j: e.matmul(ps[j][:], onesf[:], macc[:, j * 512:(j + 1) * 512], start=True, stop=True),
                      reads=[res("onesf"), res("macc")], writes=[PS[j]])
                S.add("dve", lambda e, j=j: e.tensor_tensor(mbias[:, j * 512:(j + 1) * 512], ps[j][:], mbias[:, j * 512:(j + 1) * 512], op=ALU.add),
                      reads=[PS[j], res("mbias")], writes=[res("mbias")])
            S.dma("sp", lambda e, l=l, cb=cb: e.dma_start(out=m_dram[l:l + 1, cb * D:(cb + 1) * D], in_=mbias[0:1, :]),
                  reads=[res("mbias")], writes=[res("m_dram")], key="mo")
    S.barrier()

    def bc_load(dst, src_row, r, key):
        S.dma("sp", lambda e: e.dma_start(out=dst, in_=src_row.partition_broadcast(128)), reads=[res("m_dram")], writes=[r], key=key)

    ev_flip = [0]

    def evac(dst, src, rd, wr, scale=None):
        ev_flip[0] ^= 1
        if ev_flip[0]:
            if scale is None:
                S.add("act", lambda e: e.copy(dst, src), reads=rd, writes=wr)
            else:
                S.add("act", lambda e: e.mul(dst, src, scale), reads=rd, writes=wr)
        else:
            if scale is None:
                S.add("dve", lambda e: e.tensor_copy(dst, src), reads=rd, writes=wr)
            else:
                S.add("dve", lambda e: e.tensor_scalar(dst, src, scale, None, op0=ALU.mult), reads=rd, writes=wr)

    def to_feature_major(h_ap, h_res, hT, t, hT32=None):
        for g in range(4):
            b = g % 4
            for q in range(4):
                kc = g * 4 + q
                S.add("pe", lambda e, kc=kc, b=b, q=q: e.transpose(ps[b][:, q * 128:(q + 1) * 128], h_ap[:, kc * 128:(kc + 1) * 128], ident[:]),
                      reads=[h_res, res("ident")], writes=[PS[b]])
            evac(hT[:, g * 4:(g + 1) * 4, t * 128:(t + 1) * 128], ps[b][:].rearrange("p (a b) -> p a b", b=128),
                 [PS[b]], [res("hT%d" % t), res("pstok%d" % b)])
            if hT32 is not None:
                S.add("dve", lambda e, g=g, b=b: e.tensor_copy(hT32[:, g * 4:(g + 1) * 4, :], ps[b][:].rearrange("p (a b) -> p a b", b=128)),
                      reads=[PS[b]], writes=[res("hT32"), res("pstok%d" % b)])

    def layer_norm_tile(u, u_res, g_bc, b_bc, out_ap, out_res, rd):
        for j in range(4):
            S.add("dve", lambda e, j=j: e.bn_stats(stt[:, j, :], u[:, j * 512:(j + 1) * 512]), reads=[u_res], writes=[res("stt")])
        S.add("dve", lambda e: e.bn_aggr(small[:, 0:2], stt[:]), reads=[res("stt")], writes=[res("small")])
        S.add("dve", lambda e: e.tensor_scalar(small[:, 2:3], small[:, 1:2], EPS, None, op0=ALU.add), reads=[res("small")], writes=[res("small")])
        S.add("act", lambda e: e.activation(small[:, 2:3], small[:, 2:3], AF.Ln), reads=[res("small")], writes=[res("small")])
        S.add("act", lambda e: e.activation(small[:, 2:3], small[:, 2:3], AF.Exp, scale=-0.5), reads=[res("small")], writes=[res("small")])
        S.add("dve", lambda e: e.tensor_scalar(u, u, small[:, 0:1], small[:, 2:3], op0=ALU.subtract, op1=ALU.mult),
              reads=[u_res, res("small")], writes=[u_res])
        S.add("dve", lambda e: e.tensor_tensor(u, u, g_bc, op=ALU.mult), reads=[u_res] + rd, writes=[u_res])
        S.add("dve", lambda e: e.tensor_tensor(out_ap, u, b_bc, op=ALU.add), reads=[u_res] + rd, writes=[out_res])

    def stream_w(slot_ap, src_ap, slot_res, key):
        S.dma("pool", lambda e: e.dma_start(out=slot_ap, in_=src_ap), writes=[slot_res], key=key)

    def prologue(l, x_src, hT, j_shift, j_scale):
        sc = A.f32([128, D]); sh = A.f32([128, D])
        xw = [A.f32([128, D]) for _ in range(2)]
        bc_load(sc, m_dram[l, j_scale * D:(j_scale + 1) * D], res("sc"), "bc")
        bc_load(sh, m_dram[l, j_shift * D:(j_shift + 1) * D], res("sh"), "bc")
        S.add("dve", lambda e: e.tensor_scalar(sc, sc, 1.0, None, op0=ALU.add), reads=[res("sc")], writes=[res("sc")])
        for t in range(NT):
            xt = xw[t % 2]
            xr = res("xw%d" % (t % 2))
            S.dma("sp", lambda e, t=t, xt=xt: e.dma_start(out=xt, in_=x_src[t * 128:(t + 1) * 128, :]), reads=[res("xs")], writes=[xr], key="xl%d" % (t % 2))
            S.add("dve", lambda e, xt=xt: e.tensor_tensor(xt, xt, sc, op=ALU.mult), reads=[xr, res("sc")], writes=[xr])
            S.add("dve", lambda e, xt=xt: e.tensor_tensor(xt, xt, sh, op=ALU.add), reads=[xr, res("sh")], writes=[xr])
            to_feature_major(xt, xr, hT, t)

    def epilogue(l, x_src, o_half, th, gate_j, lng, lnb, next_mod, hT_next, want_router, x_dst, l_router):
        pass

    def moe_block(l, x_src, x_dst, next_l):
        A.reset()
        hT = A.bf16([128, 16, NTOK])
        yacc = A.f32([128, NT, D])
        hid = A.bf16([128, 4, NTOK])
        ring = [A.bf16([128, 8192]) for _ in range(3)]
        RG = [Res("ring%d" % i) for i in range(3)]
        sgt = [A.bf16([128, 512]) for _ in range(2)]
        mark = A.off
        sc = A.f32([128, D]); sh = A.f32([128, D])
        xw = [A.f32([128, D])] * 2
        hT32 = A.f32([128, 16, 128])
        wr32 = A.f32([128, 16, 36])
        bc_load(sc, m_dram[l, 4 * D:5 * D], res("sc"), "bc")
        bc_load(sh, m_dram[l, 3 * D:4 * D], res("sh"), "bc")
        S.add("dve", lambda e: e.tensor_scalar(sc, sc, 1.0, None, op0=ALU.add), reads=[res("sc")], writes=[res("sc")])
        S.dma("sp", lambda e: e.dma_start(out=wr32, in_=w_rt[l].rearrange("(kc p) c -> p kc c", p=128)), writes=[res("wr32")], key="wr")
        for t in range(NT):
            xt = xw[0]
            xr = res("xw0")
            S.dma("sp", lambda e, t=t, xt=xt: e.dma_start(out=xt, in_=x_src[t * 128:(t + 1) * 128, :]), reads=[res("xs")], writes=[xr], key="xl0")
            S.add("dve", lambda e, xt=xt: e.tensor_tensor(xt, xt, sc, op=ALU.mult), reads=[xr, res("sc")], writes=[xr])
            S.add("dve", lambda e, xt=xt: e.tensor_tensor(xt, xt, sh, op=ALU.add), reads=[xr, res("sh")], writes=[xr])
            to_feature_major(xt, xr, hT, t, hT32=(hT32 if MOE_STAGE >= 0.5 else None))
            if MOE_STAGE < 1:
                continue
            for kc in range(16):
                S.add("pe", lambda e, kc=kc: e.matmul(ps[4][:, 0:36], hT32[:, kc, :], wr32[:, kc, :], start=(kc == 0), stop=(kc == 15)),
                      reads=[res("hT32"), res("wr32")], writes=[PS[4]])
            rr = res("rl")
            S.add("dve", lambda e: e.tensor_tensor(rl[:, 0:36], ps[4][:, 0:36], rbias[:, l, :], op=ALU.add), reads=[PS[4], res("rbias")], writes=[rr])
            S.add("dve", lambda e: e.tensor_reduce(small[:, 8:9], rl[:, 0:4], mybir.AxisListType.X, ALU.max), reads=[rr], writes=[res("sm_g")])
            S.add("dve", lambda e: e.tensor_scalar(rl[:, 40:44], rl[:, 0:4], small[:, 8:9], None, op0=ALU.is_equal), reads=[rr, res("sm_g")], writes=[rr])
            S.add("dve", lambda e: e.tensor_scalar(rl[:, 44:48], rl[:, 0:4], small[:, 8:9], None, op0=ALU.subtract), reads=[rr, res("sm_g")], writes=[rr])
            S.add("act", lambda e: e.activation(rl[:, 44:48], rl[:, 44:48], AF.Exp, accum_out=small[:, 9:10]), reads=[rr], writes=[rr, res("sm_g2")])
            S.add("dve", lambda e: e.reciprocal(small[:, 10:11], small[:, 9:10]), reads=[res("sm_g2")], writes=[res("sm_pg")])
            S.add("dve", lambda e: e.tensor_scalar(rl[:, 40:44], rl[:, 40:44], 1.0, 1e30, op0=ALU.subtract, op1=ALU.mult), reads=[rr], writes=[rr])
            for g_ in range(4):
                S.add("dve", lambda e, g_=g_: e.tensor_scalar(rl[:, 48 + 8 * g_:56 + 8 * g_], rl[:, 4 + 8 * g_:12 + 8 * g_], rl[:, 40 + g_:41 + g_], None, op0=ALU.add),
                      reads=[rr], writes=[rr])
            S.add("dve", lambda e: e.tensor_reduce(small[:, 11:12], rl[:, 48:80], mybir.AxisListType.X, ALU.max), reads=[rr], writes=[res("sm_m1")])
            S.add("dve", lambda e, t=t: e.tensor_scalar(comb[:, t, :], rl[:, 48:80], small[:, 11:12], None, op0=ALU.is_equal), reads=[rr, res("sm_m1")], writes=[res("comb")])
            S.add("dve", lambda e, t=t: e.scalar_tensor_tensor(rl[:, 48:80], comb[:, t, :], -1e30, rl[:, 48:80], op0=ALU.mult, op1=ALU.add),
                  reads=[rr, res("comb")], writes=[rr])
            S.add("dve", lambda e: e.tensor_reduce(small[:, 12:13], rl[:, 48:80], mybir.AxisListType.X, ALU.max), reads=[rr], writes=[res("sm_m2")])
            S.add("dve", lambda e: e.tensor_scalar(rl[:, 4:36], rl[:, 48:80], small[:, 12:13], None, op0=ALU.is_equal), reads=[rr, res("sm_m2")], writes=[rr])
            S.add("dve", lambda e: e.tensor_tensor(small[:, 13:14], small[:, 12:13], small[:, 11:12], op=ALU.subtract), reads=[res("sm_m1"), res("sm_m2")], writes=[res("sm_w")])
            S.add("act", lambda e: e.activation(small[:, 13:14], small[:, 13:14], AF.Exp), reads=[res("sm_w")], writes=[res("sm_w")])
            S.add("dve", lambda e: e.tensor_scalar(small[:, 14:15], small[:, 13:14], 1.0, None, op0=ALU.add), reads=[res("sm_w")], writes=[res("sm_d")])
            S.add("dve", lambda e: e.reciprocal(small[:, 14:15], small[:, 14:15]), reads=[res("sm_d")], writes=[res("sm_d")])
            S.add("dve", lambda e: e.tensor_tensor(small[:, 15:16], small[:, 14:15], small[:, 10:11], op=ALU.mult), reads=[res("sm_d"), res("sm_pg")], writes=[res("sm_t1")])
            S.add("dve", lambda e: e.tensor_tensor(small[:, 16:17], small[:, 15:16], small[:, 13:14], op=ALU.mult), reads=[res("sm_t1"), res("sm_w")], writes=[res("sm_t2")])
            S.add("dve", lambda e, t=t: e.tensor_scalar(comb[:, t, :], comb[:, t, :], small[:, 15:16], None, op0=ALU.mult), reads=[res("comb"), res("sm_t1")], writes=[res("comb")])
            S.add("dve", lambda e, t=t: e.scalar_tensor_tensor(comb[:, t, :], rl[:, 4:36], small[:, 16:17], comb[:, t, :], op0=ALU.mult, op1=ALU.add),
                  reads=[rr, res("sm_t2"), res("comb")], writes=[res("comb")])
        if MOE_STAGE < 2:
            S.barrier()
            return
        HT = [res("hT%d" % t) for t in range(NT)]
        si = [0]

        def nxt():
            s = si[0] % 3
            si[0] += 1
            return s
        for ex in range(32):
            sg_, su_, sd_ = nxt(), nxt(), nxt()
            G = ring[sg_].rearrange("p (k c) -> p k c", c=512)
            U = ring[su_].rearrange("p (k c) -> p k c", c=512)
            Dw = ring[sd_].rearrange("p (k c) -> p k c", c=D)
            stream_w(G, w_gate[l][ex].rearrange("(kc p) c -> p kc c", p=128), RG[sg_], "rg%d" % sg_)
            stream_w(U, w_up[l][ex].rearrange("(kc p) c -> p kc c", p=128), RG[su_], "rg%d" % su_)
            stream_w(Dw, w_down[l][ex].rearrange("(kc p) c -> p kc c", p=128), RG[sd_], "rg%d" % sd_)
            it = 0
            for th in range(2):
                for fc in range(4):
                    bg, bu = (0, 1) if it % 2 == 0 else (2, 3)
                    sgi = it % 2
                    it += 1
                    for kc in range(16):
                        S.add("pe", lambda e, kc=kc, fc=fc, th=th, bg=bg, G=G: e.matmul(ps[bg][:], G[:, kc, fc * 128:(fc + 1) * 128], hT[:, kc, th * 512:(th + 1) * 512],
                                                                                 start=(kc == 0), stop=(kc == 15)),
                              reads=[RG[sg_]] + HT[th * 4:(th + 1) * 4], writes=[PS[bg]])
                    for kc in range(16):
                        S.add("pe", lambda e, kc=kc, fc=fc, th=th, bu=bu, U=U: e.matmul(ps[bu][:], U[:, kc, fc * 128:(fc + 1) * 128], hT[:, kc, th * 512:(th + 1) * 512],
                                                                                 start=(kc == 0), stop=(kc == 15)),
                              reads=[RG[su_]] + HT[th * 4:(th + 1) * 4], writes=[PS[bu]])
                    S.add("act", lambda e, bg=bg, sgi=sgi: e.activation(sgt[sgi], ps[bg][:], AF.Silu), reads=[PS[bg]], writes=[res("sgt%d" % sgi)])
                    S.add("dve", lambda e, bu=bu, sgi=sgi, fc=fc, th=th: e.tensor_tensor(hid[:, fc, th * 512:(th + 1) * 512], sgt[sgi], ps[bu][:], op=ALU.mult),
                          reads=[PS[bu], res("sgt%d" % sgi)], writes=[res("hid%d" % th)])
            it = 0
            for t in range(NT):
                for dc in range(4):
                    b = 4 + (it % 3)
                    it += 1
                    for fc in range(4):
                        S.add("pe", lambda e, fc=fc, t=t, dc=dc, b=b, Dw=Dw: e.matmul(ps[b][:], hid[:, fc, t * 128:(t + 1) * 128], Dw[:, fc, dc * 512:(dc + 1) * 512],
                                                                               start=(fc == 0), stop=(fc == 3)),
                              reads=[RG[sd_], res("hid%d" % (t // 4))], writes=[PS[b]])
                    yr = res("yacc%d" % t)
                    if ex == 0:
                        S.add("dve", lambda e, t=t, dc=dc, b=b, ex=ex: e.tensor_scalar(yacc[:, t, dc * 512:(dc + 1) * 512], ps[b][:], comb[:, t, ex:ex + 1], None, op0=ALU.mult),
                              reads=[PS[b], res("comb")], writes=[yr])
                    else:
                        S.add("dve", lambda e, t=t, dc=dc, b=b, ex=ex: e.scalar_tensor_tensor(yacc[:, t, dc * 512:(dc + 1) * 512], ps[b][:], comb[:, t, ex:ex + 1],
                                                                                              yacc[:, t, dc * 512:(dc + 1) * 512], op0=ALU.mult, op1=ALU.add),
                              reads=[PS[b], res("comb"), yr], writes=[yr])
        if MOE_STAGE < 3:
            S.barrier()
            return
        gt = sc; lgt = sh
        lbt = hT32.rearrange("p a b -> p (a b)")
        bc_load(gt, m_dram[l, 5 * D:6 * D], res("sc"), "bc")
        S.dma("sp", lambda e: e.dma_start(out=lgt, in_=ln2_g[l, :].partition_broadcast(128)), writes=[res("sh")], key="bc")
        S.dma("sp", lambda e: e.dma_start(out=lbt, in_=ln2_b[l, :].partition_broadcast(128)), writes=[res("hT32")], key="bc")
        for t in range(NT):
            xt = xw[0]
            xr = res("xw0")
            yr = res("yacc%d" % t)
            S.dma("sp", lambda e, t=t, xt=xt: e.dma_start(out=xt, in_=x_src[t * 128:(t + 1) * 128, :]), reads=[res("xs")], writes=[xr], key="xl0")
            S.add("dve", lambda e, t=t: e.tensor_tensor(yacc[:, t, :], yacc[:, t, :], gt, op=ALU.mult), reads=[yr, res("sc")], writes=[yr])
            S.add("dve", lambda e, t=t, xt=xt: e.scalar_tensor_tensor(xt, xt, ALPHA, yacc[:, t, :], op0=ALU.mult, op1=ALU.add), reads=[yr, xr], writes=[xr])
            layer_norm_tile(xt, xr, lgt, lbt, xt, xr, [res("sh"), res("hT32")])
            S.dma("sp", lambda e, t=t, xt=xt: e.dma_start(out=x_dst[t * 128:(t + 1) * 128, :], in_=xt), reads=[xr], writes=[res("xdst")], key="xo")
        S.barrier()

    def outproj_block(l, KC, w_out_ap, x_src, x_dst):
        A.reset()
        CW = 8192 // KC
        npiece = D // CW
        yTh = A.bf16([128, KC, 512])
        oh = A.f32([128, 4, D])
        ring = [A.bf16([128, 8192]) for _ in range(3)]
        RG = [Res("oring%d" % i) for i in range(3)]
        gt = A.f32([128, D]); lgt = A.f32([128, D]); lbt = A.f32([128, D])
        xw = [A.f32([128, D]) for _ in range(2)]
        bc_load(gt, m_dram[l, 2 * D:3 * D], res("gt"), "bc")
        S.dma("sp", lambda e: e.dma_start(out=lgt, in_=ln1_g[l, :].partition_broadcast(128)), writes=[res("lgt")], key="bc")
        S.dma("sp", lambda e: e.dma_start(out=lbt, in_=ln1_b[l, :].partition_broadcast(128)), writes=[res("lbt")], key="bc")
        pc = 0
        for th in range(2):
            S.dma("sp", lambda e, th=th: e.dma_start(out=yTh, in_=yT_dram[:, 0:KC, th * 512:(th + 1) * 512]), reads=[res("yT_dram")], writes=[res("yTh")], key="yt")
            it = 0
            for p in range(npiece):
                slot = pc % 3
                pc += 1
                W = ring[slot].rearrange("p (k c) -> p k c", c=CW)
                stream_w(W, w_out_ap[:, p * CW:(p + 1) * CW].rearrange("(kc p) c -> p kc c", p=128), RG[slot], "or%d" % slot)
                for tt in range(4):
                    b = it % 4
                    it += 1
                    for kc in range(KC):
                        S.add("pe", lambda e, kc=kc, tt=tt, b=b, W=W: e.matmul(ps[b][:, 0:CW], yTh[:, kc, tt * 128:(tt + 1) * 128], W[:, kc, :], start=(kc == 0), stop=(kc == KC - 1)),
                              reads=[RG[slot], res("yTh")], writes=[PS[b]])
                    evac(oh[:, tt, p * CW:(p + 1) * CW], ps[b][:, 0:CW], [PS[b]], [res("oh%d" % tt)])
            for tt in range(4):
                t = th * 4 + tt
                xt = xw[t % 2]
                xr = res("xw%d" % (t % 2))
                orr = res("oh%d" % tt)
                S.dma("sp", lambda e, t=t, xt=xt: e.dma_start(out=xt, in_=x_src[t * 128:(t + 1) * 128, :]), reads=[res("xs")], writes=[xr], key="xl%d" % (t % 2))
                S.add("dve", lambda e, tt=tt: e.tensor_tensor(oh[:, tt, :], oh[:, tt, :], gt, op=ALU.mult), reads=[orr, res("gt")], writes=[orr])
                S.add("dve", lambda e, tt=tt, xt=xt: e.scalar_tensor_tensor(xt, xt, ALPHA, oh[:, tt, :], op0=ALU.mult, op1=ALU.add), reads=[orr, xr], writes=[xr])
                layer_norm_tile(xt, xr, lgt, lbt, xt, xr, [res("lgt"), res("lbt")])
                S.dma("sp", lambda e, t=t, xt=xt: e.dma_start(out=x_dst[t * 128:(t + 1) * 128, :], in_=xt), reads=[xr], writes=[res("xdst")], key="xo")
        S.barrier()

    def retention_block(l, x_src):
        A.reset()
        hT = A.bf16([128, 16, NTOK])
        mark0 = A.off
        prologue(l, x_src, hT, 0, 1)
        S.barrier()
        A.off = mark0
        HT = [res("hT%d" % t) for t in range(NT)]
        ring = [A.bf16([128, 8192]) for _ in range(2)]
        RG = [Res("rring%d" % i) for i in range(2)]
        QT = A.bf16([128, 2, NTOK]); KT = A.bf16([128, 2, NTOK])
        k_tm = A.bf16([64, NCH, 256]); v_tm = A.bf16([64, NCH, 512])
        sgf = A.bf16([64, NCH, 512]); sgb = A.bf16([64, NCH, 512])
        yf = A.bf16([64, NCH, 512])
        off_rc = A.off
        rc = A.bf16([64, NCH, 256]); rs = A.bf16([64, NCH, 256])
        yTh = arena[0:128, off_rc:off_rc + 2048].bitcast(BF16).rearrange("p (a b) -> p a b", b=NTOK)
        St2 = [A.f32([128, 2, 512]) for _ in range(2)]; Sb2 = [A.bf16([128, 2, 512]) for _ in range(2)]
        DT2 = [A.f32([64, 64]) for _ in range(2)]; qdec2 = [A.bf16([128, 2, 64]) for _ in range(2)]; kd2 = [A.f32([64, 2]) for _ in range(2)]
        gng = A.f32([64, 512]); gnb = A.f32([64, 512])
        t12 = [A.f32([64, 512]) for _ in range(2)]
        t1 = t12[0]; t2 = t12[1]
        qr = A.bf16([64, 256])
        Pm2 = [A.bf16([64, 64]) for _ in range(2)]; Qd2 = [A.bf16([128, 2, 64]) for _ in range(2)]; Kd2 = [A.bf16([64, 256]) for _ in range(2)]
        ybk = A.bf16([64, NCH, 512])
        S.dma("pool", lambda e: e.dma_start(out=rs, in_=ropes[:, :, :]), writes=[res("rs")], key="rt")
        pc = [0]

        def piece(c0, ncols):
            slot = pc[0] % 2
            pc[0] += 1
            W = ring[slot][:, 0:16 * ncols].rearrange("p (k c) -> p k c", c=ncols)
            stream_w(W, w_ret_in[:, c0:c0 + ncols].rearrange("(kc p) c -> p kc c", p=128), RG[slot], "rr%d" % slot)
            return W, RG[slot]

        def proj_chunk(W, Wr, c, ncols, b):
            for kc in range(16):
                S.add("pe", lambda e, kc=kc: e.matmul(ps[b][0:64, 0:ncols], hT[:, kc, c * 64:(c + 1) * 64], W[:, kc, :], start=(kc == 0), stop=(kc == 15)),
                      reads=[Wr, HT[c // 2]], writes=[PS[b]])

        def rope_to(dst, dst_res, c, b, kscale):
            p4 = ps[b][0:64, 0:256].rearrange("p (a h f) -> p a h f", a=2, h=2)
            S.add("dve", lambda e: e.tensor_tensor(t1[:, 0:256], ps[b][0:64, 0:256], rc[:, c, :], op=ALU.mult), reads=[PS[b], res("rc")], writes=[res("t1")])
            S.add("dve", lambda e: e.tensor_tensor(t2[:, 0:256].rearrange("p (a h f) -> p a h f", a=2, h=2), p4[:, :, ::-1, :],
                                                   rs[:, c, :].rearrange("p (a h f) -> p a h f", a=2, h=2), op=ALU.mult), reads=[PS[b], res("rs")], writes=[res("t2")])
            if kscale is None:
                S.add("dve", lambda e: e.tensor_tensor(dst, t1[:, 0:256], t2[:, 0:256], op=ALU.add), reads=[res("t1"), res("t2")], writes=[dst_res])
            else:
                S.add("dve", lambda e: e.tensor_tensor(t1[:, 0:256], t1[:, 0:256], t2[:, 0:256], op=ALU.add), reads=[res("t1"), res("t2")], writes=[res("t1")])
                S.add("dve", lambda e: e.tensor_scalar(dst, t1[:, 0:256], kscale, None, op0=ALU.mult), reads=[res("t1")], writes=[dst_res])

        def tr_to(dstT, dstT_res, src, src_res, c):
            for dc in range(2):
                S.add("pe", lambda e, dc=dc: e.transpose(psb[:, dc * 64:(dc + 1) * 64], src[:, dc * 128:(dc + 1) * 128], identb[0:64, 0:64]),
                      reads=[src_res, res("identb")], writes=[PSB])
            evac(dstT[:, :, c * 64:(c + 1) * 64], psb[:, 0:128].rearrange("p (a b) -> p a b", b=64), [PSB], [dstT_res])

        for h in range(8):
            S.dma("pool", lambda e: e.dma_start(out=rc, in_=ropec[:, :, :]), writes=[res("rc")], key="rtc")
            W, Wr = piece(h * 256, 256)
            for c in range(NCH):
                b = c % 2
                proj_chunk(W, Wr, c, 256, b)
                rope_to(qr, res("qr"), c, b, None)
                tr_to(QT, res("QT"), qr, res("qr"), c)
            W, Wr = piece(2048 + h * 256, 256)
            for c in range(NCH):
                b = c % 2
                proj_chunk(W, Wr, c, 256, b)
                rope_to(k_tm[:, c, :], res("k_tm"), c, b, 1.0 / 16.0)
                tr_to(KT, res("KT"), k_tm[:, c, :], res("k_tm"), c)
            W, Wr = piece(4096 + h * 512, 512)
            for c in range(NCH):
                b = c % 2
                proj_chunk(W, Wr, c, 512, b)
                evac(v_tm[:, c, :], ps[b][0:64, :], [PS[b]], [res("v_tm")])
            W, Wr = piece(8192 + h * 512, 512)
            for c in range(NCH):
                b = c % 2
                proj_chunk(W, Wr, c, 512, b)
                S.add("act", lambda e, c=c, b=b: e.activation(sgf[:, c, :], ps[b][0:64, :], AF.Silu), reads=[PS[b]], writes=[res("sgf")])
            W, Wr = piece(12288 + h * 512, 512)
            for c in range(NCH):
                b = c % 2
                proj_chunk(W, Wr, c, 512, b)
                S.add("act", lambda e, c=c, b=b: e.activation(sgb[:, c, :], ps[b][0:64, :], AF.Silu), reads=[PS[b]], writes=[res("sgb")])
            S.dma("sp", lambda e, h=h: e.dma_start(out=gng, in_=gn_g[h, :].partition_broadcast(64)), writes=[res("gng")], key="gn")
            S.dma("sp", lambda e, h=h: e.dma_start(out=gnb, in_=gn_b[h, :].partition_broadcast(64)), writes=[res("gnb")], key="gn")
            for dr in range(2):
                col = dr * 8 + h
                lgc = lg[:, col:col + 1]
                dif = cst[0:64, 384:448] if dr == 0 else cst[0:64, 512:576]
                tri = cst[0:64, 448:512] if dr == 0 else cst[0:64, 576:640]
                ramp = cst[:, 256:320] if dr == 0 else cst[:, 320:384]
                DTd, qdd, kdd = DT2[dr], qdec2[dr], kd2[dr]
                S.add("act", lambda e, dif=dif, lgc=lgc, DTd=DTd: e.activation(DTd, dif, AF.Exp, scale=lgc[0:64, :]), reads=[res("cst"), res("lg")], writes=[res("DT%d" % dr)])
                S.add("dve", lambda e, tri=tri, DTd=DTd: e.tensor_tensor(DTd, DTd, tri, op=ALU.mult), reads=[res("DT%d" % dr), res("cst")], writes=[res("DT%d" % dr)])
                for dc in range(2):
                    S.add("act", lambda e, dc=dc, ramp=ramp, lgc=lgc, qdd=qdd: e.activation(qdd[:, dc, :], ramp, AF.Exp, scale=lgc), reads=[res("cst"), res("lg")], writes=[res("qdec%d" % dr)])
                S.add("act", lambda e, dr=dr, lgc=lgc, kdd=kdd: e.activation(kdd[:, 0:1], cst[0:64, 640 + dr:641 + dr], AF.Exp, scale=lgc[0:64, :]), reads=[res("cst"), res("lg")], writes=[res("kd%d" % dr)])
                S.add("act", lambda e, dr=dr, lgc=lgc: e.activation(small[:, 20 + dr:21 + dr], cst[:, 642:643], AF.Exp, scale=lgc), reads=[res("cst"), res("lg")], writes=[res("cdec%d" % dr)])
                S.dma("sp", lambda e, dr=dr, h=h: e.dma_start(out=St2[dr], in_=s0[dr, h].rearrange("(dc p) v -> p dc v", p=128)), writes=[res("St%d" % dr)], key="s0%d" % dr)
            for idx in range(NCH):
                for dr in range(2):
                    c = idx if dr == 0 else NCH - 1 - idx
                    Std, Sbd, DTd, qdd, kdd = St2[dr], Sb2[dr], DT2[dr], qdec2[dr], kd2[dr]
                    Pmd, Qdd, Kdd, t1d = Pm2[dr], Qd2[dr], Kd2[dr], t12[dr]
                    bI, bO, bK = (2, 3, 4) if dr == 0 else (5, 6, 0)
                    sm0 = 0 if dr == 0 else 4
                    rSt, rSb, rSm, rT1 = res("St%d" % dr), res("Sb%d" % dr), res("small%d" % dr), res("t1%d" % dr)
                    if idx % 4 == 0 and idx > 0:
                        S.add("dve", lambda e, Std=Std: e.tensor_scalar(Std, Std, flg[:, 0:1], None, op0=ALU.mult), reads=[rSt, res("flg")], writes=[rSt])
                    for dc in range(2):
                        S.add("pe", lambda e, dc=dc, c=c, bI=bI: e.matmul(ps[bI][0:64, 0:64], KT[:, dc, c * 64:(c + 1) * 64], QT[:, dc, c * 64:(c + 1) * 64], start=(dc == 0), stop=(dc == 1)),
                              reads=[res("KT"), res("QT")], writes=[PS[bI]])
                    S.add("dve", lambda e, bI=bI, Pmd=Pmd, DTd=DTd: e.tensor_tensor(Pmd, ps[bI][0:64, 0:64], DTd, op=ALU.mult), reads=[PS[bI], res("DT%d" % dr)], writes=[res("Pm%d" % dr)])
                    S.add("pool", lambda e, c=c, Qdd=Qdd, qdd=qdd: e.tensor_tensor(Qdd, QT[:, :, c * 64:(c + 1) * 64], qdd, op=ALU.mult), reads=[res("QT"), res("qdec%d" % dr)], writes=[res("Qd%d" % dr)])
                    S.add("act", lambda e, Sbd=Sbd, Std=Std: e.copy(Sbd, Std), reads=[rSt], writes=[rSb])
                    S.add("pe", lambda e, c=c, bO=bO, Pmd=Pmd: e.matmul(ps[bO][0:64, :], Pmd, v_tm[:, c, :], start=True, stop=False), reads=[res("Pm%d" % dr), res("v_tm")], writes=[PS[bO]])
                    for dc in range(2):
                        S.add("pe", lambda e, dc=dc, bO=bO, Qdd=Qdd, Sbd=Sbd: e.matmul(ps[bO][0:64, :], Qdd[:, dc, :], Sbd[:, dc, :], start=False, stop=(dc == 1)), reads=[res("Qd%d" % dr), rSb], writes=[PS[bO]])
                    S.add("dve", lambda e, c=c, Kdd=Kdd, kdd=kdd: e.tensor_scalar(Kdd, k_tm[:, c, :], kdd[:, 0:1], None, op0=ALU.mult), reads=[res("k_tm"), res("kd%d" % dr)], writes=[res("Kd%d" % dr)])
                    for dc in range(2):
                        S.add("pe", lambda e, dc=dc, c=c, bK=bK, Kdd=Kdd: e.matmul(ps[bK][:, :], Kdd[:, dc * 128:(dc + 1) * 128], v_tm[:, c, :], start=True, stop=True),
                              reads=[res("Kd%d" % dr), res("v_tm")], writes=[PS[bK]])
                        S.add("dve", lambda e, dc=dc, bK=bK, Std=Std, dr=dr: e.scalar_tensor_tensor(Std[:, dc, :], Std[:, dc, :], small[:, 20 + dr:21 + dr], ps[bK][:, :], op0=ALU.mult, op1=ALU.add),
                              reads=[rSt, res("cdec%d" % dr), PS[bK]], writes=[rSt])
                    S.add("dve", lambda e, bO=bO, dr=dr: e.bn_stats(stt[0:64, dr, :], ps[bO][0:64, :]), reads=[PS[bO]], writes=[res("stt%d" % dr)])
                    S.add("dve", lambda e, dr=dr, sm0=sm0: e.bn_aggr(small[0:64, sm0:sm0 + 2], stt[0:64, dr:dr + 1, :]), reads=[res("stt%d" % dr)], writes=[rSm])
                    S.add("dve", lambda e, sm0=sm0: e.tensor_scalar(small[0:64, sm0 + 2:sm0 + 3], small[0:64, sm0 + 1:sm0 + 2], EPS, None, op0=ALU.add), reads=[rSm], writes=[rSm])
                    S.add("act", lambda e, sm0=sm0: e.activation(small[0:64, sm0 + 2:sm0 + 3], small[0:64, sm0 + 2:sm0 + 3], AF.Ln), reads=[rSm], writes=[rSm])
                    S.add("act", lambda e, sm0=sm0: e.activation(small[0:64, sm0 + 2:sm0 + 3], small[0:64, sm0 + 2:sm0 + 3], AF.Exp, scale=-0.5), reads=[rSm], writes=[rSm])
                    S.add("dve", lambda e, bO=bO, sm0=sm0, t1d=t1d: e.tensor_scalar(t1d, ps[bO][0:64, :], small[0:64, sm0:sm0 + 1], small[0:64, sm0 + 2:sm0 + 3], op0=ALU.subtract, op1=ALU.mult),
                          reads=[PS[bO], rSm], writes=[rT1])
                    S.add("dve", lambda e, t1d=t1d: e.tensor_tensor(t1d, t1d, gng, op=ALU.mult), reads=[rT1, res("gng")], writes=[rT1])
                    S.add("dve", lambda e, t1d=t1d: e.tensor_tensor(t1d, t1d, gnb, op=ALU.add), reads=[rT1, res("gnb")], writes=[rT1])
                    if dr == 0:
                        S.add("dve", lambda e, c=c, t1d=t1d: e.tensor_tensor(yf[:, c, :], t1d, sgf[:, c, :], op=ALU.mult), reads=[rT1, res("sgf")], writes=[res("yf%d" % c)])
                    else:
                        S.add("dve", lambda e, c=c, t1d=t1d: e.tensor_tensor(ybk[:, c, :], t1d, sgb[:, c, :], op=ALU.mult), reads=[rT1, res("sgb")], writes=[res("ybk%d" % c)])
                    if idx % 4 == 3:
                        sq = c // 4
                        S.dma("sp", lambda e, sq=sq, dr=dr, h=h, Std=Std: e.dma_start(out=st_out[sq, dr, h].rearrange("(dc p) v -> p dc v", p=128), in_=Std),
                              reads=[rSt], writes=[res("st_out")], key="so%d" % dr)
            for c in range(NCH):
                S.add("pool", lambda e, c=c: e.tensor_tensor(yf[:, c, :], yf[:, c, :], ybk[:, c, :], op=ALU.add), reads=[res("yf%d" % c), res("ybk%d" % c)], writes=[res("yf%d" % c)])
                po = 256 + (c % 2) * 256
                for vc in range(4):
                    S.add("pe", lambda e, vc=vc, c=c, po=po: e.transpose(psb[:, po + vc * 64:po + (vc + 1) * 64], yf[:, c, vc * 128:(vc + 1) * 128], identb[0:64, 0:64]),
                          reads=[res("yf%d" % c), res("identb")], writes=[PSB])
                evac(yTh[:, :, c * 64:(c + 1) * 64], psb[:, po:po + 256].rearrange("p (a b) -> p a b", b=64), [PSB], [res("rc")])
            S.dma("sp", lambda e, h=h: e.dma_start(out=yT_dram[:, h * 4:(h + 1) * 4, :], in_=yTh), reads=[res("rc")], writes=[res("yT_dram")], key="yo")
        S.barrier()

    def na_block(l, x_src):
        A.reset()
        hT = A.bf16([128, 16, NTOK])
        mark0 = A.off
        prologue(l, x_src, hT, 0, 1)
        S.barrier()
        A.off = mark0
        HT = [res("hT%d" % t) for t in range(NT)]
        wq = A.bf16([128, 16, 128]); wk = A.bf16([128, 16, 128]); wv = A.bf16([128, 16, 128])
        QT = A.bf16([128, NTOK]); KT = A.bf16([128, NTOK])
        Va = A.bf16([64, NCH, 130]); Vc = A.bf16([64, 8, 130])
        KcT = A.bf16([128, 512])
        kc32 = A.f32([128, 4, 128])
        ko = A.f32([64, NCH, 128]); vo = A.f32([64, NCH, 128])
        KT32 = A.f32([128, NTOK]); VT32 = A.f32([128, NTOK])
        bl = A.bf16([64, 15, 64]); rm = A.bf16([1, 16, 16, 64])
        PT2 = [A.bf16([64, 16, 64]) for _ in range(2)]
        ao2 = [A.bf16([64, 128]) for _ in range(2)]; rec2 = [A.f32([64, 2]) for _ in range(2)]
        aT = A.bf16([128, NTOK])
        S.dma("pool", lambda e: e.dma_start(out=rm, in_=narow[:, :, :, :]), writes=[res("rm")], key="nb")
        for h in range(16):
            stream_w(wq, w_na_in[:, h * 128:(h + 1) * 128].rearrange("(kc p) c -> p kc c", p=128), res("wq"), "wq")
            stream_w(wk, w_na_in[:, D + h * 128:D + (h + 1) * 128].rearrange("(kc p) c -> p kc c", p=128), res("wk"), "wk")
            stream_w(wv, w_na_in[:, 2 * D + h * 128:2 * D + (h + 1) * 128].rearrange("(kc p) c -> p kc c", p=128), res("wv"), "wv")
            S.dma("pool", lambda e, h=h: e.dma_start(out=bl, in_=nabias[:, h, :, :]), writes=[res("bl")], key="nb")
            S.dma("sp", lambda e, h=h: e.dma_start(out=kc32, in_=ck[h].rearrange("(a p) d -> p a d", p=128)), writes=[res("kc32")], key="ck")
            S.dma("pool", lambda e, h=h: e.dma_start(out=Vc[:, :, 0:128], in_=cv[h].rearrange("(a p) d -> p a d", p=64)), writes=[res("Vc")], key="cv")
            S.add("dve", lambda e: e.tensor_copy(Vc[:, :, 128:129], cst[0:64, 128:136].rearrange("p (a b) -> p a b", b=1)), reads=[res("cst")], writes=[res("Vc")])
            S.add("dve", lambda e: e.tensor_copy(Va[:, :, 128:129], cst[0:64, 128:144].rearrange("p (a b) -> p a b", b=1)), reads=[res("cst")], writes=[res("Va")])
            for a in range(4):
                S.add("pe", lambda e, a=a: e.transpose(ps[6][:, a * 128:(a + 1) * 128], kc32[:, a, :], ident[:]), reads=[res("kc32"), res("ident")], writes=[PS[6]])
            evac(KcT, ps[6][:], [PS[6]], [res("KcT")])
            for th in range(2):
                for kc in range(16):
                    S.add("pe", lambda e, kc=kc, th=th: e.matmul(ps[0][:], wq[:, kc, :], hT[:, kc, th * 512:(th + 1) * 512], start=(kc == 0), stop=(kc == 15)),
                          reads=[res("wq")] + HT[th * 4:(th + 1) * 4], writes=[PS[0]])
                evac(QT[:, th * 512:(th + 1) * 512], ps[0][:], [PS[0]], [res("QT")], scale=128.0 ** -0.5)
                for kc in range(16):
                    S.add("pe", lambda e, kc=kc, th=th: e.matmul(ps[1][:], wk[:, kc, :], hT[:, kc, th * 512:(th + 1) * 512], start=(kc == 0), stop=(kc == 15)),
                          reads=[res("wk")] + HT[th * 4:(th + 1) * 4], writes=[PS[1]])
                evac(KT32[:, th * 512:(th + 1) * 512], ps[1][:], [PS[1]], [res("KT32")])
                S.add("pool", lambda e, th=th: e.tensor_copy(KT[:, th * 512:(th + 1) * 512], KT32[:, th * 512:(th + 1) * 512]), reads=[res("KT32")], writes=[res("KT")])
                for kc in range(16):
                    S.add("pe", lambda e, kc=kc, th=th: e.matmul(ps[6][:], wv[:, kc, :], hT[:, kc, th * 512:(th + 1) * 512], start=(kc == 0), stop=(kc == 15)),
                          reads=[res("wv")] + HT[th * 4:(th + 1) * 4], writes=[PS[6]])
                evac(VT32[:, th * 512:(th + 1) * 512], ps[6][:], [PS[6]], [res("VT32")])
            for r in range(NCH):
                b = 2 + (r % 2)
                S.add("pe", lambda e, r=r, b=b: e.transpose(ps[b][0:64, 0:128], KT32[:, r * 64:(r + 1) * 64], ident[:]), reads=[res("KT32"), res("ident")], writes=[PS[b]])
                S.add("pe", lambda e, r=r, b=b: e.transpose(ps[b][0:64, 128:256], VT32[:, r * 64:(r + 1) * 64], ident[:]), reads=[res("VT32"), res("ident")], writes=[PS[b]])
                S.add("act", lambda e, r=r, b=b: e.copy(ko[:, r, :], ps[b][0:64, 0:128]), reads=[PS[b]], writes=[res("ko"), res("pstok%d" % b)])
                S.add("act", lambda e, r=r, b=b: e.copy(Va[:, r, 0:128], ps[b][0:64, 128:256]), reads=[PS[b]], writes=[res("Va"), res("pstok%d" % b)])
                S.add("dve", lambda e, r=r, b=b: e.tensor_copy(vo[:, r, :], ps[b][0:64, 128:256]), reads=[PS[b]], writes=[res("vo"), res("pstok%d" % b)])
            S.dma("sp", lambda e, h=h: e.dma_start(out=k_out[h].rearrange("(r p) d -> p r d", p=64), in_=ko), reads=[res("ko")], writes=[res("k_out")], key="kvo")
            S.dma("sp", lambda e, h=h: e.dma_start(out=v_out[h].rearrange("(r p) d -> p r d", p=64), in_=vo), reads=[res("vo")], writes=[res("v_out")], key="kvo")
            for r in range(NCH):
                r0 = min(max(r - 4, 0), 8)
                PT, ao, rec = PT2[r % 2], ao2[r % 2], rec2[r % 2]
                rPT, rao, rrec = res("PT%d" % (r % 2)), res("ao%d" % (r % 2)), res("rec%d" % (r % 2))
                pb = 4 + (r % 2)
                S4 = ps[pb][0:64, :].rearrange("p (j q) -> p j q", q=64)
                pb2 = 0 if pb == 4 else 1
                for j in range(16):
                    bank, jj = (pb, j) if j < 8 else (pb2, j - 8)
                    dst = ps[bank][0:64, jj * 64:(jj + 1) * 64]
                    if j < 8:
                        kr = r0 + j
                        drr = kr - r + 7
                        S.add("pe", lambda e, dst=dst, kr=kr, r=r: e.matmul(dst, KT[:, kr * 64:(kr + 1) * 64], QT[:, r * 64:(r + 1) * 64], start=True, stop=False),
                              reads=[res("KT"), res("QT")], writes=[PS[bank]])
                        S.add("pe", lambda e, dst=dst, drr=drr: e.matmul(dst, identb[0:64, 0:64], bl[:, drr, :], start=False, stop=False),
                              reads=[res("identb"), res("bl")], writes=[PS[bank]])
                        S.add("pe", lambda e, dst=dst, r=r, j=j: e.matmul(dst, rm[0:1, r, j, :], onesb[0:1, 0:64], start=False, stop=True),
                              reads=[res("rm"), res("onesb")], writes=[PS[bank]])
                    else:
                        p = j - 8
                        S.add("pe", lambda e, dst=dst, p=p, r=r: e.matmul(dst, KcT[:, p * 64:(p + 1) * 64], QT[:, r * 64:(r + 1) * 64], start=True, stop=True),
                              reads=[res("KcT"), res("QT")], writes=[PS[bank]])
                S.add("act", lambda e, pb=pb, PT=PT: e.activation(PT[:, 0:8, :], ps[pb][0:64, :].rearrange("p (j q) -> p j q", q=64), AF.Exp), reads=[PS[pb]], writes=[rPT])
                S.add("act", lambda e, pb2=pb2, PT=PT: e.activation(PT[:, 8:16, :], ps[pb2][0:64, :].rearrange("p (j q) -> p j q", q=64), AF.Exp, bias=flg[0:64, 1:2]),
                      reads=[PS[pb2], res("flg")], writes=[rPT])
                ob = 2 + (r % 2)
                for j in range(16):
                    rhs = Va[:, r0 + j, 0:129] if j < 8 else Vc[:, j - 8, 0:129]
                    S.add("pe", lambda e, j=j, rhs=rhs, ob=ob, PT=PT: e.matmul(ps[ob][0:64, 0:129], PT[:, j, :], rhs, start=(j == 0), stop=(j == 15)),
                          reads=[rPT, res("Va"), res("Vc")], writes=[PS[ob]])
                S.add("dve", lambda e, ob=ob, rec=rec: e.reciprocal(rec[:, 0:1], ps[ob][0:64, 128:129]), reads=[PS[ob]], writes=[rrec])
                S.add("dve", lambda e, ob=ob, ao=ao, rec=rec: e.tensor_scalar(ao, ps[ob][0:64, 0:128], rec[:, 0:1], None, op0=ALU.mult), reads=[PS[ob], rrec], writes=[rao])
                S.add("pe", lambda e, r=r, ao=ao: e.transpose(psb[:, 512 + (r % 2) * 64:512 + (r % 2 + 1) * 64], ao, identb[0:64, 0:64]), reads=[rao, res("identb")], writes=[PSB])
                evac(aT[:, r * 64:(r + 1) * 64], psb[:, 512 + (r % 2) * 64:512 + (r % 2 + 1) * 64], [PSB], [res("aT")])
            S.dma("sp", lambda e, h=h: e.dma_start(out=yT_dram[:, h, :], in_=aT), reads=[res("aT")], writes=[res("yT_dram")], key="yo")
        S.barrier()

    if upto >= 1:
        retention_block(0, x_in)
    if upto >= 2:
        outproj_block(0, 32, w_ret_out, x_in, xs)
    if upto >= 3:
        moe_block(0, xs, xs, 1)
    if upto >= 4:
        na_block(1, xs)
    if upto >= 5:
        outproj_block(1, 16, w_na_out, xs, xs)
    if upto >= 6:
        moe_block(1, xs, y_out, None)

    block = E(nc.Block())
    S.emit(stack, block)
    stack.close()
    return nc


def _consts():
    c = np.zeros((128, 1024), np.float32)
    c[:, 0:128] = np.eye(128, dtype=np.float32)
    c[:, 128:256] = 1.0
    i = np.arange(64, dtype=np.float32)
    c[:, 256:320] = (i + 1.0)[None, :]
    c[:, 320:384] = (64.0 - i)[None, :]
    jj, ii = np.meshgrid(i, i, indexing="ij")
    c[0:64, 384:448] = np.maximum(ii - jj, 0.0)
    c[0:64, 448:512] = (ii >= jj).astype(np.float32)
    c[0:64, 512:576] = np.maximum(jj - ii, 0.0)
    c[0:64, 576:640] = (jj >= ii).astype(np.float32)
    c[0:64, 640] = 63.0 - i
    c[0:64, 641] = i
    c[:, 642] = 64.0
    return c


def _rope_tables(is_sample):
    nf = 64
    t = np.arange(NTOK)
    cos2 = np.ones((NTOK, 2, 2, nf), np.float32)
    sin2 = np.zeros((NTOK, 2, 2, nf), np.float32)
    if is_sample:
        rows = (t // 64).astype(np.float32)
        cols = (t % 64).astype(np.float32)
        inv = (np.float32(10000.0) ** (-np.arange(nf, dtype=np.float32) / np.float32(nf))).astype(np.float32)
        ang = np.stack([rows[:, None] * inv, cols[:, None] * inv], axis=1).astype(np.float32)
        cs, sn = np.cos(ang).astype(np.float32), np.sin(ang).astype(np.float32)
        cos2[:, :, 0, :] = cs
        cos2[:, :, 1, :] = cs
        sin2[:, :, 0, :] = -sn
        sin2[:, :, 1, :] = sn
    rc = cos2.reshape(NCH, 64, 256).transpose(1, 0, 2)
    rs = sin2.reshape(NCH, 64, 256).transpose(1, 0, 2)
    return np.ascontiguousarray(rc), np.ascontiguousarray(rs)


def _na_tables(is_sample, rpb):
    bias = np.zeros((64, 16, 15, 64), np.float32)
    rowm = np.zeros((1, 16, 16, 64), np.float32)
    col = np.arange(64)
    if is_sample:
        c0 = np.clip(col - 8, 0, 48)
        ok = (col[None, :] >= c0[:, None]) & (col[None, :] < c0[:, None] + 16)
        dc = np.clip(col[None, :] - col[:, None], -15, 15) + 15
        g = rpb[:, :, dc]
        g = np.where(ok[None, None], g, np.float32(NEG))
        bias[:] = g.transpose(3, 0, 1, 2)
    else:
        for r in range(16):
            r0 = min(max(r - 4, 0), 8)
            for j in range(16):
                if j < 8:
                    if (r0 + j) // 4 != r // 4:
                        rowm[0, r, j, :] = NEG
                else:
                    rowm[0, r, j, :] = NEG
    return bias, rowm


_NC_CACHE = {}


def _make_in_maps(x_prompt, x_sample, state_ret, cache_na_k, cache_na_v, c, c_ctx, w_mod, b_mod, ln1_g, ln1_b, ln2_g, ln2_b,
           w_ret_in, ret_decay_logit, ret_gn_g, ret_gn_b, w_ret_out, w_na_in, na_rpb, w_na_out, w_rg, b_rg, w_re, b_re,
           w_gate, w_up, w_down):
    f = lambda a: np.ascontiguousarray(np.asarray(a, dtype=np.float32))
    x_prompt, x_sample, state_ret, cache_na_k, cache_na_v = map(f, (x_prompt, x_sample, state_ret, cache_na_k, cache_na_v))
    c, c_ctx = f(c), f(c_ctx)
    w_rt = np.concatenate([f(w_rg), f(w_re).transpose(0, 2, 1, 3).reshape(2, D, 32)], axis=2)
    b_rt = np.concatenate([f(b_rg), f(b_re).reshape(2, 32)], axis=1)
    shared = {
        "consts": _consts(), "w_mod0": f(w_mod)[0], "w_mod1": f(w_mod)[1], "b_mod": f(b_mod), "ln1_g": f(ln1_g), "ln1_b": f(ln1_b), "ln2_g": f(ln2_g), "ln2_b": f(ln2_b),
        "w_ret_in": f(w_ret_in)[0], "decay": f(ret_decay_logit).reshape(1, 16), "gn_g": f(ret_gn_g)[0], "gn_b": f(ret_gn_b)[0],
        "w_ret_out": f(w_ret_out)[0], "w_na_in": f(w_na_in)[0], "w_na_out": f(w_na_out)[0], "w_rt": np.ascontiguousarray(w_rt), "b_rt": np.ascontiguousarray(b_rt),
        "w_gate0": f(w_gate)[0], "w_gate1": f(w_gate)[1], "w_up0": f(w_up)[0], "w_up1": f(w_up)[1],
        "w_down0": f(w_down)[0], "w_down1": f(w_down)[1],
    }
    rpb = f(na_rpb)[0]
    in_maps = []
    for core in range(8):
        smp = core >= 4
        m = dict(shared)
        if smp:
            b = core - 4
            m["x_in"] = x_sample[b]
            cv_ = c[b]
            m["s0"] = np.ascontiguousarray(state_ret[b, 0])
            m["ck"] = np.ascontiguousarray(cache_na_k[b, 0]); m["cv"] = np.ascontiguousarray(cache_na_v[b, 0])
        else:
            m["x_in"] = np.ascontiguousarray(x_prompt[4 * core:4 * core + 4].reshape(NTOK, D))
            cv_ = c_ctx
            m["s0"] = np.zeros((2, 8, 256, 512), np.float32)
            m["ck"] = np.zeros((16, 512, 128), np.float32); m["cv"] = np.zeros((16, 512, 128), np.float32)
        m["cvec"] = np.ascontiguousarray(cv_.reshape(16, 128).T)
        fl = np.zeros((1, 8), np.float32)
        fl[0, 0] = 1.0 if smp else 0.0
        fl[0, 1] = 0.0 if smp else NEG
        m["flags"] = fl
        m["ropec"], m["ropes"] = _rope_tables(smp)
        m["nabias"], m["narow"] = _na_tables(smp, rpb)
        in_maps.append(m)
    return in_maps


def _assemble(R_):
    y_prompt = np.concatenate([R_[i]["y_out"].reshape(4, 256, D) for i in range(4)], axis=0)
    y_sample = np.stack([R_[4 + i]["y_out"] for i in range(4)], axis=0)
    new_state = np.concatenate([R_[i]["st_out"] for i in range(4)], axis=0)[:, None]
    def kv(name):
        parts = []
        for i in range(4):
            a = R_[i][name].reshape(16, 4, 256, 128).transpose(1, 0, 2, 3)
            parts.append(a)
        return np.ascontiguousarray(np.concatenate(parts, axis=0)[:, None])
    return (np.ascontiguousarray(y_prompt), np.ascontiguousarray(y_sample), np.ascontiguousarray(new_state), kv("k_out"), kv("v_out"))


def kernel(**inputs):
    in_maps = _make_in_maps(**inputs)
    if "nc" not in _NC_CACHE:
        _NC_CACHE["nc"] = build()
    res = run_bass_kernel_spmd(_NC_CACHE["nc"], in_maps, core_ids=list(range(8)))
    return _assemble(res.results)
```

```python
from contextlib import ExitStack
import numpy as np
import concourse.bass as bass
import concourse.mybir as mybir
from concourse.bass_utils import run_bass_kernel_spmd

F32 = mybir.dt.float32
BF16 = mybir.dt.bfloat16
AF = mybir.ActivationFunctionType
ALU = mybir.AluOpType

D = 2048
NTOK = 1024
NT = 8
NCH = 16
ALPHA = (2.0 * 2) ** 0.25
EPS = 1e-5
NEG = -1e30
MOE_STAGE = 3


class Res:
    __slots__ = ("name", "last_w", "readers")

    def __init__(self, name=""):
        self.name = name
        self.last_w = None
        self.readers = {}


class Op:
    __slots__ = ("eng", "fn", "deps", "signal", "count", "is_dma", "dma_key", "epoch")

    def __init__(self, eng, fn):
        self.epoch = 0
        self.eng = eng
        self.fn = fn
        self.deps = []
        self.signal = False
        self.count = 0
        self.is_dma = False
        self.dma_key = None


ENGINES = ("pe", "act", "dve", "pool", "sp")


class Sched:
    def __init__(self, nc):
        self.nc = nc
        self.ops = {e: [] for e in ENGINES}
        self.dma_count = {}
        self.dma_kind = {}
        self.last_dma = {}
        self.epoch = 0

    def _track(self, op, reads, writes):
        op.epoch = self.epoch
        deps = []
        for r in reads:
            if r.last_w is not None:
                deps.append(r.last_w)
        for w in writes:
            if w.last_w is not None:
                deps.append(w.last_w)
            deps.extend(w.readers.values())
        seen = set()
        for d in deps:
            if d is op or id(d) in seen:
                continue
            seen.add(id(d))
            if d.eng == "pe" and op.eng == "pe" and not d.is_dma and not op.is_dma:
                continue
            if d.epoch < self.epoch:
                continue
            op.deps.append((d, self.dma_count[d.dma_key] if d.is_dma else None))
            d.signal = True
        for r in reads:
            r.readers[op.dma_key if op.is_dma else op.eng] = op
        for w in writes:
            w.last_w = op
            w.readers = {}

    def add(self, eng, fn, reads=(), writes=()):
        op = Op(eng, fn)
        self._track(op, reads, writes)
        self.ops[eng].append(op)
        return op

    def dma(self, eng, fn, reads=(), writes=(), key="d"):
        kind = "sw" if eng == "pool" else "hw"
        key = key + "_" + kind
        self.dma_kind[key] = kind
        op = Op(eng, fn)
        op.is_dma = True
        op.dma_key = key
        self.dma_count.setdefault(key, 0)
        self._track(op, reads, writes)
        self.dma_count[key] += 1
        op.count = self.dma_count[key]
        self.ops[eng].append(op)
        self.last_dma[key] = op
        return op

    def barrier(self):
        lasts = []
        for e in ENGINES:
            for op in reversed(self.ops[e]):
                if not op.is_dma:
                    lasts.append(op)
                    break
        lasts.extend(self.last_dma.values())
        for e in ENGINES:
            op = Op(e, lambda eng: eng.nop(nofuse=True))
            for d in lasts:
                if d.eng == e and not d.is_dma:
                    continue
                op.deps.append((d, self.dma_count[d.dma_key] if d.is_dma else None))
                d.signal = True
            op.epoch = self.epoch
            self.ops[e].append(op)
        self.epoch += 1

    def emit(self, stack, block):
        nc = self.nc
        sems = {(e, ep): stack.enter_context(nc.semaphore("s_%s_%d" % (e, ep))) for e in ENGINES for ep in range(self.epoch + 1)}
        dsems = {k: stack.enter_context(nc.semaphore("d_" + k)) for k in self.dma_kind}
        for e in ENGINES:
            c = {}
            for op in self.ops[e]:
                if not op.is_dma and op.signal:
                    c[op.epoch] = c.get(op.epoch, 0) + 1
                    op.count = c[op.epoch]
            print("sched", e, "signals per epoch", c)

        def run(e, engine):
            known = {}
            for op in self.ops[e]:
                need = {}
                for d, dc_ in op.deps:
                    if d.is_dma:
                        s, v = dsems[d.dma_key], 16 * dc_
                    else:
                        s, v = sems[(d.eng, d.epoch)], d.count
                    k = id(s)
                    if k not in need or need[k][1] < v:
                        need[k] = (s, v)
                for k, (s, v) in need.items():
                    if known.get(k, 0) >= v:
                        continue
                    engine.wait_ge(s, v)
                    known[k] = v
                ins = op.fn(engine)
                if op.is_dma:
                    ins.then_inc(dsems[op.dma_key], 16)
                elif op.signal:
                    ins.then_inc(sems[(e, op.epoch)], 1)
            if e in ("pool", "sp"):
                kind = "sw" if e == "pool" else "hw"
                for k, kd in self.dma_kind.items():
                    if kd == kind:
                        engine.wait_ge(dsems[k], 16 * self.dma_count[k])

        block.tensor(lambda eng: run("pe", eng))
        block.scalar(lambda eng: run("act", eng))
        block.vector(lambda eng: run("dve", eng))
        block.gpsimd(lambda eng: run("pool", eng))
        block.sync(lambda eng: run("sp", eng))


def build(upto=6):
    nc = bass.Bass("TRN2", target_bir_lowering=False)

    def din(name, shape, dt=F32):
        return nc.dram_tensor(name, list(shape), dt, kind="ExternalInput").ap()

    def dout(name, shape, dt=F32):
        return nc.dram_tensor(name, list(shape), dt, kind="ExternalOutput").ap()

    def dint(name, shape, dt=F32):
        return nc.dram_tensor(name, list(shape), dt).ap()

    x_in = din("x_in", [NTOK, D])
    cvec = din("cvec", [128, 16])
    s0 = din("s0", [2, 8, 256, 512])
    flags = din("flags", [1, 8])
    ropec = din("ropec", [64, NCH, 256])
    ropes = din("ropes", [64, NCH, 256])
    consts = din("consts", [128, 656])
    ck = din("ck", [16, 512, 128])
    cv = din("cv", [16, 512, 128])
    nabias = din("nabias", [64, 16, 15, 64])
    narow = din("narow", [1, 16, 16, 64])
    w_mod = [din("w_mod%d" % i, [D, 6 * D]) for i in range(2)]
    b_mod = din("b_mod", [2, 6 * D])
    ln1_g = din("ln1_g", [2, D]); ln1_b = din("ln1_b", [2, D])
    ln2_g = din("ln2_g", [2, D]); ln2_b = din("ln2_b", [2, D])
    w_ret_in = din("w_ret_in", [D, 16384])
    decay = din("decay", [1, 16])
    gn_g = din("gn_g", [8, 512]); gn_b = din("gn_b", [8, 512])
    w_ret_out = din("w_ret_out", [4096, D])
    w_na_in = din("w_na_in", [D, 3 * D])
    w_na_out = din("w_na_out", [D, D])
    w_rt = din("w_rt", [2, D, 36])
    b_rt = din("b_rt", [2, 36])
    w_gate = [din("w_gate%d" % i, [32, D, 512]) for i in range(2)]
    w_up = [din("w_up%d" % i, [32, D, 512]) for i in range(2)]
    w_down = [din("w_down%d" % i, [32, 512, D]) for i in range(2)]

    y_out = dout("y_out", [NTOK, D])
    st_out = dout("st_out", [4, 2, 8, 256, 512])
    k_out = dout("k_out", [16, NTOK, 128])
    v_out = dout("v_out", [16, NTOK, 128])

    m_dram = dint("m_dram", [2, 6 * D])
    xs = dint("xs", [NTOK, D])
    yT_dram = dint("yT_dram", [128, 32, NTOK], BF16)

    S = Sched(nc)
    stack = ExitStack()
    E = stack.enter_context

    AW = 51530
    arena = E(nc.sbuf_tensor("arena", [128, AW], F32))
    ident = E(nc.sbuf_tensor("ident", [128, 128], F32))
    identb = E(nc.sbuf_tensor("identb", [128, 128], BF16))
    onesf = E(nc.sbuf_tensor("onesf", [128, 128], F32))
    onesb = E(nc.sbuf_tensor("onesb", [1, 64], BF16))
    cst = E(nc.sbuf_tensor("cst", [128, 656], F32))
    sil = E(nc.sbuf_tensor("sil", [128, 16], F32))
    flg = E(nc.sbuf_tensor("flg", [128, 8], F32))
    lg = E(nc.sbuf_tensor("lg", [128, 16], F32))
    small = E(nc.sbuf_tensor("small", [128, 64], F32))
    stt = E(nc.sbuf_tensor("stt", [128, 4, 6], F32))
    comb = E(nc.sbuf_tensor("comb", [128, NT, 32], F32))
    rl = E(nc.sbuf_tensor("rl", [128, 96], F32))
    rbias = E(nc.sbuf_tensor("rbias", [128, 2, 36], F32))

    ps = [E(nc.psum_tensor("ps%d" % i, [128, 512], F32)) for i in range(7)]
    psb = E(nc.psum_tensor("psb", [128, 1024], BF16))
    PS = [Res("ps%d" % i) for i in range(7)]
    PSB = Res("psb")

    R = {}

    def res(name):
        if name not in R:
            R[name] = Res(name)
        return R[name]

    class Arena:
        def __init__(self):
            self.off = 0

        def reset(self):
            self.off = 0

        def f32(self, shape):
            n = int(np.prod(shape[1:]))
            v = arena[0:shape[0], self.off:self.off + n]
            self.off += n
            assert self.off <= AW, ("arena overflow", self.off)
            return v if len(shape) == 2 else v.rearrange(
                "p (a b) -> p a b", b=shape[2]) if len(shape) == 3 else v.rearrange(
                "p (a b c) -> p a b c", b=shape[2], c=shape[3])

        def bf16(self, shape):
            n = int(np.prod(shape[1:]))
            assert n % 2 == 0
            v = arena[0:shape[0], self.off:self.off + n // 2].bitcast(BF16)
            self.off += n // 2
            assert self.off <= AW, ("arena overflow", self.off)
            return v if len(shape) == 2 else v.rearrange(
                "p (a b) -> p a b", b=shape[2]) if len(shape) == 3 else v.rearrange(
                "p (a b c) -> p a b c", b=shape[2], c=shape[3])

    A = Arena()

    S.dma("sp", lambda e: e.dma_start(out=cst[:], in_=consts[:, :]), writes=[res("cst")], key="c0")
    S.dma("sp", lambda e: e.dma_start(out=sil[:], in_=cvec[:, :]), writes=[res("sil")], key="c0")
    S.dma("sp", lambda e: e.dma_start(out=flg[:], in_=flags[0, :].partition_broadcast(128)), writes=[res("flg")], key="c0")
    S.dma("sp", lambda e: e.dma_start(out=lg[:], in_=decay[0, :].partition_broadcast(128)), writes=[res("lg")], key="c0")
    S.dma("sp", lambda e: e.dma_start(out=rbias[:, 0, :], in_=b_rt[0, :].partition_broadcast(128)), writes=[res("rbias")], key="c0")
    S.dma("sp", lambda e: e.dma_start(out=rbias[:, 1, :], in_=b_rt[1, :].partition_broadcast(128)), writes=[res("rbias")], key="c0")
    S.add("dve", lambda e: e.tensor_copy(ident[:], cst[:, 0:128]), reads=[res("cst")], writes=[res("ident")])
    S.add("dve", lambda e: e.tensor_copy(identb[:], cst[:, 0:128]), reads=[res("cst")], writes=[res("identb")])
    S.add("dve", lambda e: e.tensor_copy(onesf[:], cst[:, 128:256]), reads=[res("cst")], writes=[res("onesf")])
    S.add("dve", lambda e: e.tensor_copy(onesb[:], cst[0:1, 128:192]), reads=[res("cst")], writes=[res("onesb")])
    S.add("act", lambda e: e.activation(sil[:], sil[:], AF.Silu), reads=[res("sil")], writes=[res("sil")])
    S.add("act", lambda e: e.activation(lg[:], lg[:], AF.Sigmoid), reads=[res("lg")], writes=[res("lg")])
    S.add("act", lambda e: e.activation(lg[:], lg[:], AF.Ln), reads=[res("lg")], writes=[res("lg")])

    A.reset()
    macc = A.f32([128, D])
    mbias = A.f32([128, D])
    mring = [A.f32([128, D]) for _ in range(3)]
    MR = [Res("mring%d" % i) for i in range(3)]
    pi = 0
    for l in range(2):
        for cb in range(6):
            S.dma("sp", lambda e, l=l, cb=cb: e.dma_start(out=mbias[:], in_=b_mod[l, cb * D:(cb + 1) * D].partition_broadcast(128)),
                  writes=[res("mbias")], key="mb")
            for kc in range(16):
                slot = pi % 3
                pi += 1
                S.dma("sp", lambda e, l=l, cb=cb, kc=kc, slot=slot: e.dma_start(
                    out=mring[slot][:], in_=w_mod[l][kc * 128:(kc + 1) * 128, cb * D:(cb + 1) * D]),
                    writes=[MR[slot]], key="mr%d" % slot)
                if kc == 0:
                    S.add("dve", lambda e, kc=kc, slot=slot: e.tensor_scalar(macc[:], mring[slot][:], sil[:, kc:kc + 1], None, op0=ALU.mult),
                          reads=[MR[slot], res("sil")], writes=[res("macc")])
                else:
                    S.add("dve", lambda e, kc=kc, slot=slot: e.scalar_tensor_tensor(
                        macc[:], mring[slot][:], sil[:, kc:kc + 1], macc[:], op0=ALU.mult, op1=ALU.add),
                        reads=[MR[slot], res("sil"), res("macc")], writes=[res("macc")])
            for j in range(4):
                S.add("pe", lambda e, j=j: e.matmul(ps[j][:], onesf[:], macc[:, j * 512:(j + 1) * 512], start=True, stop=True),
                      reads=[res("onesf"), res("macc")], writes=[PS[j]])
                S.add("dve", lambda e, j=j: e.tensor_tensor(mbias[:, j * 512:(j + 1) * 512], ps[j][:], mbias[:, j * 512:(j + 1) * 512], op=ALU.add),
                      reads=[PS[j], res("mbias")], writes=[res("mbias")])
            S.dma("sp", lambda e, l=l, cb=cb: e.dma_start(out=m_dram[l:l + 1, cb * D:(cb + 1) * D], in_=mbias[0:1, :]),
                  reads=[res("mbias")], writes=[res("m_dram")], key="mo")
    S.barrier()

    def bc_load(dst, src_row, r, key):
        S.dma("sp", lambda e: e.dma_start(out=dst, in_=src_row.partition_broadcast(128)), reads=[res("m_dram")], writes=[r], key=key)

    ev_flip = [0]

    def evac(dst, src, rd, wr, scale=None):
        ev_flip[0] ^= 1
        if ev_flip[0]:
            if scale is None:
                S.add("act", lambda e: e.copy(dst, src), reads=rd, writes=wr)
            else:
                S.add("act", lambda e: e.mul(dst, src, scale), reads=rd, writes=wr)
        else:
            if scale is None:
                S.add("dve", lambda e: e.tensor_copy(dst, src), reads=rd, writes=wr)
            else:
                S.add("dve", lambda e: e.tensor_scalar(dst, src, scale, None, op0=ALU.mult), reads=rd, writes=wr)

    def to_feature_major(h_ap, h_res, hT, t, hT32=None):
        for g in range(4):
            b = g % 4
            for q in range(4):
                kc = g * 4 + q
                S.add("pe", lambda e, kc=kc, b=b, q=q: e.transpose(ps[b][:, q * 128:(q + 1) * 128], h_ap[:, kc * 128:(kc + 1) * 128], ident[:]),
                      reads=[h_res, res("ident")], writes=[PS[b]])
            evac(hT[:, g * 4:(g + 1) * 4, t * 128:(t + 1) * 128], ps[b][:].rearrange("p (a b) -> p a b", b=128),
                 [PS[b]], [res("hT%d" % t), res("pstok%d" % b)])
            if hT32 is not None:
                S.add("dve", lambda e, g=g, b=b: e.tensor_copy(hT32[:, g * 4:(g + 1) * 4, :], ps[b][:].rearrange("p (a b) -> p a b", b=128)),
                      reads=[PS[b]], writes=[res("hT32"), res("pstok%d" % b)])

    def layer_norm_tile(u, u_res, g_bc, b_bc, out_ap, out_res, rd):
        for j in range(4):
            S.add("dve", lambda e, j=j: e.bn_stats(stt[:, j, :], u[:, j * 512:(j + 1) * 512]), reads=[u_res], writes=[res("stt")])
        S.add("dve", lambda e: e.bn_aggr(small[:, 0:2], stt[:]), reads=[res("stt")], writes=[res("small")])
        S.add("dve", lambda e: e.tensor_scalar(small[:, 2:3], small[:, 1:2], EPS, None, op0=ALU.add), reads=[res("small")], writes=[res("small")])
        S.add("act", lambda e: e.activation(small[:, 2:3], small[:, 2:3], AF.Ln), reads=[res("small")], writes=[res("small")])
        S.add("act", lambda e: e.activation(small[:, 2:3], small[:, 2:3], AF.Exp, scale=-0.5), reads=[res("small")], writes=[res("small")])
        S.add("dve", lambda e: e.tensor_scalar(u, u, small[:, 0:1], small[:, 2:3], op0=ALU.subtract, op1=ALU.mult),
              reads=[u_res, res("small")], writes=[u_res])
        S.add("dve", lambda e: e.tensor_tensor(u, u, g_bc, op=ALU.mult), reads=[u_res] + rd, writes=[u_res])
        S.add("dve", lambda e: e.tensor_tensor(out_ap, u, b_bc, op=ALU.add), reads=[u_res] + rd, writes=[out_res])

    def stream_w(slot_ap, src_ap, slot_res, key):
        S.dma("pool", lambda e: e.dma_start(out=slot_ap, in_=src_ap), writes=[slot_res], key=key)

    def prologue(l, x_src, hT, j_shift, j_scale):
        sc = A.f32([128, D]); sh = A.f32([128, D])
        xw = [A.f32([128, D]) for _ in range(2)]
        bc_load(sc, m_dram[l, j_scale * D:(j_scale + 1) * D], res("sc"), "bc")
        bc_load(sh, m_dram[l, j_shift * D:(j_shift + 1) * D], res("sh"), "bc")
        S.add("dve", lambda e: e.tensor_scalar(sc, sc, 1.0, None, op0=ALU.add), reads=[res("sc")], writes=[res("sc")])
        for t in range(NT):
            xt = xw[t % 2]
            xr = res("xw%d" % (t % 2))
            S.dma("sp", lambda e, t=t, xt=xt: e.dma_start(out=xt, in_=x_src[t * 128:(t + 1) * 128, :]), reads=[res("xs")], writes=[xr], key="xl%d" % (t % 2))
            S.add("dve", lambda e, xt=xt: e.tensor_tensor(xt, xt, sc, op=ALU.mult), reads=[xr, res("sc")], writes=[xr])
            S.add("dve", lambda e, xt=xt: e.tensor_tensor(xt, xt, sh, op=ALU.add), reads=[xr, res("sh")], writes=[xr])
            to_feature_major(xt, xr, hT, t)

    def epilogue(l, x_src, o_half, th, gate_j, lng, lnb, next_mod, hT_next, want_router, x_dst, l_router):
        pass

    def moe_block(l, x_src, x_dst, next_l):
        A.reset()
        hT = A.bf16([128, 16, NTOK])
        yacc = A.f32([128, NT, D])
        hid = A.bf16([128, 4, NTOK])
        ring = [A.bf16([128, 8192]) for _ in range(3)]
        RG = [Res("ring%d" % i) for i in range(3)]
        sgt = [A.bf16([128, 512]) for _ in range(2)]
        mark = A.off
        sc = A.f32([128, D]); sh = A.f32([128, D])
        xw = [A.f32([128, D])] * 2
        hT32 = A.f32([128, 16, 128])
        wr32 = A.f32([128, 16, 36])
        bc_load(sc, m_dram[l, 4 * D:5 * D], res("sc"), "bc")
        bc_load(sh, m_dram[l, 3 * D:4 * D], res("sh"), "bc")
        S.add("dve", lambda e: e.tensor_scalar(sc, sc, 1.0, None, op0=ALU.add), reads=[res("sc")], writes=[res("sc")])
        S.dma("sp", lambda e: e.dma_start(out=wr32, in_=w_rt[l].rearrange("(kc p) c -> p kc c", p=128)), writes=[res("wr32")], key="wr")
        for t in range(NT):
            xt = xw[0]
            xr = res("xw0")
            S.dma("sp", lambda e, t=t, xt=xt: e.dma_start(out=xt, in_=x_src[t * 128:(t + 1) * 128, :]), reads=[res("xs")], writes=[xr], key="xl0")
            S.add("dve", lambda e, xt=xt: e.tensor_tensor(xt, xt, sc, op=ALU.mult), reads=[xr, res("sc")], writes=[xr])
            S.add("dve", lambda e, xt=xt: e.tensor_tensor(xt, xt, sh, op=ALU.add), reads=[xr, res("sh")], writes=[xr])
            to_feature_major(xt, xr, hT, t, hT32=(hT32 if MOE_STAGE >= 0.5 else None))
            if MOE_STAGE < 1:
                continue
            for kc in range(16):
                S.add("pe", lambda e, kc=kc: e.matmul(ps[4][:, 0:36], hT32[:, kc, :], wr32[:, kc, :], start=(kc == 0), stop=(kc == 15)),
                      reads=[res("hT32"), res("wr32")], writes=[PS[4]])
            rr = res("rl")
            S.add("dve", lambda e: e.tensor_tensor(rl[:, 0:36], ps[4][:, 0:36], rbias[:, l, :], op=ALU.add), reads=[PS[4], res("rbias")], writes=[rr])
            S.add("dve", lambda e: e.tensor_reduce(small[:, 8:9], rl[:, 0:4], mybir.AxisListType.X, ALU.max), reads=[rr], writes=[res("sm_g")])
            S.add("dve", lambda e: e.tensor_scalar(rl[:, 40:44], rl[:, 0:4], small[:, 8:9], None, op0=ALU.is_equal), reads=[rr, res("sm_g")], writes=[rr])
            S.add("dve", lambda e: e.tensor_scalar(rl[:, 44:48], rl[:, 0:4], small[:, 8:9], None, op0=ALU.subtract), reads=[rr, res("sm_g")], writes=[rr])
            S.add("act", lambda e: e.activation(rl[:, 44:48], rl[:, 44:48], AF.Exp, accum_out=small[:, 9:10]), reads=[rr], writes=[rr, res("sm_g2")])
            S.add("dve", lambda e: e.reciprocal(small[:, 10:11], small[:, 9:10]), reads=[res("sm_g2")], writes=[res("sm_pg")])
            S.add("dve", lambda e: e.tensor_scalar(rl[:, 40:44], rl[:, 40:44], 1.0, 1e30, op0=ALU.subtract, op1=ALU.mult), reads=[rr], writes=[rr])
            for g_ in range(4):
                S.add("dve", lambda e, g_=g_: e.tensor_scalar(rl[:, 48 + 8 * g_:56 + 8 * g_], rl[:, 4 + 8 * g_:12 + 8 * g_], rl[:, 40 + g_:41 + g_], None, op0=ALU.add),
                      reads=[rr], writes=[rr])
            S.add("dve", lambda e: e.tensor_reduce(small[:, 11:12], rl[:, 48:80], mybir.AxisListType.X, ALU.max), reads=[rr], writes=[res("sm_m1")])
            S.add("dve", lambda e, t=t: e.tensor_scalar(comb[:, t, :], rl[:, 48:80], small[:, 11:12], None, op0=ALU.is_equal), reads=[rr, res("sm_m1")], writes=[res("comb")])
            S.add("dve", lambda e, t=t: e.scalar_tensor_tensor(rl[:, 48:80], comb[:, t, :], -1e30, rl[:, 48:80], op0=ALU.mult, op1=ALU.add),
                  reads=[rr, res("comb")], writes=[rr])
            S.add("dve", lambda e: e.tensor_reduce(small[:, 12:13], rl[:, 48:80], mybir.AxisListType.X, ALU.max), reads=[rr], writes=[res("sm_m2")])
            S.add("dve", lambda e: e.tensor_scalar(rl[:, 4:36], rl[:, 48:80], small[:, 12:13], None, op0=ALU.is_equal), reads=[rr, res("sm_m2")], writes=[rr])
            S.add("dve", lambda e: e.tensor_tensor(small[:, 13:14], small[:, 12:13], small[:, 11:12], op=ALU.subtract), reads=[res("sm_m1"), res("sm_m2")], writes=[res("sm_w")])
            S.add("act", lambda e: e.activation(small[:, 13:14], small[:, 13:14], AF.Exp), reads=[res("sm_w")], writes=[res("sm_w")])
            S.add("dve", lambda e: e.tensor_scalar(small[:, 14:15], small[:, 13:14], 1.0, None, op0=ALU.add), reads=[res("sm_w")], writes=[res("sm_d")])
            S.add("dve", lambda e: e.reciprocal(small[:, 14:15], small[:, 14:15]), reads=[res("sm_d")], writes=[res("sm_d")])
            S.add("dve", lambda e: e.tensor_tensor(small[:, 15:16], small[:, 14:15], small[:, 10:11], op=ALU.mult), reads=[res("sm_d"), res("sm_pg")], writes=[res("sm_t1")])
            S.add("dve", lambda e: e.tensor_tensor(small[:, 16:17], small[:, 15:16], small[:, 13:14], op=ALU.mult), reads=[res("sm_t1"), res("sm_w")], writes=[res("sm_t2")])
            S.add("dve", lambda e, t=t: e.tensor_scalar(comb[:, t, :], comb[:, t, :], small[:, 15:16], None, op0=ALU.mult), reads=[res("comb"), res("sm_t1")], writes=[res("comb")])
            S.add("dve", lambda e, t=t: e.scalar_tensor_tensor(comb[:, t, :], rl[:, 4:36], small[:, 16:17], comb[:, t, :], op0=ALU.mult, op1=ALU.add),
                  reads=[rr, res("sm_t2"), res("comb")], writes=[res("comb")])
        if MOE_STAGE < 2:
            S.barrier()
            return
        HT = [res("hT%d" % t) for t in range(NT)]
        si = [0]

        def nxt():
            s = si[0] % 3
            si[0] += 1
            return s
        for ex in range(32):
            sg_, su_, sd_ = nxt(), nxt(), nxt()
            G = ring[sg_].rearrange("p (k c) -> p k c", c=512)
            U = ring[su_].rearrange("p (k c) -> p k c", c=512)
            Dw = ring[sd_].rearrange("p (k c) -> p k c", c=D)
            stream_w(G, w_gate[l][ex].rearrange("(kc p) c -> p kc c", p=128), RG[sg_], "rg%d" % sg_)
            stream_w(U, w_up[l][ex].rearrange("(kc p) c -> p kc c", p=128), RG[su_], "rg%d" % su_)
            stream_w(Dw, w_down[l][ex].rearrange("(kc p) c -> p kc c", p=128), RG[sd_], "rg%d" % sd_)
            it = 0
            for th in range(2):
                for fc in range(4):
                    bg, bu = (0, 1) if it % 2 == 0 else (2, 3)
                    sgi = it % 2
                    it += 1
                    for kc in range(16):
                        S.add("pe", lambda e, kc=kc, fc=fc, th=th, bg=bg, G=G: e.matmul(ps[bg][:], G[:, kc, fc * 128:(fc + 1) * 128], hT[:, kc, th * 512:(th + 1) * 512],
                                                                                 start=(kc == 0), stop=(kc == 15)),
                              reads=[RG[sg_]] + HT[th * 4:(th + 1) * 4], writes=[PS[bg]])
                    for kc in range(16):
                        S.add("pe", lambda e, kc=kc, fc=fc, th=th, bu=bu, U=U: e.matmul(ps[bu][:], U[:, kc, fc * 128:(fc + 1) * 128], hT[:, kc, th * 512:(th + 1) * 512],
                                                                                 start=(kc == 0), stop=(kc == 15)),
                              reads=[RG[su_]] + HT[th * 4:(th + 1) * 4], writes=[PS[bu]])
                    S.add("act", lambda e, bg=bg, sgi=sgi: e.activation(sgt[sgi], ps[bg][:], AF.Silu), reads=[PS[bg]], writes=[res("sgt%d" % sgi)])
                    S.add("dve", lambda e, bu=bu, sgi=sgi, fc=fc, th=th: e.tensor_tensor(hid[:, fc, th * 512:(th + 1) * 512], sgt[sgi], ps[bu][:], op=ALU.mult),
                          reads=[PS[bu], res("sgt%d" % sgi)], writes=[res("hid%d" % th)])
            it = 0
            for t in range(NT):
                for dc in range(4):
                    b = 4 + (it % 3)
                    it += 1
                    for fc in range(4):
                        S.add("pe", lambda e, fc=fc, t=t, dc=dc, b=b, Dw=Dw: e.matmul(ps[b][:], hid[:, fc, t * 128:(t + 1) * 128], Dw[:, fc, dc * 512:(dc + 1) * 512],
                                                                               start=(fc == 0), stop=(fc == 3)),
                              reads=[RG[sd_], res("hid%d" % (t // 4))], writes=[PS[b]])
                    yr = res("yacc%d" % t)
                    if ex == 0:
                        S.add("dve", lambda e, t=t, dc=dc, b=b, ex=ex: e.tensor_scalar(yacc[:, t, dc * 512:(dc + 1) * 512], ps[b][:], comb[:, t, ex:ex + 1], None, op0=ALU.mult),
                              reads=[PS[b], res("comb")], writes=[yr])
                    else:
                        S.add("dve", lambda e, t=t, dc=dc, b=b, ex=ex: e.scalar_tensor_tensor(yacc[:, t, dc * 512:(dc + 1) * 512], ps[b][:], comb[:, t, ex:ex + 1],
                                                                                              yacc[:, t, dc * 512:(dc + 1) * 512], op0=ALU.mult, op1=ALU.add),
                              reads=[PS[b], res("comb"), yr], writes=[yr])
        if MOE_STAGE < 3:
            S.barrier()
            return
        gt = sc; lgt = sh
        lbt = hT32.rearrange("p a b -> p (a b)")
        bc_load(gt, m_dram[l, 5 * D:6 * D], res("sc"), "bc")
        S.dma("sp", lambda e: e.dma_start(out=lgt, in_=ln2_g[l, :].partition_broadcast(128)), writes=[res("sh")], key="bc")
        S.dma("sp", lambda e: e.dma_start(out=lbt, in_=ln2_b[l, :].partition_broadcast(128)), writes=[res("hT32")], key="bc")
        for t in range(NT):
            xt = xw[0]
            xr = res("xw0")
            yr = res("yacc%d" % t)
            S.dma("sp", lambda e, t=t, xt=xt: e.dma_start(out=xt, in_=x_src[t * 128:(t + 1) * 128, :]), reads=[res("xs")], writes=[xr], key="xl0")
            S.add("dve", lambda e, t=t: e.tensor_tensor(yacc[:, t, :], yacc[:, t, :], gt, op=ALU.mult), reads=[yr, res("sc")], writes=[yr])
            S.add("dve", lambda e, t=t, xt=xt: e.scalar_tensor_tensor(xt, xt, ALPHA, yacc[:, t, :], op0=ALU.mult, op1=ALU.add), reads=[yr, xr], writes=[xr])
            layer_norm_tile(xt, xr, lgt, lbt, xt, xr, [res("sh"), res("hT32")])
            S.dma("sp", lambda e, t=t, xt=xt: e.dma_start(out=x_dst[t * 128:(t + 1) * 128, :], in_=xt), reads=[xr], writes=[res("xdst")], key="xo")
        S.barrier()

    def outproj_block(l, KC, w_out_ap, x_src, x_dst):
        A.reset()
        CW = 8192 // KC
        npiece = D // CW
        yTh = A.bf16([128, KC, 512])
        oh = A.f32([128, 4, D])
        ring = [A.bf16([128, 8192]) for _ in range(3)]
        RG = [Res("oring%d" % i) for i in range(3)]
        gt = A.f32([128, D]); lgt = A.f32([128, D]); lbt = A.f32([128, D])
        xw = [A.f32([128, D]) for _ in range(2)]
        bc_load(gt, m_dram[l, 2 * D:3 * D], res("gt"), "bc")
        S.dma("sp", lambda e: e.dma_start(out=lgt, in_=ln1_g[l, :].partition_broadcast(128)), writes=[res("lgt")], key="bc")
        S.dma("sp", lambda e: e.dma_start(out=lbt, in_=ln1_b[l, :].partition_broadcast(128)), writes=[res("lbt")], key="bc")
        pc = 0
        for th in range(2):
            S.dma("sp", lambda e, th=th: e.dma_start(out=yTh, in_=yT_dram[:, 0:KC, th * 512:(th + 1) * 512]), reads=[res("yT_dram")], writes=[res("yTh")], key="yt")
            it = 0
            for p in range(npiece):
                slot = pc % 3
                pc += 1
                W = ring[slot].rearrange("p (k c) -> p k c", c=CW)
                stream_w(W, w_out_ap[:, p * CW:(p + 1) * CW].rearrange("(kc p) c -> p kc c", p=128), RG[slot], "or%d" % slot)
                for tt in range(4):
                    b = it % 4
                    it += 1
                    for kc in range(KC):
                        S.add("pe", lambda e, kc=kc, tt=tt, b=b, W=W: e.matmul(ps[b][:, 0:CW], yTh[:, kc, tt * 128:(tt + 1) * 128], W[:, kc, :], start=(kc == 0), stop=(kc == KC - 1)),
                              reads=[RG[slot], res("yTh")], writes=[PS[b]])
                    evac(oh[:, tt, p * CW:(p + 1) * CW], ps[b][:, 0:CW], [PS[b]], [res("oh%d" % tt)])
            for tt in range(4):
                t = th * 4 + tt
                xt = xw[t % 2]
                xr = res("xw%d" % (t % 2))
                orr = res("oh%d" % tt)
                S.dma("sp", lambda e, t=t, xt=xt: e.dma_start(out=xt, in_=x_src[t * 128:(t + 1) * 128, :]), reads=[res("xs")], writes=[xr], key="xl%d" % (t % 2))
                S.add("dve", lambda e, tt=tt: e.tensor_tensor(oh[:, tt, :], oh[:, tt, :], gt, op=ALU.mult), reads=[orr, res("gt")], writes=[orr])
                S.add("dve", lambda e, tt=tt, xt=xt: e.scalar_tensor_tensor(xt, xt, ALPHA, oh[:, tt, :], op0=ALU.mult, op1=ALU.add), reads=[orr, xr], writes=[xr])
                layer_norm_tile(xt, xr, lgt, lbt, xt, xr, [res("lgt"), res("lbt")])
                S.dma("sp", lambda e, t=t, xt=xt: e.dma_start(out=x_dst[t * 128:(t + 1) * 128, :], in_=xt), reads=[xr], writes=[res("xdst")], key="xo")
        S.barrier()

    def retention_block(l, x_src):
        A.reset()
        hT = A.bf16([128, 16, NTOK])
        mark0 = A.off
        prologue(l, x_src, hT, 0, 1)
        S.barrier()
        A.off = mark0
        HT = [res("hT%d" % t) for t in range(NT)]
        ring = [A.bf16([128, 8192]) for _ in range(2)]
        RG = [Res("rring%d" % i) for i in range(2)]
        QT = A.bf16([128, 2, NTOK]); KT = A.bf16([128, 2, NTOK])
        k_tm = A.bf16([64, NCH, 256]); v_tm = A.bf16([64, NCH, 512])
        sgf = A.bf16([64, NCH, 512]); sgb = A.bf16([64, NCH, 512])
        yf = A.bf16([64, NCH, 512])
        off_rc = A.off
        rc = A.bf16([64, NCH, 256]); rs = A.bf16([64, NCH, 256])
        yTh = arena[0:128, off_rc:off_rc + 2048].bitcast(BF16).rearrange("p (a b) -> p a b", b=NTOK)
        St2 = [A.f32([128, 2, 512]) for _ in range(2)]; Sb2 = [A.bf16([128, 2, 512]) for _ in range(2)]
        DT2 = [A.f32([64, 64]) for _ in range(2)]; qdec2 = [A.bf16([128, 2, 64]) for _ in range(2)]; kd2 = [A.f32([64, 2]) for _ in range(2)]
        gng = A.f32([64, 512]); gnb = A.f32([64, 512])
        t12 = [A.f32([64, 512]) for _ in range(2)]
        t1 = t12[0]; t2 = t12[1]
        qr = A.bf16([64, 256])
        Pm2 = [A.bf16([64, 64]) for _ in range(2)]; Qd2 = [A.bf16([128, 2, 64]) for _ in range(2)]; Kd2 = [A.bf16([64, 256]) for _ in range(2)]
        ybk = A.bf16([64, NCH, 512])
        stg = [A.bf16([128, 512]) for _ in range(2)]
        stg_i = [0]
        S.dma("pool", lambda e: e.dma_start(out=rs, in_=ropes[:, :, :]), writes=[res("rs")], key="rt")
        pc = [0]

        def piece(c0, ncols):
            slot = pc[0] % 2
            pc[0] += 1
            W = ring[slot][:, 0:16 * ncols].rearrange("p (k c) -> p k c", c=ncols)
            stream_w(W, w_ret_in[:, c0:c0 + ncols].rearrange("(kc p) c -> p kc c", p=128), RG[slot], "rr%d" % slot)
            return W, RG[slot]

        def proj_chunk(W, Wr, c, ncols, b):
            for kc in range(16):
                S.add("pe", lambda e, kc=kc: e.matmul(ps[b][0:64, 0:ncols], hT[:, kc, c * 64:(c + 1) * 64], W[:, kc, :], start=(kc == 0), stop=(kc == 15)),
                      reads=[Wr, HT[c // 2]], writes=[PS[b]])

        def rope_to(dst, dst_res, c, b, kscale):
            p4 = ps[b][0:64, 0:256].rearrange("p (a h f) -> p a h f", a=2, h=2)
            S.add("dve", lambda e: e.tensor_tensor(t1[:, 0:256], ps[b][0:64, 0:256], rc[:, c, :], op=ALU.mult), reads=[PS[b], res("rc")], writes=[res("t1")])
            S.add("dve", lambda e: e.tensor_tensor(t2[:, 0:256].rearrange("p (a h f) -> p a h f", a=2, h=2), p4[:, :, ::-1, :],
                                                   rs[:, c, :].rearrange("p (a h f) -> p a h f", a=2, h=2), op=ALU.mult), reads=[PS[b], res("rs")], writes=[res("t2")])
            if kscale is None:
                S.add("dve", lambda e: e.tensor_tensor(dst, t1[:, 0:256], t2[:, 0:256], op=ALU.add), reads=[res("t1"), res("t2")], writes=[dst_res])
            else:
                S.add("dve", lambda e: e.tensor_tensor(t1[:, 0:256], t1[:, 0:256], t2[:, 0:256], op=ALU.add), reads=[res("t1"), res("t2")], writes=[res("t1")])
                S.add("dve", lambda e: e.tensor_scalar(dst, t1[:, 0:256], kscale, None, op0=ALU.mult), reads=[res("t1")], writes=[dst_res])

        def tr_to(dstT, dstT_res, src, src_res, c):
            for dc in range(2):
                S.add("pe", lambda e, dc=dc: e.transpose(psb[:, dc * 64:(dc + 1) * 64], src[:, dc * 128:(dc + 1) * 128], identb[0:64, 0:64]),
                      reads=[src_res, res("identb")], writes=[PSB])
            evac(dstT[:, :, c * 64:(c + 1) * 64], psb[:, 0:128].rearrange("p (a b) -> p a b", b=64), [PSB], [dstT_res])

        for h in range(8):
            S.dma("pool", lambda e: e.dma_start(out=rc, in_=ropec[:, :, :]), writes=[res("rc")], key="rtc")
            W, Wr = piece(h * 256, 256)
            for c in range(NCH):
                b = c % 2
                proj_chunk(W, Wr, c, 256, b)
                rope_to(qr, res("qr"), c, b, None)
                tr_to(QT, res("QT"), qr, res("qr"), c)
            W, Wr = piece(2048 + h * 256, 256)
            for c in range(NCH):
                b = c % 2
                proj_chunk(W, Wr, c, 256, b)
                rope_to(k_tm[:, c, :], res("k_tm"), c, b, 1.0 / 16.0)
                tr_to(KT, res("KT"), k_tm[:, c, :], res("k_tm"), c)
            for (c0_, dst_, dres_, fn_) in ((4096, v_tm, "v_tm", AF.Copy), (8192, sgf, "sgf", AF.Silu), (12288, sgb, "sgb", AF.Silu)):
                W, Wr = piece(c0_ + h * 512, 512)
                for t in range(NT):
                    b = t % 2
                    sslot = stg_i[0] % 2
                    stg_i[0] += 1
                    for kc in range(16):
                        S.add("pe", lambda e, kc=kc, t=t, b=b, W=W: e.matmul(ps[b][:, :], hT[:, kc, t * 128:(t + 1) * 128], W[:, kc, :], start=(kc == 0), stop=(kc == 15)),
                              reads=[Wr, HT[t]], writes=[PS[b]])
                    S.add("act", lambda e, t=t, b=b, dst_=dst_, fn_=fn_: e.activation(dst_[:, 2 * t, :], ps[b][0:64, :], fn_), reads=[PS[b]], writes=[res(dres_)])
                    S.add("act", lambda e, b=b, sslot=sslot, fn_=fn_: e.activation(stg[sslot][64:128, :], ps[b][64:128, :], fn_), reads=[PS[b]], writes=[res("stg%d" % sslot)])
                    S.dma("sp", lambda e, t=t, sslot=sslot, dst_=dst_: e.dma_start(out=dst_[:, 2 * t + 1, :], in_=stg[sslot][64:128, :]),
                          reads=[res("stg%d" % sslot)], writes=[res(dres_)], key="stg%d" % sslot)
            S.dma("sp", lambda e, h=h: e.dma_start(out=gng, in_=gn_g[h, :].partition_broadcast(64)), writes=[res("gng")], key="gn")
            S.dma("sp", lambda e, h=h: e.dma_start(out=gnb, in_=gn_b[h, :].partition_broadcast(64)), writes=[res("gnb")], key="gn")
            for dr in range(2):
                col = dr * 8 + h
                lgc = lg[:, col:col + 1]
                dif = cst[0:64, 384:448] if dr == 0 else cst[0:64, 512:576]
                tri = cst[0:64, 448:512] if dr == 0 else cst[0:64, 576:640]
                ramp = cst[:, 256:320] if dr == 0 else cst[:, 320:384]
                DTd, qdd, kdd = DT2[dr], qdec2[dr], kd2[dr]
                S.add("act", lambda e, dif=dif, lgc=lgc, DTd=DTd: e.activation(DTd, dif, AF.Exp, scale=lgc[0:64, :]), reads=[res("cst"), res("lg")], writes=[res("DT%d" % dr)])
                S.add("dve", lambda e, tri=tri, DTd=DTd: e.tensor_tensor(DTd, DTd, tri, op=ALU.mult), reads=[res("DT%d" % dr), res("cst")], writes=[res("DT%d" % dr)])
                for dc in range(2):
                    S.add("act", lambda e, dc=dc, ramp=ramp, lgc=lgc, qdd=qdd: e.activation(qdd[:, dc, :], ramp, AF.Exp, scale=lgc), reads=[res("cst"), res("lg")], writes=[res("qdec%d" % dr)])
                S.add("act", lambda e, dr=dr, lgc=lgc, kdd=kdd: e.activation(kdd[:, 0:1], cst[0:64, 640 + dr:641 + dr], AF.Exp, scale=lgc[0:64, :]), reads=[res("cst"), res("lg")], writes=[res("kd%d" % dr)])
                S.add("act", lambda e, dr=dr, lgc=lgc: e.activation(small[:, 20 + dr:21 + dr], cst[:, 642:643], AF.Exp, scale=lgc), reads=[res("cst"), res("lg")], writes=[res("cdec%d" % dr)])
                S.dma("sp", lambda e, dr=dr, h=h: e.dma_start(out=St2[dr], in_=s0[dr, h].rearrange("(dc p) v -> p dc v", p=128)), writes=[res("St%d" % dr)], key="s0%d" % dr)
            for idx in range(NCH):
                for dr in range(2):
                    c = idx if dr == 0 else NCH - 1 - idx
                    Std, Sbd, DTd, qdd, kdd = St2[dr], Sb2[dr], DT2[dr], qdec2[dr], kd2[dr]
                    Pmd, Qdd, Kdd, t1d = Pm2[dr], Qd2[dr], Kd2[dr], t12[dr]
                    bI, bO, bK = (2, 3, 4) if dr == 0 else (5, 6, 0)
                    sm0 = 0 if dr == 0 else 4
                    rSt, rSb, rSm, rT1 = res("St%d" % dr), res("Sb%d" % dr), res("small%d" % dr), res("t1%d" % dr)
                    if idx % 4 == 0 and idx > 0:
                        S.add("dve", lambda e, Std=Std: e.tensor_scalar(Std, Std, flg[:, 0:1], None, op0=ALU.mult), reads=[rSt, res("flg")], writes=[rSt])
                    for dc in range(2):
                        S.add("pe", lambda e, dc=dc, c=c, bI=bI: e.matmul(ps[bI][0:64, 0:64], KT[:, dc, c * 64:(c + 1) * 64], QT[:, dc, c * 64:(c + 1) * 64], start=(dc == 0), stop=(dc == 1)),
                              reads=[res("KT"), res("QT")], writes=[PS[bI]])
                    S.add("dve", lambda e, bI=bI, Pmd=Pmd, DTd=DTd: e.tensor_tensor(Pmd, ps[bI][0:64, 0:64], DTd, op=ALU.mult), reads=[PS[bI], res("DT%d" % dr)], writes=[res("Pm%d" % dr)])
                    S.add("pool", lambda e, c=c, Qdd=Qdd, qdd=qdd: e.tensor_tensor(Qdd, QT[:, :, c * 64:(c + 1) * 64], qdd, op=ALU.mult), reads=[res("QT"), res("qdec%d" % dr)], writes=[res("Qd%d" % dr)])
                    S.add("act", lambda e, Sbd=Sbd, Std=Std: e.copy(Sbd, Std), reads=[rSt], writes=[rSb])
                    S.add("pe", lambda e, c=c, bO=bO, Pmd=Pmd: e.matmul(ps[bO][0:64, :], Pmd, v_tm[:, c, :], start=True, stop=False), reads=[res("Pm%d" % dr), res("v_tm")], writes=[PS[bO]])
                    for dc in range(2):
                        S.add("pe", lambda e, dc=dc, bO=bO, Qdd=Qdd, Sbd=Sbd: e.matmul(ps[bO][0:64, :], Qdd[:, dc, :], Sbd[:, dc, :], start=False, stop=(dc == 1)), reads=[res("Qd%d" % dr), rSb], writes=[PS[bO]])
                    S.add("dve", lambda e, c=c, Kdd=Kdd, kdd=kdd: e.tensor_scalar(Kdd, k_tm[:, c, :], kdd[:, 0:1], None, op0=ALU.mult), reads=[res("k_tm"), res("kd%d" % dr)], writes=[res("Kd%d" % dr)])
                    for dc in range(2):
                        S.add("pe", lambda e, dc=dc, c=c, bK=bK, Kdd=Kdd: e.matmul(ps[bK][:, :], Kdd[:, dc * 128:(dc + 1) * 128], v_tm[:, c, :], start=True, stop=True),
                              reads=[res("Kd%d" % dr), res("v_tm")], writes=[PS[bK]])
                        S.add("dve", lambda e, dc=dc, bK=bK, Std=Std, dr=dr: e.scalar_tensor_tensor(Std[:, dc, :], Std[:, dc, :], small[:, 20 + dr:21 + dr], ps[bK][:, :], op0=ALU.mult, op1=ALU.add),
                              reads=[rSt, res("cdec%d" % dr), PS[bK]], writes=[rSt])
                    S.add("dve", lambda e, bO=bO, dr=dr: e.bn_stats(stt[0:64, dr, :], ps[bO][0:64, :]), reads=[PS[bO]], writes=[res("stt%d" % dr)])
                    S.add("dve", lambda e, dr=dr, sm0=sm0: e.bn_aggr(small[0:64, sm0:sm0 + 2], stt[0:64, dr:dr + 1, :]), reads=[res("stt%d" % dr)], writes=[rSm])
                    S.add("dve", lambda e, sm0=sm0: e.tensor_scalar(small[0:64, sm0 + 2:sm0 + 3], small[0:64, sm0 + 1:sm0 + 2], EPS, None, op0=ALU.add), reads=[rSm], writes=[rSm])
                    S.add("act", lambda e, sm0=sm0: e.activation(small[0:64, sm0 + 2:sm0 + 3], small[0:64, sm0 + 2:sm0 + 3], AF.Ln), reads=[rSm], writes=[rSm])
                    S.add("act", lambda e, sm0=sm0: e.activation(small[0:64, sm0 + 2:sm0 + 3], small[0:64, sm0 + 2:sm0 + 3], AF.Exp, scale=-0.5), reads=[rSm], writes=[rSm])
                    S.add("dve", lambda e, bO=bO, sm0=sm0, t1d=t1d: e.tensor_scalar(t1d, ps[bO][0:64, :], small[0:64, sm0:sm0 + 1], small[0:64, sm0 + 2:sm0 + 3], op0=ALU.subtract, op1=ALU.mult),
                          reads=[PS[bO], rSm], writes=[rT1])
                    S.add("dve", lambda e, t1d=t1d: e.tensor_tensor(t1d, t1d, gng, op=ALU.mult), reads=[rT1, res("gng")], writes=[rT1])
                    S.add("dve", lambda e, t1d=t1d: e.tensor_tensor(t1d, t1d, gnb, op=ALU.add), reads=[rT1, res("gnb")], writes=[rT1])
                    if dr == 0:
                        S.add("dve", lambda e, c=c, t1d=t1d: e.tensor_tensor(yf[:, c, :], t1d, sgf[:, c, :], op=ALU.mult), reads=[rT1, res("sgf")], writes=[res("yf%d" % c)])
                    else:
                        S.add("dve", lambda e, c=c, t1d=t1d: e.tensor_tensor(ybk[:, c, :], t1d, sgb[:, c, :], op=ALU.mult), reads=[rT1, res("sgb")], writes=[res("ybk%d" % c)])
                    if idx % 4 == 3:
                        sq = c // 4
                        S.dma("sp", lambda e, sq=sq, dr=dr, h=h, Std=Std: e.dma_start(out=st_out[sq, dr, h].rearrange("(dc p) v -> p dc v", p=128), in_=Std),
                              reads=[rSt], writes=[res("st_out")], key="so%d" % dr)
            for c in range(NCH):
                S.add("pool", lambda e, c=c: e.tensor_tensor(yf[:, c, :], yf[:, c, :], ybk[:, c, :], op=ALU.add), reads=[res("yf%d" % c), res("ybk%d" % c)], writes=[res("yf%d" % c)])
                po = 256 + (c % 2) * 256
                for vc in range(4):
                    S.add("pe", lambda e, vc=vc, c=c, po=po: e.transpose(psb[:, po + vc * 64:po + (vc + 1) * 64], yf[:, c, vc * 128:(vc + 1) * 128], identb[0:64, 0:64]),
                          reads=[res("yf%d" % c), res("identb")], writes=[PSB])
                evac(yTh[:, :, c * 64:(c + 1) * 64], psb[:, po:po + 256].rearrange("p (a b) -> p a b", b=64), [PSB], [res("rc")])
            S.dma("sp", lambda e, h=h: e.dma_start(out=yT_dram[:, h * 4:(h + 1) * 4, :], in_=yTh), reads=[res("rc")], writes=[res("yT_dram")], key="yo")
        S.barrier()

    def na_block(l, x_src):
        A.reset()
        hT = A.bf16([128, 16, NTOK])
        mark0 = A.off
        prologue(l, x_src, hT, 0, 1)
        S.barrier()
        A.off = mark0
        HT = [res("hT%d" % t) for t in range(NT)]
        wq = A.bf16([128, 16, 128]); wk = A.bf16([128, 16, 128]); wv = A.bf16([128, 16, 128])
        QT = A.bf16([128, NTOK]); KT = A.bf16([128, NTOK])
        Va = A.bf16([64, NCH, 130]); Vc = A.bf16([64, 8, 130])
        KcT = A.bf16([128, 512])
        kc32 = A.f32([128, 4, 128])
        ko = A.f32([64, NCH, 128]); vo = A.f32([64, NCH, 128])
        KT32 = A.f32([128, NTOK]); VT32 = A.f32([128, NTOK])
        bl = A.bf16([64, 15, 64]); rm = A.bf16([1, 16, 16, 64])
        PT2 = [A.bf16([64, 16, 64]) for _ in range(2)]
        ao2 = [A.bf16([64, 128]) for _ in range(2)]; rec2 = [A.f32([64, 2]) for _ in range(2)]
        aT = A.bf16([128, NTOK])
        S.dma("pool", lambda e: e.dma_start(out=rm, in_=narow[:, :, :, :]), writes=[res("rm")], key="nb")
        for h in range(16):
            stream_w(wq, w_na_in[:, h * 128:(h + 1) * 128].rearrange("(kc p) c -> p kc c", p=128), res("wq"), "wq")
            stream_w(wk, w_na_in[:, D + h * 128:D + (h + 1) * 128].rearrange("(kc p) c -> p kc c", p=128), res("wk"), "wk")
            stream_w(wv, w_na_in[:, 2 * D + h * 128:2 * D + (h + 1) * 128].rearrange("(kc p) c -> p kc c", p=128), res("wv"), "wv")
            S.dma("pool", lambda e, h=h: e.dma_start(out=bl, in_=nabias[:, h, :, :]), writes=[res("bl")], key="nb")
            S.dma("sp", lambda e, h=h: e.dma_start(out=kc32, in_=ck[h].rearrange("(a p) d -> p a d", p=128)), writes=[res("kc32")], key="ck")
            S.dma("pool", lambda e, h=h: e.dma_start(out=Vc[:, :, 0:128], in_=cv[h].rearrange("(a p) d -> p a d", p=64)), writes=[res("Vc")], key="cv")
            S.add("dve", lambda e: e.tensor_copy(Vc[:, :, 128:129], cst[0:64, 128:136].rearrange("p (a b) -> p a b", b=1)), reads=[res("cst")], writes=[res("Vc")])
            S.add("dve", lambda e: e.tensor_copy(Va[:, :, 128:129], cst[0:64, 128:144].rearrange("p (a b) -> p a b", b=1)), reads=[res("cst")], writes=[res("Va")])
            for a in range(4):
                S.add("pe", lambda e, a=a: e.transpose(ps[6][:, a * 128:(a + 1) * 128], kc32[:, a, :], ident[:]), reads=[res("kc32"), res("ident")], writes=[PS[6]])
            evac(KcT, ps[6][:], [PS[6]], [res("KcT")])
            for th in range(2):
                for kc in range(16):
                    S.add("pe", lambda e, kc=kc, th=th: e.matmul(ps[0][:], wq[:, kc, :], hT[:, kc, th * 512:(th + 1) * 512], start=(kc == 0), stop=(kc == 15)),
                          reads=[res("wq")] + HT[th * 4:(th + 1) * 4], writes=[PS[0]])
                evac(QT[:, th * 512:(th + 1) * 512], ps[0][:], [PS[0]], [res("QT")], scale=128.0 ** -0.5)
                for kc in range(16):
                    S.add("pe", lambda e, kc=kc, th=th: e.matmul(ps[1][:], wk[:, kc, :], hT[:, kc, th * 512:(th + 1) * 512], start=(kc == 0), stop=(kc == 15)),
                          reads=[res("wk")] + HT[th * 4:(th + 1) * 4], writes=[PS[1]])
                evac(KT32[:, th * 512:(th + 1) * 512], ps[1][:], [PS[1]], [res("KT32")])
                S.add("pool", lambda e, th=th: e.tensor_copy(KT[:, th * 512:(th + 1) * 512], KT32[:, th * 512:(th + 1) * 512]), reads=[res("KT32")], writes=[res("KT")])
                for kc in range(16):
                    S.add("pe", lambda e, kc=kc, th=th: e.matmul(ps[6][:], wv[:, kc, :], hT[:, kc, th * 512:(th + 1) * 512], start=(kc == 0), stop=(kc == 15)),
                          reads=[res("wv")] + HT[th * 4:(th + 1) * 4], writes=[PS[6]])
                evac(VT32[:, th * 512:(th + 1) * 512], ps[6][:], [PS[6]], [res("VT32")])
            for r in range(NCH):
                b = 2 + (r % 2)
                S.add("pe", lambda e, r=r, b=b: e.transpose(ps[b][0:64, 0:128], KT32[:, r * 64:(r + 1) * 64], ident[:]), reads=[res("KT32"), res("ident")], writes=[PS[b]])
                S.add("pe", lambda e, r=r, b=b: e.transpose(ps[b][0:64, 128:256], VT32[:, r * 64:(r + 1) * 64], ident[:]), reads=[res("VT32"), res("ident")], writes=[PS[b]])
                S.add("act", lambda e, r=r, b=b: e.copy(ko[:, r, :], ps[b][0:64, 0:128]), reads=[PS[b]], writes=[res("ko"), res("pstok%d" % b)])
                S.add("act", lambda e, r=r, b=b: e.copy(Va[:, r, 0:128], ps[b][0:64, 128:256]), reads=[PS[b]], writes=[res("Va"), res("pstok%d" % b)])
                S.add("dve", lambda e, r=r, b=b: e.tensor_copy(vo[:, r, :], ps[b][0:64, 128:256]), reads=[PS[b]], writes=[res("vo"), res("pstok%d" % b)])
            S.dma("sp", lambda e, h=h: e.dma_start(out=k_out[h].rearrange("(r p) d -> p r d", p=64), in_=ko), reads=[res("ko")], writes=[res("k_out")], key="kvo")
            S.dma("sp", lambda e, h=h: e.dma_start(out=v_out[h].rearrange("(r p) d -> p r d", p=64), in_=vo), reads=[res("vo")], writes=[res("v_out")], key="kvo")
            for r in range(NCH):
                r0 = min(max(r - 4, 0), 8)
                PT, ao, rec = PT2[r % 2], ao2[r % 2], rec2[r % 2]
                rPT, rao, rrec = res("PT%d" % (r % 2)), res("ao%d" % (r % 2)), res("rec%d" % (r % 2))
                pb = 4 + (r % 2)
                S4 = ps[pb][0:64, :].rearrange("p (j q) -> p j q", q=64)
                pb2 = 0 if pb == 4 else 1
                for j in range(16):
                    bank, jj = (pb, j) if j < 8 else (pb2, j - 8)
                    dst = ps[bank][0:64, jj * 64:(jj + 1) * 64]
                    if j < 8:
                        kr = r0 + j
                        drr = kr - r + 7
                        S.add("pe", lambda e, dst=dst, kr=kr, r=r: e.matmul(dst, KT[:, kr * 64:(kr + 1) * 64], QT[:, r * 64:(r + 1) * 64], start=True, stop=False),
                              reads=[res("KT"), res("QT")], writes=[PS[bank]])
                        S.add("pe", lambda e, dst=dst, drr=drr: e.matmul(dst, identb[0:64, 0:64], bl[:, drr, :], start=False, stop=False),
                              reads=[res("identb"), res("bl")], writes=[PS[bank]])
                        S.add("pe", lambda e, dst=dst, r=r, j=j: e.matmul(dst, rm[0:1, r, j, :], onesb[0:1, 0:64], start=False, stop=True),
                              reads=[res("rm"), res("onesb")], writes=[PS[bank]])
                    else:
                        p = j - 8
                        S.add("pe", lambda e, dst=dst, p=p, r=r: e.matmul(dst, KcT[:, p * 64:(p + 1) * 64], QT[:, r * 64:(r + 1) * 64], start=True, stop=True),
                              reads=[res("KcT"), res("QT")], writes=[PS[bank]])
                S.add("act", lambda e, pb=pb, PT=PT: e.activation(PT[:, 0:8, :], ps[pb][0:64, :].rearrange("p (j q) -> p j q", q=64), AF.Exp), reads=[PS[pb]], writes=[rPT])
                S.add("act", lambda e, pb2=pb2, PT=PT: e.activation(PT[:, 8:16, :], ps[pb2][0:64, :].rearrange("p (j q) -> p j q", q=64), AF.Exp, bias=flg[0:64, 1:2]),
                      reads=[PS[pb2], res("flg")], writes=[rPT])
                ob = 2 + (r % 2)
                for j in range(16):
                    rhs = Va[:, r0 + j, 0:129] if j < 8 else Vc[:, j - 8, 0:129]
                    S.add("pe", lambda e, j=j, rhs=rhs, ob=ob, PT=PT: e.matmul(ps[ob][0:64, 0:129], PT[:, j, :], rhs, start=(j == 0), stop=(j == 15)),
                          reads=[rPT, res("Va"), res("Vc")], writes=[PS[ob]])
                S.add("dve", lambda e, ob=ob, rec=rec: e.reciprocal(rec[:, 0:1], ps[ob][0:64, 128:129]), reads=[PS[ob]], writes=[rrec])
                S.add("dve", lambda e, ob=ob, ao=ao, rec=rec: e.tensor_scalar(ao, ps[ob][0:64, 0:128], rec[:, 0:1], None, op0=ALU.mult), reads=[PS[ob], rrec], writes=[rao])
                S.add("pe", lambda e, r=r, ao=ao: e.transpose(psb[:, 512 + (r % 2) * 64:512 + (r % 2 + 1) * 64], ao, identb[0:64, 0:64]), reads=[rao, res("identb")], writes=[PSB])
                evac(aT[:, r * 64:(r + 1) * 64], psb[:, 512 + (r % 2) * 64:512 + (r % 2 + 1) * 64], [PSB], [res("aT")])
            S.dma("sp", lambda e, h=h: e.dma_start(out=yT_dram[:, h, :], in_=aT), reads=[res("aT")], writes=[res("yT_dram")], key="yo")
        S.barrier()

    if upto >= 1:
        retention_block(0, x_in)
    if upto >= 2:
        outproj_block(0, 32, w_ret_out, x_in, xs)
    if upto >= 3:
        moe_block(0, xs, xs, 1)
    if upto >= 4:
        na_block(1, xs)
    if upto >= 5:
        outproj_block(1, 16, w_na_out, xs, xs)
    if upto >= 6:
        moe_block(1, xs, y_out, None)

    block = E(nc.Block())
    S.emit(stack, block)
    stack.close()
    return nc


def _consts():
    c = np.zeros((128, 656), np.float32)
    c[:, 0:128] = np.eye(128, dtype=np.float32)
    c[:, 128:256] = 1.0
    i = np.arange(64, dtype=np.float32)
    c[:, 256:320] = (i + 1.0)[None, :]
    c[:, 320:384] = (64.0 - i)[None, :]
    jj, ii = np.meshgrid(i, i, indexing="ij")
    c[0:64, 384:448] = np.maximum(ii - jj, 0.0)
    c[0:64, 448:512] = (ii >= jj).astype(np.float32)
    c[0:64, 512:576] = np.maximum(jj - ii, 0.0)
    c[0:64, 576:640] = (jj >= ii).astype(np.float32)
    c[0:64, 640] = 63.0 - i
    c[0:64, 641] = i
    c[:, 642] = 64.0
    return c


def _rope_tables(is_sample):
    nf = 64
    t = np.arange(NTOK)
    cos2 = np.ones((NTOK, 2, 2, nf), np.float32)
    sin2 = np.zeros((NTOK, 2, 2, nf), np.float32)
    if is_sample:
        rows = (t // 64).astype(np.float32)
        cols = (t % 64).astype(np.float32)
        inv = (np.float32(10000.0) ** (-np.arange(nf, dtype=np.float32) / np.float32(nf))).astype(np.float32)
        ang = np.stack([rows[:, None] * inv, cols[:, None] * inv], axis=1).astype(np.float32)
        cs, sn = np.cos(ang).astype(np.float32), np.sin(ang).astype(np.float32)
        cos2[:, :, 0, :] = cs
        cos2[:, :, 1, :] = cs
        sin2[:, :, 0, :] = -sn
        sin2[:, :, 1, :] = sn
    rc = cos2.reshape(NCH, 64, 256).transpose(1, 0, 2)
    rs = sin2.reshape(NCH, 64, 256).transpose(1, 0, 2)
    return np.ascontiguousarray(rc), np.ascontiguousarray(rs)


def _na_tables(is_sample, rpb):
    bias = np.zeros((64, 16, 15, 64), np.float32)
    rowm = np.zeros((1, 16, 16, 64), np.float32)
    col = np.arange(64)
    if is_sample:
        c0 = np.clip(col - 8, 0, 48)
        ok = (col[None, :] >= c0[:, None]) & (col[None, :] < c0[:, None] + 16)
        dc = np.clip(col[None, :] - col[:, None], -15, 15) + 15
        g = rpb[:, :, dc]
        g = np.where(ok[None, None], g, np.float32(NEG))
        bias[:] = g.transpose(3, 0, 1, 2)
    else:
        for r in range(16):
            r0 = min(max(r - 4, 0), 8)
            for j in range(16):
                if j < 8:
                    if (r0 + j) // 4 != r // 4:
                        rowm[0, r, j, :] = NEG
                else:
                    rowm[0, r, j, :] = NEG
    return bias, rowm


_NC_CACHE = {}


def _make_in_maps(x_prompt, x_sample, state_ret, cache_na_k, cache_na_v, c, c_ctx, w_mod, b_mod, ln1_g, ln1_b, ln2_g, ln2_b,
           w_ret_in, ret_decay_logit, ret_gn_g, ret_gn_b, w_ret_out, w_na_in, na_rpb, w_na_out, w_rg, b_rg, w_re, b_re,
           w_gate, w_up, w_down):
    f = lambda a: np.ascontiguousarray(np.asarray(a, dtype=np.float32))
    x_prompt, x_sample, state_ret, cache_na_k, cache_na_v = map(f, (x_prompt, x_sample, state_ret, cache_na_k, cache_na_v))
    c, c_ctx = f(c), f(c_ctx)
    w_rt = np.concatenate([f(w_rg), f(w_re).transpose(0, 2, 1, 3).reshape(2, D, 32)], axis=2)
    b_rt = np.concatenate([f(b_rg), f(b_re).reshape(2, 32)], axis=1)
    shared = {
        "consts": _consts(), "w_mod0": f(w_mod)[0], "w_mod1": f(w_mod)[1], "b_mod": f(b_mod), "ln1_g": f(ln1_g), "ln1_b": f(ln1_b), "ln2_g": f(ln2_g), "ln2_b": f(ln2_b),
        "w_ret_in": f(w_ret_in)[0], "decay": f(ret_decay_logit).reshape(1, 16), "gn_g": f(ret_gn_g)[0], "gn_b": f(ret_gn_b)[0],
        "w_ret_out": f(w_ret_out)[0], "w_na_in": f(w_na_in)[0], "w_na_out": f(w_na_out)[0], "w_rt": np.ascontiguousarray(w_rt), "b_rt": np.ascontiguousarray(b_rt),
        "w_gate0": f(w_gate)[0], "w_gate1": f(w_gate)[1], "w_up0": f(w_up)[0], "w_up1": f(w_up)[1],
        "w_down0": f(w_down)[0], "w_down1": f(w_down)[1],
    }
    rpb = f(na_rpb)[0]
    in_maps = []
    for core in range(8):
        smp = core >= 4
        m = dict(shared)
        if smp:
            b = core - 4
            m["x_in"] = x_sample[b]
            cv_ = c[b]
            m["s0"] = np.ascontiguousarray(state_ret[b, 0])
            m["ck"] = np.ascontiguousarray(cache_na_k[b, 0]); m["cv"] = np.ascontiguousarray(cache_na_v[b, 0])
        else:
            m["x_in"] = np.ascontiguousarray(x_prompt[4 * core:4 * core + 4].reshape(NTOK, D))
            cv_ = c_ctx
            m["s0"] = np.zeros((2, 8, 256, 512), np.float32)
            m["ck"] = np.zeros((16, 512, 128), np.float32); m["cv"] = np.zeros((16, 512, 128), np.float32)
        m["cvec"] = np.ascontiguousarray(cv_.reshape(16, 128).T)
        fl = np.zeros((1, 8), np.float32)
        fl[0, 0] = 1.0 if smp else 0.0
        fl[0, 1] = 0.0 if smp else NEG
        m["flags"] = fl
        m["ropec"], m["ropes"] = _rope_tables(smp)
        m["nabias"], m["narow"] = _na_tables(smp, rpb)
        in_maps.append(m)
    return in_maps


def _assemble(R_):
    y_prompt = np.concatenate([R_[i]["y_out"].reshape(4, 256, D) for i in range(4)], axis=0)
    y_sample = np.stack([R_[4 + i]["y_out"] for i in range(4)], axis=0)
    new_state = np.concatenate([R_[i]["st_out"] for i in range(4)], axis=0)[:, None]
    def kv(name):
        parts = []
        for i in range(4):
            a = R_[i][name].reshape(16, 4, 256, 128).transpose(1, 0, 2, 3)
            parts.append(a)
        return np.ascontiguousarray(np.concatenate(parts, axis=0)[:, None])
    return (np.ascontiguousarray(y_prompt), np.ascontiguousarray(y_sample), np.ascontiguousarray(new_state), kv("k_out"), kv("v_out"))


def kernel(**inputs):
    in_maps = _make_in_maps(**inputs)
    if "nc" not in _NC_CACHE:
        _NC_CACHE["nc"] = build()
    res = run_bass_kernel_spmd(_NC_CACHE["nc"], in_maps, core_ids=list(range(8)))
    return _assemble(res.results)
```

```python
from contextlib import ExitStack
import numpy as np
import concourse.bass as bass
import concourse.mybir as mybir
from concourse.bass_utils import run_bass_kernel_spmd

F32 = mybir.dt.float32
BF16 = mybir.dt.bfloat16
AF = mybir.ActivationFunctionType
ALU = mybir.AluOpType

D = 2048
NTOK = 1024
NT = 8
NCH = 16
ALPHA = (2.0 * 2) ** 0.25
EPS = 1e-5
NEG = -1e30
MOE_STAGE = 3


class Res:
    __slots__ = ("name", "last_w", "readers")

    def __init__(self, name=""):
        self.name = name
        self.last_w = None
        self.readers = {}


class Op:
    __slots__ = ("eng", "fn", "deps", "signal", "count", "is_dma", "dma_key", "epoch")

    def __init__(self, eng, fn):
        self.epoch = 0
        self.eng = eng
        self.fn = fn
        self.deps = []
        self.signal = False
        self.count = 0
        self.is_dma = False
        self.dma_key = None


ENGINES = ("pe", "act", "dve", "pool", "sp")


class Sched:
    def __init__(self, nc):
        self.nc = nc
        self.ops = {e: [] for e in ENGINES}
        self.dma_count = {}
        self.dma_kind = {}
        self.last_dma = {}
        self.epoch = 0

    def _track(self, op, reads, writes):
        op.epoch = self.epoch
        deps = []
        for r in reads:
            if r.last_w is not None:
                deps.append(r.last_w)
        for w in writes:
            if w.last_w is not None:
                deps.append(w.last_w)
            deps.extend(w.readers.values())
        seen = set()
        for d in deps:
            if d is op or id(d) in seen:
                continue
            seen.add(id(d))
            if d.eng == "pe" and op.eng == "pe" and not d.is_dma and not op.is_dma:
                continue
            if d.epoch < self.epoch:
                continue
            op.deps.append((d, self.dma_count[d.dma_key] if d.is_dma else None))
            d.signal = True
        for r in reads:
            r.readers[op.dma_key if op.is_dma else op.eng] = op
        for w in writes:
            w.last_w = op
            w.readers = {}

    def add(self, eng, fn, reads=(), writes=()):
        op = Op(eng, fn)
        self._track(op, reads, writes)
        self.ops[eng].append(op)
        return op

    def dma(self, eng, fn, reads=(), writes=(), key="d"):
        kind = "sw" if eng == "pool" else "hw"
        key = key + "_" + kind
        self.dma_kind[key] = kind
        op = Op(eng, fn)
        op.is_dma = True
        op.dma_key = key
        self.dma_count.setdefault(key, 0)
        self._track(op, reads, writes)
        self.dma_count[key] += 1
        op.count = self.dma_count[key]
        self.ops[eng].append(op)
        self.last_dma[key] = op
        return op

    def barrier(self):
        lasts = []
        for e in ENGINES:
            for op in reversed(self.ops[e]):
                if not op.is_dma:
                    lasts.append(op)
                    break
        lasts.extend(self.last_dma.values())
        for e in ENGINES:
            op = Op(e, lambda eng: eng.nop(nofuse=True))
            for d in lasts:
                if d.eng == e and not d.is_dma:
                    continue
                op.deps.append((d, self.dma_count[d.dma_key] if d.is_dma else None))
                d.signal = True
            op.epoch = self.epoch
            self.ops[e].append(op)
        self.epoch += 1

    def emit(self, stack, block):
        nc = self.nc
        sems = {(e, ep): stack.enter_context(nc.semaphore("s_%s_%d" % (e, ep))) for e in ENGINES for ep in range(self.epoch + 1)}
        dsems = {k: stack.enter_context(nc.semaphore("d_" + k)) for k in self.dma_kind}
        for e in ENGINES:
            c = {}
            for op in self.ops[e]:
                if not op.is_dma and op.signal:
                    c[op.epoch] = c.get(op.epoch, 0) + 1
                    op.count = c[op.epoch]
            print("sched", e, "signals per epoch", c)

        def run(e, engine):
            known = {}
            for op in self.ops[e]:
                need = {}
                for d, dc_ in op.deps:
                    if d.is_dma:
                        s, v = dsems[d.dma_key], 16 * dc_
                    else:
                        s, v = sems[(d.eng, d.epoch)], d.count
                    k = id(s)
                    if k not in need or need[k][1] < v:
                        need[k] = (s, v)
                for k, (s, v) in need.items():
                    if known.get(k, 0) >= v:
                        continue
                    engine.wait_ge(s, v)
                    known[k] = v
                ins = op.fn(engine)
                if op.is_dma:
                    ins.then_inc(dsems[op.dma_key], 16)
                elif op.signal:
                    ins.then_inc(sems[(e, op.epoch)], 1)
            if e in ("pool", "sp"):
                kind = "sw" if e == "pool" else "hw"
                for k, kd in self.dma_kind.items():
                    if kd == kind:
                        engine.wait_ge(dsems[k], 16 * self.dma_count[k])

        block.tensor(lambda eng: run("pe", eng))
        block.scalar(lambda eng: run("act", eng))
        block.vector(lambda eng: run("dve", eng))
        block.gpsimd(lambda eng: run("pool", eng))
        block.sync(lambda eng: run("sp", eng))


def build(upto=6):
    nc = bass.Bass("TRN2", target_bir_lowering=False)

    def din(name, shape, dt=F32):
        return nc.dram_tensor(name, list(shape), dt, kind="ExternalInput").ap()

    def dout(name, shape, dt=F32):
        return nc.dram_tensor(name, list(shape), dt, kind="ExternalOutput").ap()

    def dint(name, shape, dt=F32):
        return nc.dram_tensor(name, list(shape), dt).ap()

    x_in = din("x_in", [NTOK, D])
    cvec = din("cvec", [128, 16])
    s0 = din("s0", [2, 8, 256, 512])
    flags = din("flags", [1, 8])
    ropec = din("ropec", [64, NCH, 256])
    ropes = din("ropes", [64, NCH, 256])
    consts = din("consts", [128, 656])
    ck = din("ck", [16, 512, 128])
    cv = din("cv", [16, 512, 128])
    nabias = din("nabias", [64, 16, 128, 64])
    w_mod = [din("w_mod%d" % i, [D, 6 * D]) for i in range(2)]
    b_mod = din("b_mod", [2, 6 * D])
    ln1_g = din("ln1_g", [2, D]); ln1_b = din("ln1_b", [2, D])
    ln2_g = din("ln2_g", [2, D]); ln2_b = din("ln2_b", [2, D])
    w_ret_in = din("w_ret_in", [D, 16384])
    decay = din("decay", [1, 16])
    gn_g = din("gn_g", [8, 512]); gn_b = din("gn_b", [8, 512])
    w_ret_out = din("w_ret_out", [4096, D])
    w_na_in = din("w_na_in", [D, 3 * D])
    w_na_out = din("w_na_out", [D, D])
    w_rt = din("w_rt", [2, D, 36])
    b_rt = din("b_rt", [2, 36])
    w_gate = [din("w_gate%d" % i, [32, D, 512]) for i in range(2)]
    w_up = [din("w_up%d" % i, [32, D, 512]) for i in range(2)]
    w_down = [din("w_down%d" % i, [32, 512, D]) for i in range(2)]

    y_out = dout("y_out", [NTOK, D])
    st_out = dout("st_out", [4, 2, 8, 256, 512])
    k_out = dout("k_out", [16, NTOK, 128])
    v_out = dout("v_out", [16, NTOK, 128])

    m_dram = dint("m_dram", [2, 6 * D])
    xs = dint("xs", [NTOK, D])
    yT_dram = dint("yT_dram", [128, 32, NTOK], BF16)

    S = Sched(nc)
    stack = ExitStack()
    E = stack.enter_context

    AW = 51530
    arena = E(nc.sbuf_tensor("arena", [128, AW], F32))
    ident = E(nc.sbuf_tensor("ident", [128, 128], F32))
    identb = E(nc.sbuf_tensor("identb", [128, 128], BF16))
    onesf = E(nc.sbuf_tensor("onesf", [128, 128], F32))
    onesb = E(nc.sbuf_tensor("onesb", [1, 64], BF16))
    cst = E(nc.sbuf_tensor("cst", [128, 656], F32))
    sil = E(nc.sbuf_tensor("sil", [128, 16], F32))
    flg = E(nc.sbuf_tensor("flg", [128, 8], F32))
    lg = E(nc.sbuf_tensor("lg", [128, 16], F32))
    small = E(nc.sbuf_tensor("small", [128, 64], F32))
    stt = E(nc.sbuf_tensor("stt", [128, 4, 6], F32))
    comb = E(nc.sbuf_tensor("comb", [128, NT, 32], F32))
    rl = E(nc.sbuf_tensor("rl", [128, 96], F32))
    rbias = E(nc.sbuf_tensor("rbias", [128, 2, 36], F32))

    ps = [E(nc.psum_tensor("ps%d" % i, [128, 512], F32)) for i in range(7)]
    psb = E(nc.psum_tensor("psb", [128, 1024], BF16))
    PS = [Res("ps%d" % i) for i in range(7)]
    PSB = Res("psb")

    R = {}

    def res(name):
        if name not in R:
            R[name] = Res(name)
        return R[name]

    class Arena:
        def __init__(self):
            self.off = 0

        def reset(self):
            self.off = 0

        def f32(self, shape):
            n = int(np.prod(shape[1:]))
            v = arena[0:shape[0], self.off:self.off + n]
            self.off += n
            assert self.off <= AW, ("arena overflow", self.off)
            return v if len(shape) == 2 else v.rearrange(
                "p (a b) -> p a b", b=shape[2]) if len(shape) == 3 else v.rearrange(
                "p (a b c) -> p a b c", b=shape[2], c=shape[3])

        def bf16(self, shape):
            n = int(np.prod(shape[1:]))
            assert n % 2 == 0
            v = arena[0:shape[0], self.off:self.off + n // 2].bitcast(BF16)
            self.off += n // 2
            assert self.off <= AW, ("arena overflow", self.off)
            return v if len(shape) == 2 else v.rearrange(
                "p (a b) -> p a b", b=shape[2]) if len(shape) == 3 else v.rearrange(
                "p (a b c) -> p a b c", b=shape[2], c=shape[3])

    A = Arena()

    S.dma("sp", lambda e: e.dma_start(out=cst[:], in_=consts[:, :]), writes=[res("cst")], key="c0")
    S.dma("sp", lambda e: e.dma_start(out=sil[:], in_=cvec[:, :]), writes=[res("sil")], key="c0")
    S.dma("sp", lambda e: e.dma_start(out=flg[:], in_=flags[0, :].partition_broadcast(128)), writes=[res("flg")], key="c0")
    S.dma("sp", lambda e: e.dma_start(out=lg[:], in_=decay[0, :].partition_broadcast(128)), writes=[res("lg")], key="c0")
    S.dma("sp", lambda e: e.dma_start(out=rbias[:, 0, :], in_=b_rt[0, :].partition_broadcast(128)), writes=[res("rbias")], key="c0")
    S.dma("sp", lambda e: e.dma_start(out=rbias[:, 1, :], in_=b_rt[1, :].partition_broadcast(128)), writes=[res("rbias")], key="c0")
    S.add("dve", lambda e: e.tensor_copy(ident[:], cst[:, 0:128]), reads=[res("cst")], writes=[res("ident")])
    S.add("dve", lambda e: e.tensor_copy(identb[:], cst[:, 0:128]), reads=[res("cst")], writes=[res("identb")])
    S.add("dve", lambda e: e.tensor_copy(onesf[:], cst[:, 128:256]), reads=[res("cst")], writes=[res("onesf")])
    S.add("dve", lambda e: e.tensor_copy(onesb[:], cst[0:1, 128:192]), reads=[res("cst")], writes=[res("onesb")])
    S.add("act", lambda e: e.activation(sil[:], sil[:], AF.Silu), reads=[res("sil")], writes=[res("sil")])
    S.add("act", lambda e: e.activation(lg[:], lg[:], AF.Sigmoid), reads=[res("lg")], writes=[res("lg")])
    S.add("act", lambda e: e.activation(lg[:], lg[:], AF.Ln), reads=[res("lg")], writes=[res("lg")])

    A.reset()
    macc = A.f32([128, D])
    mbias = A.f32([128, D])
    mring = [A.f32([128, D]) for _ in range(3)]
    MR = [Res("mring%d" % i) for i in range(3)]
    pi = 0
    for l in range(2):
        for cb in range(6):
            S.dma("sp", lambda e, l=l, cb=cb: e.dma_start(out=mbias[:], in_=b_mod[l, cb * D:(cb + 1) * D].partition_broadcast(128)),
                  writes=[res("mbias")], key="mb")
            for kc in range(16):
                slot = pi % 3
                pi += 1
                S.dma("sp", lambda e, l=l, cb=cb, kc=kc, slot=slot: e.dma_start(
                    out=mring[slot][:], in_=w_mod[l][kc * 128:(kc + 1) * 128, cb * D:(cb + 1) * D]),
                    writes=[MR[slot]], key="mr%d" % slot)
                if kc == 0:
                    S.add("dve", lambda e, kc=kc, slot=slot: e.tensor_scalar(macc[:], mring[slot][:], sil[:, kc:kc + 1], None, op0=ALU.mult),
                          reads=[MR[slot], res("sil")], writes=[res("macc")])
                else:
                    S.add("dve", lambda e, kc=kc, slot=slot: e.scalar_tensor_tensor(
                        macc[:], mring[slot][:], sil[:, kc:kc + 1], macc[:], op0=ALU.mult, op1=ALU.add),
                        reads=[MR[slot], res("sil"), res("macc")], writes=[res("macc")])
            for j in range(4):
                S.add("pe", lambda e, j=j: e.matmul(ps[j][:], onesf[:], macc[:, j * 512:(j + 1) * 512], start=True, stop=True),
                      reads=[res("onesf"), res("macc")], writes=[PS[j]])
                S.add("dve", lambda e, j=j: e.tensor_tensor(mbias[:, j * 512:(j + 1) * 512], ps[j][:], mbias[:, j * 512:(j + 1) * 512], op=ALU.add),
                      reads=[PS[j], res("mbias")], writes=[res("mbias")])
            S.dma("sp", lambda e, l=l, cb=cb: e.dma_start(out=m_dram[l:l + 1, cb * D:(cb + 1) * D], in_=mbias[0:1, :]),
                  reads=[res("mbias")], writes=[res("m_dram")], key="mo")
    S.barrier()

    def bc_load(dst, src_row, r, key):
        S.dma("sp", lambda e: e.dma_start(out=dst, in_=src_row.partition_broadcast(128)), reads=[res("m_dram")], writes=[r], key=key)

    ev_flip = [0]

    def evac(dst, src, rd, wr, scale=None):
        ev_flip[0] ^= 1
        if ev_flip[0]:
            if scale is None:
                S.add("act", lambda e: e.copy(dst, src), reads=rd, writes=wr)
            else:
                S.add("act", lambda e: e.mul(dst, src, scale), reads=rd, writes=wr)
        else:
            if scale is None:
                S.add("dve", lambda e: e.tensor_copy(dst, src), reads=rd, writes=wr)
            else:
                S.add("dve", lambda e: e.tensor_scalar(dst, src, scale, None, op0=ALU.mult), reads=rd, writes=wr)

    def to_feature_major(h_ap, h_res, hT, t, hT32=None):
        for g in range(4):
            b = g % 4
            for q in range(4):
                kc = g * 4 + q
                S.add("pe", lambda e, kc=kc, b=b, q=q: e.transpose(ps[b][:, q * 128:(q + 1) * 128], h_ap[:, kc * 128:(kc + 1) * 128], ident[:]),
                      reads=[h_res, res("ident")], writes=[PS[b]])
            evac(hT[:, g * 4:(g + 1) * 4, t * 128:(t + 1) * 128], ps[b][:].rearrange("p (a b) -> p a b", b=128),
                 [PS[b]], [res("hT%d" % t), res("pstok%d" % b)])
            if hT32 is not None:
                S.add("dve", lambda e, g=g, b=b: e.tensor_copy(hT32[:, g * 4:(g + 1) * 4, :], ps[b][:].rearrange("p (a b) -> p a b", b=128)),
                      reads=[PS[b]], writes=[res("hT32"), res("pstok%d" % b)])

    def layer_norm_tile(u, u_res, g_bc, b_bc, out_ap, out_res, rd):
        for j in range(4):
            S.add("dve", lambda e, j=j: e.bn_stats(stt[:, j, :], u[:, j * 512:(j + 1) * 512]), reads=[u_res], writes=[res("stt")])
        S.add("dve", lambda e: e.bn_aggr(small[:, 0:2], stt[:]), reads=[res("stt")], writes=[res("small")])
        S.add("dve", lambda e: e.tensor_scalar(small[:, 2:3], small[:, 1:2], EPS, None, op0=ALU.add), reads=[res("small")], writes=[res("small")])
        S.add("act", lambda e: e.activation(small[:, 2:3], small[:, 2:3], AF.Ln), reads=[res("small")], writes=[res("small")])
        S.add("act", lambda e: e.activation(small[:, 2:3], small[:, 2:3], AF.Exp, scale=-0.5), reads=[res("small")], writes=[res("small")])
        S.add("dve", lambda e: e.tensor_scalar(u, u, small[:, 0:1], small[:, 2:3], op0=ALU.subtract, op1=ALU.mult),
              reads=[u_res, res("small")], writes=[u_res])
        S.add("dve", lambda e: e.tensor_tensor(u, u, g_bc, op=ALU.mult), reads=[u_res] + rd, writes=[u_res])
        S.add("dve", lambda e: e.tensor_tensor(out_ap, u, b_bc, op=ALU.add), reads=[u_res] + rd, writes=[out_res])

    def stream_w(slot_ap, src_ap, slot_res, key):
        S.dma("pool", lambda e: e.dma_start(out=slot_ap, in_=src_ap), writes=[slot_res], key=key)

    def prologue(l, x_src, hT, j_shift, j_scale):
        sc = A.f32([128, D]); sh = A.f32([128, D])
        xw = [A.f32([128, D]) for _ in range(2)]
        bc_load(sc, m_dram[l, j_scale * D:(j_scale + 1) * D], res("sc"), "bc")
        bc_load(sh, m_dram[l, j_shift * D:(j_shift + 1) * D], res("sh"), "bc")
        S.add("dve", lambda e: e.tensor_scalar(sc, sc, 1.0, None, op0=ALU.add), reads=[res("sc")], writes=[res("sc")])
        for t in range(NT):
            xt = xw[t % 2]
            xr = res("xw%d" % (t % 2))
            S.dma("sp", lambda e, t=t, xt=xt: e.dma_start(out=xt, in_=x_src[t * 128:(t + 1) * 128, :]), reads=[res("xs")], writes=[xr], key="xl%d" % (t % 2))
            S.add("dve", lambda e, xt=xt: e.tensor_tensor(xt, xt, sc, op=ALU.mult), reads=[xr, res("sc")], writes=[xr])
            S.add("dve", lambda e, xt=xt: e.tensor_tensor(xt, xt, sh, op=ALU.add), reads=[xr, res("sh")], writes=[xr])
            to_feature_major(xt, xr, hT, t)

    def epilogue(l, x_src, o_half, th, gate_j, lng, lnb, next_mod, hT_next, want_router, x_dst, l_router):
        pass

    def moe_block(l, x_src, x_dst, next_l):
        A.reset()
        hT = A.bf16([128, 16, NTOK])
        yacc = A.f32([128, NT, D])
        hid = A.bf16([128, 4, NTOK])
        ring = [A.bf16([128, 8192]) for _ in range(3)]
        RG = [Res("ring%d" % i) for i in range(3)]
        sgt = [A.bf16([128, 512]) for _ in range(2)]
        mark = A.off
        sc = A.f32([128, D]); sh = A.f32([128, D])
        xw = [A.f32([128, D])] * 2
        hT32 = A.f32([128, 16, 128])
        wr32 = A.f32([128, 16, 36])
        bc_load(sc, m_dram[l, 4 * D:5 * D], res("sc"), "bc")
        bc_load(sh, m_dram[l, 3 * D:4 * D], res("sh"), "bc")
        S.add("dve", lambda e: e.tensor_scalar(sc, sc, 1.0, None, op0=ALU.add), reads=[res("sc")], writes=[res("sc")])
        S.dma("sp", lambda e: e.dma_start(out=wr32, in_=w_rt[l].rearrange("(kc p) c -> p kc c", p=128)), writes=[res("wr32")], key="wr")
        for t in range(NT):
            xt = xw[0]
            xr = res("xw0")
            S.dma("sp", lambda e, t=t, xt=xt: e.dma_start(out=xt, in_=x_src[t * 128:(t + 1) * 128, :]), reads=[res("xs")], writes=[xr], key="xl0")
            S.add("dve", lambda e, xt=xt: e.tensor_tensor(xt, xt, sc, op=ALU.mult), reads=[xr, res("sc")], writes=[xr])
            S.add("dve", lambda e, xt=xt: e.tensor_tensor(xt, xt, sh, op=ALU.add), reads=[xr, res("sh")], writes=[xr])
            to_feature_major(xt, xr, hT, t, hT32=(hT32 if MOE_STAGE >= 0.5 else None))
            if MOE_STAGE < 1:
                continue
            for kc in range(16):
                S.add("pe", lambda e, kc=kc: e.matmul(ps[4][:, 0:36], hT32[:, kc, :], wr32[:, kc, :], start=(kc == 0), stop=(kc == 15)),
                      reads=[res("hT32"), res("wr32")], writes=[PS[4]])
            rr = res("rl")
            S.add("dve", lambda e: e.tensor_tensor(rl[:, 0:36], ps[4][:, 0:36], rbias[:, l, :], op=ALU.add), reads=[PS[4], res("rbias")], writes=[rr])
            S.add("dve", lambda e: e.tensor_reduce(small[:, 8:9], rl[:, 0:4], mybir.AxisListType.X, ALU.max), reads=[rr], writes=[res("sm_g")])
            S.add("dve", lambda e: e.tensor_scalar(rl[:, 40:44], rl[:, 0:4], small[:, 8:9], None, op0=ALU.is_equal), reads=[rr, res("sm_g")], writes=[rr])
            S.add("dve", lambda e: e.tensor_scalar(rl[:, 44:48], rl[:, 0:4], small[:, 8:9], None, op0=ALU.subtract), reads=[rr, res("sm_g")], writes=[rr])
            S.add("act", lambda e: e.activation(rl[:, 44:48], rl[:, 44:48], AF.Exp, accum_out=small[:, 9:10]), reads=[rr], writes=[rr, res("sm_g2")])
            S.add("dve", lambda e: e.reciprocal(small[:, 10:11], small[:, 9:10]), reads=[res("sm_g2")], writes=[res("sm_pg")])
            S.add("dve", lambda e: e.tensor_scalar(rl[:, 40:44], rl[:, 40:44], 1.0, 1e30, op0=ALU.subtract, op1=ALU.mult), reads=[rr], writes=[rr])
            for g_ in range(4):
                S.add("dve", lambda e, g_=g_: e.tensor_scalar(rl[:, 48 + 8 * g_:56 + 8 * g_], rl[:, 4 + 8 * g_:12 + 8 * g_], rl[:, 40 + g_:41 + g_], None, op0=ALU.add),
                      reads=[rr], writes=[rr])
            S.add("dve", lambda e: e.tensor_reduce(small[:, 11:12], rl[:, 48:80], mybir.AxisListType.X, ALU.max), reads=[rr], writes=[res("sm_m1")])
            S.add("dve", lambda e, t=t: e.tensor_scalar(comb[:, t, :], rl[:, 48:80], small[:, 11:12], None, op0=ALU.is_equal), reads=[rr, res("sm_m1")], writes=[res("comb")])
            S.add("dve", lambda e, t=t: e.scalar_tensor_tensor(rl[:, 48:80], comb[:, t, :], -1e30, rl[:, 48:80], op0=ALU.mult, op1=ALU.add),
                  reads=[rr, res("comb")], writes=[rr])
            S.add("dve", lambda e: e.tensor_reduce(small[:, 12:13], rl[:, 48:80], mybir.AxisListType.X, ALU.max), reads=[rr], writes=[res("sm_m2")])
            S.add("dve", lambda e: e.tensor_scalar(rl[:, 4:36], rl[:, 48:80], small[:, 12:13], None, op0=ALU.is_equal), reads=[rr, res("sm_m2")], writes=[rr])
            S.add("dve", lambda e: e.tensor_tensor(small[:, 13:14], small[:, 12:13], small[:, 11:12], op=ALU.subtract), reads=[res("sm_m1"), res("sm_m2")], writes=[res("sm_w")])
            S.add("act", lambda e: e.activation(small[:, 13:14], small[:, 13:14], AF.Exp), reads=[res("sm_w")], writes=[res("sm_w")])
            S.add("dve", lambda e: e.tensor_scalar(small[:, 14:15], small[:, 13:14], 1.0, None, op0=ALU.add), reads=[res("sm_w")], writes=[res("sm_d")])
            S.add("dve", lambda e: e.reciprocal(small[:, 14:15], small[:, 14:15]), reads=[res("sm_d")], writes=[res("sm_d")])
            S.add("dve", lambda e: e.tensor_tensor(small[:, 15:16], small[:, 14:15], small[:, 10:11], op=ALU.mult), reads=[res("sm_d"), res("sm_pg")], writes=[res("sm_t1")])
            S.add("dve", lambda e: e.tensor_tensor(small[:, 16:17], small[:, 15:16], small[:, 13:14], op=ALU.mult), reads=[res("sm_t1"), res("sm_w")], writes=[res("sm_t2")])
            S.add("dve", lambda e, t=t: e.tensor_scalar(comb[:, t, :], comb[:, t, :], small[:, 15:16], None, op0=ALU.mult), reads=[res("comb"), res("sm_t1")], writes=[res("comb")])
            S.add("dve", lambda e, t=t: e.scalar_tensor_tensor(comb[:, t, :], rl[:, 4:36], small[:, 16:17], comb[:, t, :], op0=ALU.mult, op1=ALU.add),
                  reads=[rr, res("sm_t2"), res("comb")], writes=[res("comb")])
        if MOE_STAGE < 2:
            S.barrier()
            return
        HT = [res("hT%d" % t) for t in range(NT)]
        si = [0]

        def nxt():
            s = si[0] % 3
            si[0] += 1
            return s
        for ex in range(32):
            sg_, su_, sd_ = nxt(), nxt(), nxt()
            G = ring[sg_].rearrange("p (k c) -> p k c", c=512)
            U = ring[su_].rearrange("p (k c) -> p k c", c=512)
            Dw = ring[sd_].rearrange("p (k c) -> p k c", c=D)
            stream_w(G, w_gate[l][ex].rearrange("(kc p) c -> p kc c", p=128), RG[sg_], "rg%d" % sg_)
            stream_w(U, w_up[l][ex].rearrange("(kc p) c -> p kc c", p=128), RG[su_], "rg%d" % su_)
            stream_w(Dw, w_down[l][ex].rearrange("(kc p) c -> p kc c", p=128), RG[sd_], "rg%d" % sd_)
            it = 0
            for th in range(2):
                for fc in range(4):
                    bg, bu = (0, 1) if it % 2 == 0 else (2, 3)
                    sgi = it % 2
                    it += 1
                    for kc in range(16):
                        S.add("pe", lambda e, kc=kc, fc=fc, th=th, bg=bg, G=G: e.matmul(ps[bg][:], G[:, kc, fc * 128:(fc + 1) * 128], hT[:, kc, th * 512:(th + 1) * 512],
                                                                                 start=(kc == 0), stop=(kc == 15)),
                              reads=[RG[sg_]] + HT[th * 4:(th + 1) * 4], writes=[PS[bg]])
                    for kc in range(16):
                        S.add("pe", lambda e, kc=kc, fc=fc, th=th, bu=bu, U=U: e.matmul(ps[bu][:], U[:, kc, fc * 128:(fc + 1) * 128], hT[:, kc, th * 512:(th + 1) * 512],
                                                                                 start=(kc == 0), stop=(kc == 15)),
                              reads=[RG[su_]] + HT[th * 4:(th + 1) * 4], writes=[PS[bu]])
                    S.add("act", lambda e, bg=bg, sgi=sgi: e.activation(sgt[sgi], ps[bg][:], AF.Silu), reads=[PS[bg]], writes=[res("sgt%d" % sgi)])
                    S.add("dve", lambda e, bu=bu, sgi=sgi, fc=fc, th=th: e.tensor_tensor(hid[:, fc, th * 512:(th + 1) * 512], sgt[sgi], ps[bu][:], op=ALU.mult),
                          reads=[PS[bu], res("sgt%d" % sgi)], writes=[res("hid%d" % th)])
            it = 0
            for t in range(NT):
                for dc in range(4):
                    b = 4 + (it % 3)
                    it += 1
                    for fc in range(4):
                        S.add("pe", lambda e, fc=fc, t=t, dc=dc, b=b, Dw=Dw: e.matmul(ps[b][:], hid[:, fc, t * 128:(t + 1) * 128], Dw[:, fc, dc * 512:(dc + 1) * 512],
                                                                               start=(fc == 0), stop=(fc == 3)),
                              reads=[RG[sd_], res("hid%d" % (t // 4))], writes=[PS[b]])
                    yr = res("yacc%d" % t)
                    if ex == 0:
                        S.add("dve", lambda e, t=t, dc=dc, b=b, ex=ex: e.tensor_scalar(yacc[:, t, dc * 512:(dc + 1) * 512], ps[b][:], comb[:, t, ex:ex + 1], None, op0=ALU.mult),
                              reads=[PS[b], res("comb")], writes=[yr])
                    else:
                        S.add("dve", lambda e, t=t, dc=dc, b=b, ex=ex: e.scalar_tensor_tensor(yacc[:, t, dc * 512:(dc + 1) * 512], ps[b][:], comb[:, t, ex:ex + 1],
                                                                                              yacc[:, t, dc * 512:(dc + 1) * 512], op0=ALU.mult, op1=ALU.add),
                              reads=[PS[b], res("comb"), yr], writes=[yr])
        if MOE_STAGE < 3:
            S.barrier()
            return
        gt = sc; lgt = sh
        lbt = hT32.rearrange("p a b -> p (a b)")
        bc_load(gt, m_dram[l, 5 * D:6 * D], res("sc"), "bc")
        S.dma("sp", lambda e: e.dma_start(out=lgt, in_=ln2_g[l, :].partition_broadcast(128)), writes=[res("sh")], key="bc")
        S.dma("sp", lambda e: e.dma_start(out=lbt, in_=ln2_b[l, :].partition_broadcast(128)), writes=[res("hT32")], key="bc")
        for t in range(NT):
            xt = xw[0]
            xr = res("xw0")
            yr = res("yacc%d" % t)
            S.dma("sp", lambda e, t=t, xt=xt: e.dma_start(out=xt, in_=x_src[t * 128:(t + 1) * 128, :]), reads=[res("xs")], writes=[xr], key="xl0")
            S.add("dve", lambda e, t=t: e.tensor_tensor(yacc[:, t, :], yacc[:, t, :], gt, op=ALU.mult), reads=[yr, res("sc")], writes=[yr])
            S.add("dve", lambda e, t=t, xt=xt: e.scalar_tensor_tensor(xt, xt, ALPHA, yacc[:, t, :], op0=ALU.mult, op1=ALU.add), reads=[yr, xr], writes=[xr])
            layer_norm_tile(xt, xr, lgt, lbt, xt, xr, [res("sh"), res("hT32")])
            S.dma("sp", lambda e, t=t, xt=xt: e.dma_start(out=x_dst[t * 128:(t + 1) * 128, :], in_=xt), reads=[xr], writes=[res("xdst")], key="xo")
        S.barrier()

    def outproj_block(l, KC, w_out_ap, x_src, x_dst):
        A.reset()
        CW = 8192 // KC
        npiece = D // CW
        yTh = A.bf16([128, KC, 512])
        oh = A.f32([128, 4, D])
        ring = [A.bf16([128, 8192]) for _ in range(3)]
        RG = [Res("oring%d" % i) for i in range(3)]
        gt = A.f32([128, D]); lgt = A.f32([128, D]); lbt = A.f32([128, D])
        xw = [A.f32([128, D]) for _ in range(2)]
        bc_load(gt, m_dram[l, 2 * D:3 * D], res("gt"), "bc")
        S.dma("sp", lambda e: e.dma_start(out=lgt, in_=ln1_g[l, :].partition_broadcast(128)), writes=[res("lgt")], key="bc")
        S.dma("sp", lambda e: e.dma_start(out=lbt, in_=ln1_b[l, :].partition_broadcast(128)), writes=[res("lbt")], key="bc")
        pc = 0
        for th in range(2):
            S.dma("sp", lambda e, th=th: e.dma_start(out=yTh, in_=yT_dram[:, 0:KC, th * 512:(th + 1) * 512]), reads=[res("yT_dram")], writes=[res("yTh")], key="yt")
            it = 0
            for p in range(npiece):
                slot = pc % 3
                pc += 1
                W = ring[slot].rearrange("p (k c) -> p k c", c=CW)
                stream_w(W, w_out_ap[:, p * CW:(p + 1) * CW].rearrange("(kc p) c -> p kc c", p=128), RG[slot], "or%d" % slot)
                for tt in range(4):
                    b = it % 4
                    it += 1
                    for kc in range(KC):
                        S.add("pe", lambda e, kc=kc, tt=tt, b=b, W=W: e.matmul(ps[b][:, 0:CW], yTh[:, kc, tt * 128:(tt + 1) * 128], W[:, kc, :], start=(kc == 0), stop=(kc == KC - 1)),
                              reads=[RG[slot], res("yTh")], writes=[PS[b]])
                    evac(oh[:, tt, p * CW:(p + 1) * CW], ps[b][:, 0:CW], [PS[b]], [res("oh%d" % tt)])
            for tt in range(4):
                t = th * 4 + tt
                xt = xw[t % 2]
                xr = res("xw%d" % (t % 2))
                orr = res("oh%d" % tt)
                S.dma("sp", lambda e, t=t, xt=xt: e.dma_start(out=xt, in_=x_src[t * 128:(t + 1) * 128, :]), reads=[res("xs")], writes=[xr], key="xl%d" % (t % 2))
                S.add("dve", lambda e, tt=tt: e.tensor_tensor(oh[:, tt, :], oh[:, tt, :], gt, op=ALU.mult), reads=[orr, res("gt")], writes=[orr])
                S.add("dve", lambda e, tt=tt, xt=xt: e.scalar_tensor_tensor(xt, xt, ALPHA, oh[:, tt, :], op0=ALU.mult, op1=ALU.add), reads=[orr, xr], writes=[xr])
                layer_norm_tile(xt, xr, lgt, lbt, xt, xr, [res("lgt"), res("lbt")])
                S.dma("sp", lambda e, t=t, xt=xt: e.dma_start(out=x_dst[t * 128:(t + 1) * 128, :], in_=xt), reads=[xr], writes=[res("xdst")], key="xo")
        S.barrier()

    def retention_block(l, x_src):
        A.reset()
        hT = A.bf16([128, 16, NTOK])
        mark0 = A.off
        prologue(l, x_src, hT, 0, 1)
        S.barrier()
        A.off = mark0
        HT = [res("hT%d" % t) for t in range(NT)]
        ring = [A.bf16([128, 8192]) for _ in range(2)]
        RG = [Res("rring%d" % i) for i in range(2)]
        QT = A.bf16([128, 2, NTOK]); KT = A.bf16([128, 2, NTOK])
        k_tm = A.bf16([64, NCH, 256]); v_tm = A.bf16([64, NCH, 512])
        sgf = A.bf16([64, NCH, 512]); sgb = A.bf16([64, NCH, 512])
        yf = A.bf16([64, NCH, 512])
        off_rc = A.off
        rc = A.bf16([64, NCH, 256]); rs = A.bf16([64, NCH, 256])
        yTh = arena[0:128, off_rc:off_rc + 2048].bitcast(BF16).rearrange("p (a b) -> p a b", b=NTOK)
        St2 = [A.f32([128, 2, 512]) for _ in range(2)]; Sb2 = [A.bf16([128, 2, 512]) for _ in range(2)]
        DT2 = [A.f32([64, 64]) for _ in range(2)]; qdec2 = [A.bf16([128, 2, 64]) for _ in range(2)]; kd2 = [A.f32([64, 2]) for _ in range(2)]
        gng = A.f32([64, 512]); gnb = A.f32([64, 512])
        t12 = [A.f32([64, 512]) for _ in range(2)]
        t1 = t12[0]; t2 = t12[1]
        qr = A.bf16([64, 256])
        Pm2 = [A.bf16([64, 64]) for _ in range(2)]; Qd2 = [A.bf16([128, 2, 64]) for _ in range(2)]; Kd2 = [A.bf16([64, 256]) for _ in range(2)]
        ybk = A.bf16([64, NCH, 512])
        stg = [A.bf16([128, 512]) for _ in range(2)]
        stg_i = [0]
        S.dma("pool", lambda e: e.dma_start(out=rs, in_=ropes[:, :, :]), writes=[res("rs")], key="rt")
        pc = [0]

        def piece(c0, ncols):
            slot = pc[0] % 2
            pc[0] += 1
            W = ring[slot][:, 0:16 * ncols].rearrange("p (k c) -> p k c", c=ncols)
            stream_w(W, w_ret_in[:, c0:c0 + ncols].rearrange("(kc p) c -> p kc c", p=128), RG[slot], "rr%d" % slot)
            return W, RG[slot]

        def proj_chunk(W, Wr, c, ncols, b):
            for kc in range(16):
                S.add("pe", lambda e, kc=kc: e.matmul(ps[b][0:64, 0:ncols], hT[:, kc, c * 64:(c + 1) * 64], W[:, kc, :], start=(kc == 0), stop=(kc == 15)),
                      reads=[Wr, HT[c // 2]], writes=[PS[b]])

        def rope_to(dst, dst_res, c, b, kscale):
            p4 = ps[b][0:64, 0:256].rearrange("p (a h f) -> p a h f", a=2, h=2)
            S.add("dve", lambda e: e.tensor_tensor(t1[:, 0:256], ps[b][0:64, 0:256], rc[:, c, :], op=ALU.mult), reads=[PS[b], res("rc")], writes=[res("t1")])
            S.add("dve", lambda e: e.tensor_tensor(t2[:, 0:256].rearrange("p (a h f) -> p a h f", a=2, h=2), p4[:, :, ::-1, :],
                                                   rs[:, c, :].rearrange("p (a h f) -> p a h f", a=2, h=2), op=ALU.mult), reads=[PS[b], res("rs")], writes=[res("t2")])
            if kscale is None:
                S.add("dve", lambda e: e.tensor_tensor(dst, t1[:, 0:256], t2[:, 0:256], op=ALU.add), reads=[res("t1"), res("t2")], writes=[dst_res])
            else:
                S.add("dve", lambda e: e.tensor_tensor(t1[:, 0:256], t1[:, 0:256], t2[:, 0:256], op=ALU.add), reads=[res("t1"), res("t2")], writes=[res("t1")])
                S.add("dve", lambda e: e.tensor_scalar(dst, t1[:, 0:256], kscale, None, op0=ALU.mult), reads=[res("t1")], writes=[dst_res])

        def tr_to(dstT, dstT_res, src, src_res, c):
            for dc in range(2):
                S.add("pe", lambda e, dc=dc: e.transpose(psb[:, dc * 64:(dc + 1) * 64], src[:, dc * 128:(dc + 1) * 128], identb[0:64, 0:64]),
                      reads=[src_res, res("identb")], writes=[PSB])
            evac(dstT[:, :, c * 64:(c + 1) * 64], psb[:, 0:128].rearrange("p (a b) -> p a b", b=64), [PSB], [dstT_res])

        for h in range(8):
            S.dma("pool", lambda e: e.dma_start(out=rc, in_=ropec[:, :, :]), writes=[res("rc")], key="rtc")
            W, Wr = piece(h * 256, 256)
            for c in range(NCH):
                b = c % 2
                proj_chunk(W, Wr, c, 256, b)
                rope_to(qr, res("qr"), c, b, None)
                tr_to(QT, res("QT"), qr, res("qr"), c)
            W, Wr = piece(2048 + h * 256, 256)
            for c in range(NCH):
                b = c % 2
                proj_chunk(W, Wr, c, 256, b)
                rope_to(k_tm[:, c, :], res("k_tm"), c, b, 1.0 / 16.0)
                tr_to(KT, res("KT"), k_tm[:, c, :], res("k_tm"), c)
            for (c0_, dst_, dres_, fn_) in ((4096, v_tm, "v_tm", AF.Copy), (8192, sgf, "sgf", AF.Silu), (12288, sgb, "sgb", AF.Silu)):
                W, Wr = piece(c0_ + h * 512, 512)
                for t in range(NT):
                    b = t % 2
                    sslot = stg_i[0] % 2
                    stg_i[0] += 1
                    for kc in range(16):
                        S.add("pe", lambda e, kc=kc, t=t, b=b, W=W: e.matmul(ps[b][:, :], hT[:, kc, t * 128:(t + 1) * 128], W[:, kc, :], start=(kc == 0), stop=(kc == 15)),
                              reads=[Wr, HT[t]], writes=[PS[b]])
                    S.add("act", lambda e, t=t, b=b, dst_=dst_, fn_=fn_: e.activation(dst_[:, 2 * t, :], ps[b][0:64, :], fn_), reads=[PS[b]], writes=[res(dres_)])
                    S.add("act", lambda e, b=b, sslot=sslot, fn_=fn_: e.activation(stg[sslot][64:128, :], ps[b][64:128, :], fn_), reads=[PS[b]], writes=[res("stg%d" % sslot)])
                    S.dma("sp", lambda e, t=t, sslot=sslot, dst_=dst_: e.dma_start(out=dst_[:, 2 * t + 1, :], in_=stg[sslot][64:128, :]),
                          reads=[res("stg%d" % sslot)], writes=[res(dres_)], key="stg%d" % sslot)
            S.dma("sp", lambda e, h=h: e.dma_start(out=gng, in_=gn_g[h, :].partition_broadcast(64)), writes=[res("gng")], key="gn")
            S.dma("sp", lambda e, h=h: e.dma_start(out=gnb, in_=gn_b[h, :].partition_broadcast(64)), writes=[res("gnb")], key="gn")
            for dr in range(2):
                col = dr * 8 + h
                lgc = lg[:, col:col + 1]
                dif = cst[0:64, 384:448] if dr == 0 else cst[0:64, 512:576]
                tri = cst[0:64, 448:512] if dr == 0 else cst[0:64, 576:640]
                ramp = cst[:, 256:320] if dr == 0 else cst[:, 320:384]
                DTd, qdd, kdd = DT2[dr], qdec2[dr], kd2[dr]
                S.add("act", lambda e, dif=dif, lgc=lgc, DTd=DTd: e.activation(DTd, dif, AF.Exp, scale=lgc[0:64, :]), reads=[res("cst"), res("lg")], writes=[res("DT%d" % dr)])
                S.add("dve", lambda e, tri=tri, DTd=DTd: e.tensor_tensor(DTd, DTd, tri, op=ALU.mult), reads=[res("DT%d" % dr), res("cst")], writes=[res("DT%d" % dr)])
                for dc in range(2):
                    S.add("act", lambda e, dc=dc, ramp=ramp, lgc=lgc, qdd=qdd: e.activation(qdd[:, dc, :], ramp, AF.Exp, scale=lgc), reads=[res("cst"), res("lg")], writes=[res("qdec%d" % dr)])
                S.add("act", lambda e, dr=dr, lgc=lgc, kdd=kdd: e.activation(kdd[:, 0:1], cst[0:64, 640 + dr:641 + dr], AF.Exp, scale=lgc[0:64, :]), reads=[res("cst"), res("lg")], writes=[res("kd%d" % dr)])
                S.add("act", lambda e, dr=dr, lgc=lgc: e.activation(small[:, 20 + dr:21 + dr], cst[:, 642:643], AF.Exp, scale=lgc), reads=[res("cst"), res("lg")], writes=[res("cdec%d" % dr)])
                S.dma("sp", lambda e, dr=dr, h=h: e.dma_start(out=St2[dr], in_=s0[dr, h].rearrange("(dc p) v -> p dc v", p=128)), writes=[res("St%d" % dr)], key="s0%d" % dr)
            for idx in range(NCH):
                for dr in range(2):
                    c = idx if dr == 0 else NCH - 1 - idx
                    Std, Sbd, DTd, qdd, kdd = St2[dr], Sb2[dr], DT2[dr], qdec2[dr], kd2[dr]
                    Pmd, Qdd, Kdd, t1d = Pm2[dr], Qd2[dr], Kd2[dr], t12[dr]
                    bI, bO, bK = (2, 3, 4) if dr == 0 else (5, 6, 0)
                    sm0 = 0 if dr == 0 else 4
                    rSt, rSb, rSm, rT1 = res("St%d" % dr), res("Sb%d" % dr), res("small%d" % dr), res("t1%d" % dr)
                    if idx % 4 == 0 and idx > 0:
                        S.add("dve", lambda e, Std=Std: e.tensor_scalar(Std, Std, flg[:, 0:1], None, op0=ALU.mult), reads=[rSt, res("flg")], writes=[rSt])
                    for dc in range(2):
                        S.add("pe", lambda e, dc=dc, c=c, bI=bI: e.matmul(ps[bI][0:64, 0:64], KT[:, dc, c * 64:(c + 1) * 64], QT[:, dc, c * 64:(c + 1) * 64], start=(dc == 0), stop=(dc == 1)),
                              reads=[res("KT"), res("QT")], writes=[PS[bI]])
                    S.add("dve", lambda e, bI=bI, Pmd=Pmd, DTd=DTd: e.tensor_tensor(Pmd, ps[bI][0:64, 0:64], DTd, op=ALU.mult), reads=[PS[bI], res("DT%d" % dr)], writes=[res("Pm%d" % dr)])
                    S.add("pool", lambda e, c=c, Qdd=Qdd, qdd=qdd: e.tensor_tensor(Qdd, QT[:, :, c * 64:(c + 1) * 64], qdd, op=ALU.mult), reads=[res("QT"), res("qdec%d" % dr)], writes=[res("Qd%d" % dr)])
                    S.add("act", lambda e, Sbd=Sbd, Std=Std: e.copy(Sbd, Std), reads=[rSt], writes=[rSb])
                    S.add("pe", lambda e, c=c, bO=bO, Pmd=Pmd: e.matmul(ps[bO][0:64, :], Pmd, v_tm[:, c, :], start=True, stop=False), reads=[res("Pm%d" % dr), res("v_tm")], writes=[PS[bO]])
                    for dc in range(2):
                        S.add("pe", lambda e, dc=dc, bO=bO, Qdd=Qdd, Sbd=Sbd: e.matmul(ps[bO][0:64, :], Qdd[:, dc, :], Sbd[:, dc, :], start=False, stop=(dc == 1)), reads=[res("Qd%d" % dr), rSb], writes=[PS[bO]])
                    S.add("dve", lambda e, c=c, Kdd=Kdd, kdd=kdd: e.tensor_scalar(Kdd, k_tm[:, c, :], kdd[:, 0:1], None, op0=ALU.mult), reads=[res("k_tm"), res("kd%d" % dr)], writes=[res("Kd%d" % dr)])
                    for dc in range(2):
                        S.add("pe", lambda e, dc=dc, c=c, bK=bK, Kdd=Kdd: e.matmul(ps[bK][:, :], Kdd[:, dc * 128:(dc + 1) * 128], v_tm[:, c, :], start=True, stop=True),
                              reads=[res("Kd%d" % dr), res("v_tm")], writes=[PS[bK]])
                        S.add("dve", lambda e, dc=dc, bK=bK, Std=Std, dr=dr: e.scalar_tensor_tensor(Std[:, dc, :], Std[:, dc, :], small[:, 20 + dr:21 + dr], ps[bK][:, :], op0=ALU.mult, op1=ALU.add),
                              reads=[rSt, res("cdec%d" % dr), PS[bK]], writes=[rSt])
                    S.add("dve", lambda e, bO=bO, dr=dr: e.bn_stats(stt[0:64, dr, :], ps[bO][0:64, :]), reads=[PS[bO]], writes=[res("stt%d" % dr)])
                    S.add("dve", lambda e, dr=dr, sm0=sm0: e.bn_aggr(small[0:64, sm0:sm0 + 2], stt[0:64, dr:dr + 1, :]), reads=[res("stt%d" % dr)], writes=[rSm])
                    S.add("dve", lambda e, sm0=sm0: e.tensor_scalar(small[0:64, sm0 + 2:sm0 + 3], small[0:64, sm0 + 1:sm0 + 2], EPS, None, op0=ALU.add), reads=[rSm], writes=[rSm])
                    S.add("act", lambda e, sm0=sm0: e.activation(small[0:64, sm0 + 2:sm0 + 3], small[0:64, sm0 + 2:sm0 + 3], AF.Ln), reads=[rSm], writes=[rSm])
                    S.add("act", lambda e, sm0=sm0: e.activation(small[0:64, sm0 + 2:sm0 + 3], small[0:64, sm0 + 2:sm0 + 3], AF.Exp, scale=-0.5), reads=[rSm], writes=[rSm])
                    S.add("dve", lambda e, bO=bO, sm0=sm0, t1d=t1d: e.tensor_scalar(t1d, ps[bO][0:64, :], small[0:64, sm0:sm0 + 1], small[0:64, sm0 + 2:sm0 + 3], op0=ALU.subtract, op1=ALU.mult),
                          reads=[PS[bO], rSm], writes=[rT1])
                    S.add("dve", lambda e, t1d=t1d: e.tensor_tensor(t1d, t1d, gng, op=ALU.mult), reads=[rT1, res("gng")], writes=[rT1])
                    S.add("dve", lambda e, t1d=t1d: e.tensor_tensor(t1d, t1d, gnb, op=ALU.add), reads=[rT1, res("gnb")], writes=[rT1])
                    if dr == 0:
                        S.add("dve", lambda e, c=c, t1d=t1d: e.tensor_tensor(yf[:, c, :], t1d, sgf[:, c, :], op=ALU.mult), reads=[rT1, res("sgf")], writes=[res("yf%d" % c)])
                    else:
                        S.add("dve", lambda e, c=c, t1d=t1d: e.tensor_tensor(ybk[:, c, :], t1d, sgb[:, c, :], op=ALU.mult), reads=[rT1, res("sgb")], writes=[res("ybk%d" % c)])
                    if idx % 4 == 3:
                        sq = c // 4
                        S.dma("sp", lambda e, sq=sq, dr=dr, h=h, Std=Std: e.dma_start(out=st_out[sq, dr, h].rearrange("(dc p) v -> p dc v", p=128), in_=Std),
                              reads=[rSt], writes=[res("st_out")], key="so%d" % dr)
            for c in range(NCH):
                S.add("pool", lambda e, c=c: e.tensor_tensor(yf[:, c, :], yf[:, c, :], ybk[:, c, :], op=ALU.add), reads=[res("yf%d" % c), res("ybk%d" % c)], writes=[res("yf%d" % c)])
                po = 256 + (c % 2) * 256
                for vc in range(4):
                    S.add("pe", lambda e, vc=vc, c=c, po=po: e.transpose(psb[:, po + vc * 64:po + (vc + 1) * 64], yf[:, c, vc * 128:(vc + 1) * 128], identb[0:64, 0:64]),
                          reads=[res("yf%d" % c), res("identb")], writes=[PSB])
                evac(yTh[:, :, c * 64:(c + 1) * 64], psb[:, po:po + 256].rearrange("p (a b) -> p a b", b=64), [PSB], [res("rc")])
            S.dma("sp", lambda e, h=h: e.dma_start(out=yT_dram[:, h * 4:(h + 1) * 4, :], in_=yTh), reads=[res("rc")], writes=[res("yT_dram")], key="yo")
        S.barrier()

    def na_block(l, x_src):
        A.reset()
        hT = A.bf16([128, 16, NTOK])
        mark0 = A.off
        prologue(l, x_src, hT, 0, 1)
        S.barrier()
        A.off = mark0
        HT = [res("hT%d" % t) for t in range(NT)]
        wq = A.bf16([128, 16, 128]); wk = A.bf16([128, 16, 128]); wv = A.bf16([128, 16, 128])
        QT = A.bf16([128, NTOK]); KT = A.bf16([128, NTOK])
        Va = A.bf16([64, NCH, 130]); Vc = A.bf16([64, 8, 130])
        KcT = A.bf16([128, 512])
        kc32 = A.f32([128, 4, 128])
        ko = A.f32([64, NCH, 128]); vo = A.f32([64, NCH, 128])
        KT32 = A.f32([128, NTOK]); VT32 = A.f32([128, NTOK])
        bl = A.bf16([64, 128, 64])
        PT2 = [A.bf16([64, 16, 64]) for _ in range(2)]
        ao2 = [A.bf16([64, 128]) for _ in range(2)]; rec2 = [A.f32([64, 2]) for _ in range(2)]
        aT = A.bf16([128, NTOK])
        for h in range(16):
            stream_w(wq, w_na_in[:, h * 128:(h + 1) * 128].rearrange("(kc p) c -> p kc c", p=128), res("wq"), "wq")
            stream_w(wk, w_na_in[:, D + h * 128:D + (h + 1) * 128].rearrange("(kc p) c -> p kc c", p=128), res("wk"), "wk")
            stream_w(wv, w_na_in[:, 2 * D + h * 128:2 * D + (h + 1) * 128].rearrange("(kc p) c -> p kc c", p=128), res("wv"), "wv")
            S.dma("pool", lambda e, h=h: e.dma_start(out=bl, in_=nabias[:, h, :, :]), writes=[res("bl")], key="nb")
            S.dma("sp", lambda e, h=h: e.dma_start(out=kc32, in_=ck[h].rearrange("(a p) d -> p a d", p=128)), writes=[res("kc32")], key="ck")
            S.dma("pool", lambda e, h=h: e.dma_start(out=Vc[:, :, 0:128], in_=cv[h].rearrange("(a p) d -> p a d", p=64)), writes=[res("Vc")], key="cv")
            S.add("dve", lambda e: e.tensor_copy(Vc[:, :, 128:129], cst[0:64, 128:136].rearrange("p (a b) -> p a b", b=1)), reads=[res("cst")], writes=[res("Vc")])
            S.add("dve", lambda e: e.tensor_copy(Va[:, :, 128:129], cst[0:64, 128:144].rearrange("p (a b) -> p a b", b=1)), reads=[res("cst")], writes=[res("Va")])
            for a in range(4):
                S.add("pe", lambda e, a=a: e.transpose(ps[6][:, a * 128:(a + 1) * 128], kc32[:, a, :], ident[:]), reads=[res("kc32"), res("ident")], writes=[PS[6]])
            evac(KcT, ps[6][:], [PS[6]], [res("KcT")])
            for th in range(2):
                for kc in range(16):
                    S.add("pe", lambda e, kc=kc, th=th: e.matmul(ps[0][:], wq[:, kc, :], hT[:, kc, th * 512:(th + 1) * 512], start=(kc == 0), stop=(kc == 15)),
                          reads=[res("wq")] + HT[th * 4:(th + 1) * 4], writes=[PS[0]])
                evac(QT[:, th * 512:(th + 1) * 512], ps[0][:], [PS[0]], [res("QT")], scale=128.0 ** -0.5)
                for kc in range(16):
                    S.add("pe", lambda e, kc=kc, th=th: e.matmul(ps[1][:], wk[:, kc, :], hT[:, kc, th * 512:(th + 1) * 512], start=(kc == 0), stop=(kc == 15)),
                          reads=[res("wk")] + HT[th * 4:(th + 1) * 4], writes=[PS[1]])
                evac(KT32[:, th * 512:(th + 1) * 512], ps[1][:], [PS[1]], [res("KT32")])
                S.add("pool", lambda e, th=th: e.tensor_copy(KT[:, th * 512:(th + 1) * 512], KT32[:, th * 512:(th + 1) * 512]), reads=[res("KT32")], writes=[res("KT")])
                for kc in range(16):
                    S.add("pe", lambda e, kc=kc, th=th: e.matmul(ps[6][:], wv[:, kc, :], hT[:, kc, th * 512:(th + 1) * 512], start=(kc == 0), stop=(kc == 15)),
                          reads=[res("wv")] + HT[th * 4:(th + 1) * 4], writes=[PS[6]])
                evac(VT32[:, th * 512:(th + 1) * 512], ps[6][:], [PS[6]], [res("VT32")])
            for r in range(NCH):
                b = 2 + (r % 2)
                S.add("pe", lambda e, r=r, b=b: e.transpose(ps[b][0:64, 0:128], KT32[:, r * 64:(r + 1) * 64], ident[:]), reads=[res("KT32"), res("ident")], writes=[PS[b]])
                S.add("pe", lambda e, r=r, b=b: e.transpose(ps[b][0:64, 128:256], VT32[:, r * 64:(r + 1) * 64], ident[:]), reads=[res("VT32"), res("ident")], writes=[PS[b]])
                S.add("act", lambda e, r=r, b=b: e.copy(ko[:, r, :], ps[b][0:64, 0:128]), reads=[PS[b]], writes=[res("ko"), res("pstok%d" % b)])
                S.add("act", lambda e, r=r, b=b: e.copy(Va[:, r, 0:128], ps[b][0:64, 128:256]), reads=[PS[b]], writes=[res("Va"), res("pstok%d" % b)])
                S.add("dve", lambda e, r=r, b=b: e.tensor_copy(vo[:, r, :], ps[b][0:64, 128:256]), reads=[PS[b]], writes=[res("vo"), res("pstok%d" % b)])
            S.dma("sp", lambda e, h=h: e.dma_start(out=k_out[h].rearrange("(r p) d -> p r d", p=64), in_=ko), reads=[res("ko")], writes=[res("k_out")], key="kvo")
            S.dma("sp", lambda e, h=h: e.dma_start(out=v_out[h].rearrange("(r p) d -> p r d", p=64), in_=vo), reads=[res("vo")], writes=[res("v_out")], key="kvo")
            for r in range(NCH):
                r0 = min(max(r - 4, 0), 8)
                PT, ao, rec = PT2[r % 2], ao2[r % 2], rec2[r % 2]
                rPT, rao, rrec = res("PT%d" % (r % 2)), res("ao%d" % (r % 2)), res("rec%d" % (r % 2))
                pb = 4 + (r % 2)
                S4 = ps[pb][0:64, :].rearrange("p (j q) -> p j q", q=64)
                pb2 = 0 if pb == 4 else 1
                for j in range(16):
                    bank, jj = (pb, j) if j < 8 else (pb2, j - 8)
                    dst = ps[bank][0:64, jj * 64:(jj + 1) * 64]
                    if j < 8:
                        kr = r0 + j
                        drr = kr - r + 7
                        S.add("pe", lambda e, dst=dst, kr=kr, r=r: e.matmul(dst, KT[:, kr * 64:(kr + 1) * 64], QT[:, r * 64:(r + 1) * 64], start=True, stop=False),
                              reads=[res("KT"), res("QT")], writes=[PS[bank]])
                        S.add("pe", lambda e, dst=dst, r=r, j=j: e.matmul(dst, identb[0:64, 0:64], bl[:, r * 8 + j, :], start=False, stop=True),
                              reads=[res("identb"), res("bl")], writes=[PS[bank]])
                    else:
                        p = j - 8
                        S.add("pe", lambda e, dst=dst, p=p, r=r: e.matmul(dst, KcT[:, p * 64:(p + 1) * 64], QT[:, r * 64:(r + 1) * 64], start=True, stop=True),
                              reads=[res("KcT"), res("QT")], writes=[PS[bank]])
                S.add("act", lambda e, pb=pb, PT=PT: e.activation(PT[:, 0:8, :], ps[pb][0:64, :].rearrange("p (j q) -> p j q", q=64), AF.Exp), reads=[PS[pb]], writes=[rPT])
                S.add("act", lambda e, pb2=pb2, PT=PT: e.activation(PT[:, 8:16, :], ps[pb2][0:64, :].rearrange("p (j q) -> p j q", q=64), AF.Exp, bias=flg[0:64, 1:2]),
                      reads=[PS[pb2], res("flg")], writes=[rPT])
                ob = 2 + (r % 2)
                for j in range(16):
                    rhs = Va[:, r0 + j, 0:129] if j < 8 else Vc[:, j - 8, 0:129]
                    S.add("pe", lambda e, j=j, rhs=rhs, ob=ob, PT=PT: e.matmul(ps[ob][0:64, 0:129], PT[:, j, :], rhs, start=(j == 0), stop=(j == 15)),
                          reads=[rPT, res("Va"), res("Vc")], writes=[PS[ob]])
                S.add("dve", lambda e, ob=ob, rec=rec: e.reciprocal(rec[:, 0:1], ps[ob][0:64, 128:129]), reads=[PS[ob]], writes=[rrec])
                S.add("dve", lambda e, ob=ob, ao=ao, rec=rec: e.tensor_scalar(ao, ps[ob][0:64, 0:128], rec[:, 0:1], None, op0=ALU.mult), reads=[PS[ob], rrec], writes=[rao])
                S.add("pe", lambda e, r=r, ao=ao: e.transpose(psb[:, 512 + (r % 2) * 64:512 + (r % 2 + 1) * 64], ao, identb[0:64, 0:64]), reads=[rao, res("identb")], writes=[PSB])
                evac(aT[:, r * 64:(r + 1) * 64], psb[:, 512 + (r % 2) * 64:512 + (r % 2 + 1) * 64], [PSB], [res("aT")])
            S.dma("sp", lambda e, h=h: e.dma_start(out=yT_dram[:, h, :], in_=aT), reads=[res("aT")], writes=[res("yT_dram")], key="yo")
        S.barrier()

    if upto >= 1:
        retention_block(0, x_in)
    if upto >= 2:
        outproj_block(0, 32, w_ret_out, x_in, xs)
    if upto >= 3:
        moe_block(0, xs, xs, 1)
    if upto >= 4:
        na_block(1, xs)
    if upto >= 5:
        outproj_block(1, 16, w_na_out, xs, xs)
    if upto >= 6:
        moe_block(1, xs, y_out, None)

    block = E(nc.Block())
    S.emit(stack, block)
    stack.close()
    return nc


def _consts():
    c = np.zeros((128, 656), np.float32)
    c[:, 0:128] = np.eye(128, dtype=np.float32)
    c[:, 128:256] = 1.0
    i = np.arange(64, dtype=np.float32)
    c[:, 256:320] = (i + 1.0)[None, :]
    c[:, 320:384] = (64.0 - i)[None, :]
    jj, ii = np.meshgrid(i, i, indexing="ij")
    c[0:64, 384:448] = np.maximum(ii - jj, 0.0)
    c[0:64, 448:512] = (ii >= jj).astype(np.float32)
    c[0:64, 512:576] = np.maximum(jj - ii, 0.0)
    c[0:64, 576:640] = (jj >= ii).astype(np.float32)
    c[0:64, 640] = 63.0 - i
    c[0:64, 641] = i
    c[:, 642] = 64.0
    return c


def _rope_tables(is_sample):
    nf = 64
    t = np.arange(NTOK)
    cos2 = np.ones((NTOK, 2, 2, nf), np.float32)
    sin2 = np.zeros((NTOK, 2, 2, nf), np.float32)
    if is_sample:
        rows = (t // 64).astype(np.float32)
        cols = (t % 64).astype(np.float32)
        inv = (np.float32(10000.0) ** (-np.arange(nf, dtype=np.float32) / np.float32(nf))).astype(np.float32)
        ang = np.stack([rows[:, None] * inv, cols[:, None] * inv], axis=1).astype(np.float32)
        cs, sn = np.cos(ang).astype(np.float32), np.sin(ang).astype(np.float32)
        cos2[:, :, 0, :] = cs
        cos2[:, :, 1, :] = cs
        sin2[:, :, 0, :] = -sn
        sin2[:, :, 1, :] = sn
    rc = cos2.reshape(NCH, 64, 256).transpose(1, 0, 2)
    rs = sin2.reshape(NCH, 64, 256).transpose(1, 0, 2)
    return np.ascontiguousarray(rc), np.ascontiguousarray(rs)


def _na_tables(is_sample, rpb):
    bias = np.zeros((64, 16, 128, 64), np.float32)
    rowm = np.zeros((1, 16, 16, 64), np.float32)
    col = np.arange(64)
    if is_sample:
        c0 = np.clip(col - 8, 0, 48)
        ok = (col[None, :] >= c0[:, None]) & (col[None, :] < c0[:, None] + 16)
        dc = np.clip(col[None, :] - col[:, None], -15, 15) + 15
        g = rpb[:, :, dc]
        g = np.where(ok[None, None], g, np.float32(NEG)).transpose(3, 0, 1, 2)
        for r in range(16):
            r0 = min(max(r - 4, 0), 8)
            for j in range(8):
                bias[:, :, r * 8 + j, :] = g[:, :, r0 + j - r + 7, :]
    else:
        for r in range(16):
            r0 = min(max(r - 4, 0), 8)
            for j in range(8):
                if (r0 + j) // 4 != r // 4:
                    bias[:, :, r * 8 + j, :] = NEG
    return bias, rowm


_NC_CACHE = {}


def _make_in_maps(x_prompt, x_sample, state_ret, cache_na_k, cache_na_v, c, c_ctx, w_mod, b_mod, ln1_g, ln1_b, ln2_g, ln2_b,
           w_ret_in, ret_decay_logit, ret_gn_g, ret_gn_b, w_ret_out, w_na_in, na_rpb, w_na_out, w_rg, b_rg, w_re, b_re,
           w_gate, w_up, w_down):
    f = lambda a: np.ascontiguousarray(np.asarray(a, dtype=np.float32))
    x_prompt, x_sample, state_ret, cache_na_k, cache_na_v = map(f, (x_prompt, x_sample, state_ret, cache_na_k, cache_na_v))
    c, c_ctx = f(c), f(c_ctx)
    w_rt = np.concatenate([f(w_rg), f(w_re).transpose(0, 2, 1, 3).reshape(2, D, 32)], axis=2)
    b_rt = np.concatenate([f(b_rg), f(b_re).reshape(2, 32)], axis=1)
    shared = {
        "consts": _consts(), "w_mod0": f(w_mod)[0], "w_mod1": f(w_mod)[1], "b_mod": f(b_mod), "ln1_g": f(ln1_g), "ln1_b": f(ln1_b), "ln2_g": f(ln2_g), "ln2_b": f(ln2_b),
        "w_ret_in": f(w_ret_in)[0], "decay": f(ret_decay_logit).reshape(1, 16), "gn_g": f(ret_gn_g)[0], "gn_b": f(ret_gn_b)[0],
        "w_ret_out": f(w_ret_out)[0], "w_na_in": f(w_na_in)[0], "w_na_out": f(w_na_out)[0], "w_rt": np.ascontiguousarray(w_rt), "b_rt": np.ascontiguousarray(b_rt),
        "w_gate0": f(w_gate)[0], "w_gate1": f(w_gate)[1], "w_up0": f(w_up)[0], "w_up1": f(w_up)[1],
        "w_down0": f(w_down)[0], "w_down1": f(w_down)[1],
    }
    rpb = f(na_rpb)[0]
    in_maps = []
    for core in range(8):
        smp = core >= 4
        m = dict(shared)
        if smp:
            b = core - 4
            m["x_in"] = x_sample[b]
            cv_ = c[b]
            m["s0"] = np.ascontiguousarray(state_ret[b, 0])
            m["ck"] = np.ascontiguousarray(cache_na_k[b, 0]); m["cv"] = np.ascontiguousarray(cache_na_v[b, 0])
        else:
            m["x_in"] = np.ascontiguousarray(x_prompt[4 * core:4 * core + 4].reshape(NTOK, D))
            cv_ = c_ctx
            m["s0"] = np.zeros((2, 8, 256, 512), np.float32)
            m["ck"] = np.zeros((16, 512, 128), np.float32); m["cv"] = np.zeros((16, 512, 128), np.float32)
        m["cvec"] = np.ascontiguousarray(cv_.reshape(16, 128).T)
        fl = np.zeros((1, 8), np.float32)
        fl[0, 0] = 1.0 if smp else 0.0
        fl[0, 1] = 0.0 if smp else NEG
        m["flags"] = fl
        m["ropec"], m["ropes"] = _rope_tables(smp)
        m["nabias"], _unused = _na_tables(smp, rpb)
        in_maps.append(m)
    return in_maps


def _assemble(R_):
    y_prompt = np.concatenate([R_[i]["y_out"].reshape(4, 256, D) for i in range(4)], axis=0)
    y_sample = np.stack([R_[4 + i]["y_out"] for i in range(4)], axis=0)
    new_state = np.concatenate([R_[i]["st_out"] for i in range(4)], axis=0)[:, None]
    def kv(name):
        parts = []
        for i in range(4):
            a = R_[i][name].reshape(16, 4, 256, 128).transpose(1, 0, 2, 3)
            parts.append(a)
        return np.ascontiguousarray(np.concatenate(parts, axis=0)[:, None])
    return (np.ascontiguousarray(y_prompt), np.ascontiguousarray(y_sample), np.ascontiguousarray(new_state), kv("k_out"), kv("v_out"))


def kernel(**inputs):
    in_maps = _make_in_maps(**inputs)
    if "nc" not in _NC_CACHE:
        _NC_CACHE["nc"] = build()
    res = run_bass_kernel_spmd(_NC_CACHE["nc"], in_maps, core_ids=list(range(8)))
    return _assemble(res.results)
```
